# Optimizing a Trainium2 kernel written in Bass

```python
import jax, jax.numpy as jnp
from jax import lax
import numpy as np

D_MODEL = 1024
BATCH = 4
SEQ = 4096
DEPTH = 1

CHUNK = 64
Q_BLOCK = 128
N_HEADS = 8
Q_LORA = 256
KV_LORA = 128
QK_NOPE = 64
QK_ROPE = 32
V_HEAD = 64
QK_HEAD = QK_NOPE + QK_ROPE
ROPE_THETA = 10000.0
CONV_WIDTH = 512
CONV_K = 3
N_EXPERTS = 32
TOP_K = 4
D_EXPERT = 1024
SWIGLU_LIMIT = 7.0
SWIGLU_ALPHA = 1.702
MOE_BLOCK = 256
RMS_EPS = 1e-6
IN_SPLITS = (Q_LORA, KV_LORA, QK_ROPE, CONV_WIDTH, CONV_WIDTH, CONV_WIDTH, D_MODEL, D_MODEL)
IN_WIDTH = Q_LORA + KV_LORA + QK_ROPE + 3 * CONV_WIDTH + 2 * D_MODEL

kernel_name = 'hybrid_mla_shortconv_moe_adaln_block'


def rms_norm(x, g):
    xf = x.astype(jnp.float32)
    xf = xf * lax.rsqrt(jnp.mean(xf * xf, axis=-1, keepdims=True) + RMS_EPS)
    return xf.astype(x.dtype) * g


def rope_tables(positions, dtype):
    inv_freq = 1.0 / (ROPE_THETA ** (jnp.arange(0, QK_ROPE, 2, dtype=jnp.float32) / QK_ROPE))
    ang = positions.astype(jnp.float32)[..., None] * inv_freq
    return jnp.cos(ang).astype(dtype), jnp.sin(ang).astype(dtype)


def apply_rope(x, cos, sin):
    half = x.shape[-1] // 2
    x1, x2 = x[..., :half], x[..., half:]
    return jnp.concatenate([x1 * cos - x2 * sin, x1 * sin + x2 * cos], axis=-1)


def block_causal_attention(q, k, v):
    B, S, H, Dh = q.shape
    nqb = S // Q_BLOCK
    key_chunk = jnp.arange(S) // CHUNK
    scale = QK_HEAD ** -0.5
    qb = q.reshape(B, nqb, Q_BLOCK, H, Dh).transpose(1, 0, 2, 3, 4)

    def one_block(args):
        q_blk, i = args
        q_chunk = (i * Q_BLOCK + jnp.arange(Q_BLOCK)) // CHUNK
        allowed = key_chunk[None, :] <= q_chunk[:, None]
        s = jnp.einsum('bqhd,bkhd->bhqk', q_blk, k, preferred_element_type=jnp.float32) * scale
        s = jnp.where(allowed[None, None], s, -jnp.inf)
        p = jax.nn.softmax(s, axis=-1).astype(v.dtype)
        return jnp.einsum('bhqk,bkhd->bqhd', p, v)

    out = lax.map(one_block, (qb, jnp.arange(nqb)))
    return out.transpose(1, 0, 2, 3, 4).reshape(B, S, H, v.shape[-1])


def causal_depthwise_conv(z, w):
    C = z.shape[-1]
    return lax.conv_general_dilated(
        z, w[:, None, :].astype(z.dtype), window_strides=(1,), padding=((CONV_K - 1, 0),),
        dimension_numbers=('NWC', 'WIO', 'NWC'), feature_group_count=C)


def hybrid_mixer(h, cos, sin, w_in, q_norm_g, w_uq, kv_norm_g, w_ukv, w_up_attn, conv_w, w_up_conv, w_o):
    B, S, _ = h.shape
    proj = h @ w_in
    split_at = np.cumsum(IN_SPLITS)[:-1].tolist()
    q_lat, kv_lat, k_pe, u, c_gate, b_gate, g_attn, g_conv = jnp.split(proj, split_at, axis=-1)
    q = (rms_norm(q_lat, q_norm_g) @ w_uq).reshape(B, S, N_HEADS, QK_HEAD)
    q = jnp.concatenate([q[..., :QK_NOPE], apply_rope(q[..., QK_NOPE:], cos[:, :, None], sin[:, :, None])], axis=-1)
    kv = (rms_norm(kv_lat, kv_norm_g) @ w_ukv).reshape(B, S, N_HEADS, QK_NOPE + V_HEAD)
    k_nope, v = kv[..., :QK_NOPE], kv[..., QK_NOPE:]
    k_pe = apply_rope(k_pe, cos, sin)[:, :, None, :]
    k = jnp.concatenate([k_nope, jnp.broadcast_to(k_pe, (B, S, N_HEADS, QK_ROPE))], axis=-1)
    attn = block_causal_attention(q, k, v).reshape(B, S, N_HEADS * V_HEAD)
    a_branch = attn @ w_up_attn
    z = causal_depthwise_conv(c_gate * u, conv_w)
    c_branch = (b_gate * z) @ w_up_conv
    merged = jax.nn.sigmoid(g_attn) * a_branch + jax.nn.sigmoid(g_conv) * c_branch
    return merged @ w_o


def moe_ffn(h, router_w, router_b, w_gu, b_gu, w_down, b_down):
    T, D = h.shape
    logits = (h @ router_w + router_b).astype(jnp.float32)
    top_val, top_idx = lax.top_k(logits, TOP_K)
    gate_w = jax.nn.softmax(top_val, axis=-1)
    flat_e = top_idx.reshape(-1)
    flat_tok = jnp.arange(T * TOP_K, dtype=jnp.int32) // TOP_K
    flat_w = gate_w.reshape(-1)
    order = jnp.argsort(flat_e)
    sorted_e = flat_e[order]
    counts = jnp.bincount(flat_e, length=N_EXPERTS)
    padded = (counts + MOE_BLOCK - 1) // MOE_BLOCK * MOE_BLOCK
    pad_end = jnp.cumsum(padded)
    pad_start = pad_end - padded
    grp_start = jnp.cumsum(counts) - counts
    rank = jnp.arange(T * TOP_K) - grp_start[sorted_e]
    dest = pad_start[sorted_e] + rank
    n_rows = T * TOP_K + N_EXPERTS * MOE_BLOCK
    n_blocks = n_rows // MOE_BLOCK
    row_tok = jnp.zeros((n_rows,), jnp.int32).at[dest].set(flat_tok[order])
    row_w = jnp.zeros((n_rows,), jnp.float32).at[dest].set(flat_w[order])
    block_start = jnp.arange(n_blocks) * MOE_BLOCK
    block_expert = jnp.minimum(jnp.searchsorted(pad_end, block_start, side='right'), N_EXPERTS - 1)
    xs = h[row_tok].reshape(n_blocks, MOE_BLOCK, D)

    def expert_block(args):
        xb, e = args
        gu = xb @ w_gu[e] + b_gu[e]
        gate, up = gu[:, :D_EXPERT], gu[:, D_EXPERT:]
        gate = jnp.minimum(gate, SWIGLU_LIMIT)
        up = jnp.clip(up, -SWIGLU_LIMIT, SWIGLU_LIMIT)
        act = (up + 1.0) * (gate * jax.nn.sigmoid(gate * SWIGLU_ALPHA))
        return act @ w_down[e] + b_down[e]

    ys = lax.map(expert_block, (xs, block_expert)).reshape(n_rows, D)
    ys = ys * row_w[:, None].astype(h.dtype)
    return jax.ops.segment_sum(ys, row_tok, num_segments=T)


def setup_inputs(seed: int = 0) -> dict:
    key = jax.random.key(seed)
    ks = jax.random.split(key, 24)
    f32 = jnp.float32
    L, D = DEPTH, D_MODEL

    def nrm(k, shape, scale):
        return jax.random.normal(k, shape, f32) * scale

    x = jax.random.normal(ks[0], (BATCH, SEQ, D), f32)
    c = jax.random.normal(ks[1], (BATCH, D), f32)
    offsets = jax.random.randint(ks[2], (BATCH, 1), 0, 4096, dtype=jnp.int32)
    positions = offsets + jnp.arange(SEQ, dtype=jnp.int32)[None, :]
    return {
        'x': x,
        'c': c,
        'positions': positions,
        'w_ada': nrm(ks[3], (L, D, 6 * D), 0.5 * D ** -0.5),
        'b_ada': nrm(ks[4], (L, 6 * D), 0.01),
        'norm_mix_g': 1.0 + nrm(ks[5], (L, D), 0.05),
        'w_in': nrm(ks[6], (L, D, IN_WIDTH), D ** -0.5),
        'q_norm_g': 1.0 + nrm(ks[7], (L, Q_LORA), 0.05),
        'w_uq': nrm(ks[8], (L, Q_LORA, N_HEADS * QK_HEAD), Q_LORA ** -0.5),
        'kv_norm_g': 1.0 + nrm(ks[9], (L, KV_LORA), 0.05),
        'w_ukv': nrm(ks[10], (L, KV_LORA, N_HEADS * (QK_NOPE + V_HEAD)), KV_LORA ** -0.5),
        'w_up_attn': nrm(ks[11], (L, N_HEADS * V_HEAD, D), (N_HEADS * V_HEAD) ** -0.5),
        'conv_w': nrm(ks[12], (L, CONV_K, CONV_WIDTH), CONV_K ** -0.5),
        'w_up_conv': nrm(ks[13], (L, CONV_WIDTH, D), CONV_WIDTH ** -0.5),
        'w_o': nrm(ks[14], (L, D, D), D ** -0.5),
        'norm_ffn_g': 1.0 + nrm(ks[15], (L, D), 0.05),
        'router_w': nrm(ks[16], (L, D, N_EXPERTS), D ** -0.5),
        'router_b': nrm(ks[17], (L, N_EXPERTS), 0.01),
        'w_gu': nrm(ks[18], (L, N_EXPERTS, D, 2 * D_EXPERT), D ** -0.5),
        'b_gu': nrm(ks[19], (L, N_EXPERTS, 2 * D_EXPERT), 0.01),
        'w_down': nrm(ks[20], (L, N_EXPERTS, D_EXPERT, D), D_EXPERT ** -0.5),
        'b_down': nrm(ks[21], (L, N_EXPERTS, D), 0.01),
        'norm_final_g': 1.0 + nrm(ks[22], (D,), 0.05),
    }


def reference(x, c, positions, w_ada, b_ada, norm_mix_g, w_in, q_norm_g, w_uq, kv_norm_g, w_ukv,
              w_up_attn, conv_w, w_up_conv, w_o, norm_ffn_g, router_w, router_b, w_gu, b_gu,
              w_down, b_down, norm_final_g):
    B, S, D = x.shape
    cos, sin = rope_tables(positions, x.dtype)
    c_act = jax.nn.silu(c)
    for l in range(DEPTH):
        ada = (c_act @ w_ada[l] + b_ada[l])[:, None, :]
        sh_m, sc_m, g_m, sh_f, sc_f, g_f = jnp.split(ada, 6, axis=-1)
        h = rms_norm(x, norm_mix_g[l]) * (1.0 + sc_m) + sh_m
        mix = hybrid_mixer(h, cos, sin, w_in[l], q_norm_g[l], w_uq[l], kv_norm_g[l], w_ukv[l],
                           w_up_attn[l], conv_w[l], w_up_conv[l], w_o[l])
        x = x + g_m * mix
        h = rms_norm(x, norm_ffn_g[l]) * (1.0 + sc_f) + sh_f
        ffn = moe_ffn(h.reshape(B * S, D), router_w[l], router_b[l], w_gu[l], b_gu[l], w_down[l], b_down[l])
        x = x + g_f * ffn.reshape(B, S, D)
    return rms_norm(x, norm_final_g)
```

```python
import numpy as np
from contextlib import ExitStack
import concourse.bass as bass
import concourse.mybir as mybir
from concourse.bass_utils import run_bass_kernel_spmd

F32 = mybir.dt.float32
BF16 = mybir.dt.bfloat16
I32 = mybir.dt.int32
AF = mybir.ActivationFunctionType
ALU = mybir.AluOpType

D = 1024
SEQ = 4096
TOK = 2048
NT = TOK // 128
NG = TOK // 512
NH = 8
NE = 32
EPS = 1e-6
TWO_PI = float(2 * np.pi)
CW1 = 6.28125
CW2 = float(2 * np.pi - 6.28125)
SM_SCALE = float(96 ** -0.5)
NEG = -30000.0

ENGS = ("sync", "scalar", "gpsimd", "vector", "tensor")


class Tok:
    __slots__ = ("sem", "val")

    def __init__(self, sem, val):
        self.sem = sem
        self.val = val


class Buf:
    def __init__(self, name, h):
        self.name = name
        self.h = h
        self.last_w = None
        self.reads = {}
        self.dsem = None
        self.dcnt = 0

    def __getitem__(self, idx):
        return self.h[idx]


class _Rec:
    def __init__(self):
        self.call = None

    def __getattr__(self, name):
        def f(*a, **k):
            assert self.call is None
            self.call = (name, a, k)
            return self
        return f


class Prog:
    def __init__(self, nc, es):
        self.nc = nc
        self.es = es
        self.streams = {e: [] for e in ENGS}
        self.cnt = {e: 0 for e in ENGS}
        self.pending = {e: False for e in ENGS}
        self.esem = {e: es.enter_context(nc.semaphore("c_" + e)) for e in ENGS}
        self.waited = {e: {} for e in ENGS}
        self.nsem = len(ENGS)
        self.fence = {}

    def sb(self, name, shape, dt, es=None):
        h = (es or self.es).enter_context(self.nc.sbuf_tensor(name, list(shape), dt))
        b = Buf(name, h)
        b.reads = dict(self.fence)
        if es is not None:
            es.callback(self.release, [b])
        return b

    def release(self, bufs):
        for b in bufs:
            toks = list(b.reads.values()) + ([b.last_w] if b.last_w is not None else [])
            for t in toks:
                k = id(t.sem)
                if k not in self.fence or self.fence[k].val < t.val:
                    self.fence[k] = t

    def ps(self, name, shape, dt, es=None):
        h = (es or self.es).enter_context(self.nc.psum_tensor(name, list(shape), dt))
        return Buf(name, h)

    def dram(self, name, h):
        return Buf(name, h)

    def _dsem(self, b):
        if b.dsem is None:
            b.dsem = self.es.enter_context(self.nc.semaphore("d_" + b.name))
            self.nsem += 1
        return b.dsem

    def _deps(self, eng, reads, writes):
        deps = []
        for b in reads:
            if b.last_w is not None:
                deps.append(b.last_w)
        for b in writes:
            if b.last_w is not None:
                deps.append(b.last_w)
            deps.extend(b.reads.values())
        out = {}
        for t in deps:
            if t.sem is self.esem[eng] and eng == "tensor":
                continue
            k = id(t.sem)
            if self.waited[eng].get(k, 0) >= t.val:
                continue
            if k not in out or out[k].val < t.val:
                out[k] = t
        for k, t in out.items():
            self.waited[eng][k] = t.val
        return list(out.values())

    def _record(self, tok, reads, writes):
        for b in reads:
            b.reads[id(tok.sem)] = tok
        for b in writes:
            b.last_w = tok
            b.reads = {}

    def op(self, eng, fn, reads=(), writes=(), sig=True):
        deps = self._deps(eng, reads, writes)
        sem = self.esem[eng]
        if sig:
            self.cnt[eng] += 1
            self.pending[eng] = False
            tok = Tok(sem, self.cnt[eng])
        else:
            self.pending[eng] = True
            tok = Tok(sem, self.cnt[eng] + 1)

        rec = _Rec()
        fn(rec)
        name, a, k = rec.call

        def emit(e, deps=deps, name=name, a=a, k=k, sig=sig, sem=sem):
            for t in deps:
                e.wait_ge(t.sem, t.val)
            ins = getattr(e, name)(*a, **k)
            if sig:
                ins.then_inc(sem, 1)

        self.streams[eng].append(emit)
        self._record(tok, reads, writes)

    def dma(self, eng, out, in_, reads=(), writes=(), sembuf=None):
        deps = self._deps(eng, reads, writes)
        sb = sembuf or (writes[0] if writes else reads[0])
        sem = self._dsem(sb)
        sb.dcnt += 16
        tok = Tok(sem, sb.dcnt)

        def emit(e, deps=deps, out=out, in_=in_, sem=sem):
            for t in deps:
                e.wait_ge(t.sem, t.val)
            e.dma_start(out=out, in_=in_).then_inc(sem, 16)

        self.streams[eng].append(emit)
        self._record(tok, reads, writes)
        return tok

    def wait_tok(self, eng, tok):
        def emit(e, tok=tok):
            e.wait_ge(tok.sem, tok.val)
        self.streams[eng].append(emit)

    def finish(self):
        nc = self.nc
        for e in ENGS:
            assert not self.pending[e], e
        with nc.Block() as block:
            @block.sync
            def _(e):
                for f in self.streams["sync"]:
                    f(e)

            @block.scalar
            def _(e):
                for f in self.streams["scalar"]:
                    f(e)

            @block.gpsimd
            def _(e):
                for f in self.streams["gpsimd"]:
                    f(e)

            @block.vector
            def _(e):
                for f in self.streams["vector"]:
                    f(e)

            @block.tensor
            def _(e):
                for f in self.streams["tensor"]:
                    f(e)


def build_program(dbg=None, n_experts=NE, stop_after=None, e_off=0, acc_in=False, final=True):
    nc = bass.Bass("TRN2", target_bir_lowering=False)

    def din(name, shape, dt=F32):
        return nc.dram_tensor(name, list(shape), dt, kind="ExternalInput").ap()

    x_own = din("x_own", [TOK, D])
    x_ctx = din("x_ctx", [TOK, D])
    pos_all = din("pos_all", [1, SEQ], I32)
    flags = din("flags", [128, 2])
    c_col = din("c_col", [128, 8])
    w_ada = din("w_ada", [D, 6 * D])
    b_ada = din("b_ada", [1, 6 * D])
    gmix_col = din("gmix_col", [128, 8])
    gffn_col = din("gffn_col", [128, 8])
    w_in = din("w_in", [D, 4000])
    w_kpe_sw = din("w_kpe_sw", [D, 96])
    qg_col = din("qg_col", [128, 2])
    kvg_col = din("kvg_col", [128, 1])
    w_uq = din("w_uq", [256, 768])
    w_uq_sw = din("w_uq_sw", [256, 768])
    w_uk = din("w_uk", [128, 512])
    w_uv = din("w_uv", [128, 512])
    w_up_attn = din("w_up_attn", [512, D])
    conv_col = din("conv_col", [128, 12])
    w_up_conv = din("w_up_conv", [512, D])
    w_o = din("w_o", [D, D])
    router_w = din("router_w", [D, NE])
    router_b = din("router_b", [1, NE])
    w_gu = din("w_gu", [NE, D, 2 * D])
    bgu_col = din("bgu_col", [128, NE * 16])
    w_down = din("w_down", [NE, D, D])
    b_down = din("b_down", [NE, D])
    gfin_b = din("gfin_b", [128, D])
    rope_c = din("rope_c", [128, 2])
    ident_in = din("ident", [128, 128])
    y = nc.dram_tensor("y", [TOK, D], F32, kind="ExternalOutput").ap()
    acc_prev = din("acc_prev", [TOK, D]) if acc_in else None
    x1_scr = nc.dram_tensor("x1_scr", [TOK, D], F32, kind="Internal").ap()
    dbg_aps = {}
    if dbg:
        for k, shp in dbg.items():
            dbg_aps[k] = nc.dram_tensor("dbg_" + k, list(shp), F32, kind="ExternalOutput").ap()

    es = ExitStack()
    with es:
        P = Prog(nc, es)
        SY, AC, GP, VE, PE = "sync", "scalar", "gpsimd", "vector", "tensor"
        x1d = P.dram("x1d", None)
        dbgx = P.dram("dbgx", None)
        yd = P.dram("yd", None)

        psb = [P.ps("ps%d" % i, [128, 512], F32) for i in range(8)]
        ps_rr = [0]

        def next_ps():
            b = psb[ps_rr[0] % 8]
            ps_rr[0] += 1
            return b

        ident_f = P.sb("ident_f", [128, 128], F32)
        ident_b = P.sb("ident_b", [128, 128], BF16)
        ones_f = P.sb("ones_f", [128, 128], F32)
        ones_b = P.sb("ones_b", [128, 128], BF16)
        zero_c = P.sb("zero_c", [128, 1], F32)
        flg = P.sb("flg", [128, 2], F32)
        adacol = P.sb("adacol", [128, 32], F32)
        scl_m = P.sb("scl_m", [128, 8], F32)
        scl_f = P.sb("scl_f", [128, 8], F32)
        gm_b = P.sb("gm_b", [128, D], F32)
        gf_b = P.sb("gf_b", [128, D], F32)

        P.dma(SY, ident_f[:], ident_in[:, :], writes=[ident_f])
        P.dma(SY, flg[:], flags[:, :], writes=[flg])
        P.op(VE, lambda e: e.tensor_copy(out=ident_b[:], in_=ident_f[:]), [ident_f], [ident_b])
        P.op(VE, lambda e: e.memset(ones_f[:], 1.0), [], [ones_f])
        P.op(VE, lambda e: e.memset(ones_b[:], 1.0), [], [ones_b])
        P.op(VE, lambda e: e.memset(zero_c[:], 0.0), [], [zero_c])

        with ExitStack() as s0:
            ccol = P.sb("ccol", [128, 8], F32, s0)
            cact = P.sb("cact", [128, 8], F32, s0)
            gmc = P.sb("gmc", [128, 8], F32, s0)
            gfc = P.sb("gfc", [128, 8], F32, s0)
            ada_row = P.sb("ada_row", [1, 6 * D], F32, s0)
            bada = P.sb("bada", [1, 6 * D], F32, s0)
            wa = [P.sb("wa%d" % i, [128, 3072], F32, s0) for i in range(2)]
            P.dma(SY, ccol[:], c_col[:, :], writes=[ccol])
            P.dma(SY, gmc[:], gmix_col[:, :], writes=[gmc])
            P.dma(SY, gfc[:], gffn_col[:, :], writes=[gfc])
            P.dma(SY, bada[:], b_ada[:, :], writes=[bada])
            P.op(AC, lambda e: e.activation(out=cact[:], in_=ccol[:], func=AF.Silu), [ccol], [cact])
            it = 0
            for hh in range(2):
                banks = [next_ps() for _ in range(6)]
                for k in range(8):
                    w = wa[it % 2]
                    it += 1
                    P.dma(SY, w[:], w_ada[k * 128:(k + 1) * 128, hh * 3072:(hh + 1) * 3072], writes=[w])
                    for n in range(6):
                        P.op(PE, lambda e, b=banks[n], w=w, k=k, n=n: e.matmul(
                            b[0:1, :], lhsT=cact[:, k:k + 1], rhs=w[:, n * 512:(n + 1) * 512],
                            start=(k == 0), stop=(k == 7)),
                            [cact, w], [banks[n]], sig=True)
                for n in range(6):
                    c0 = hh * 3072 + n * 512
                    P.op(VE, lambda e, b=banks[n], c0=c0: e.tensor_tensor(
                        out=ada_row[0:1, c0:c0 + 512], in0=b[0:1, :], in1=bada[0:1, c0:c0 + 512], op=ALU.add),
                        [banks[n], bada], [ada_row])
            pcol = next_ps()
            segs = [0, 1, 3, 4]
            for si, sg in enumerate(segs):
                for j in range(8):
                    c0 = sg * D + j * 128
                    idx = si * 8 + j
                    P.op(PE, lambda e, c0=c0, idx=idx: e.matmul(
                        pcol[:, idx:idx + 1], lhsT=ada_row[0:1, c0:c0 + 128], rhs=ones_f[0:1, 0:1],
                        start=True, stop=True), [ada_row, ones_f], [pcol])
            P.op(VE, lambda e: e.tensor_copy(out=adacol[:], in_=pcol[:, 0:32]), [pcol], [adacol])
            P.op(VE, lambda e: e.scalar_tensor_tensor(out=scl_m[:], in0=adacol[:, 8:16], scalar=1.0, in1=gmc[:],
                                                      op0=ALU.add, op1=ALU.mult), [adacol, gmc], [scl_m])
            P.op(VE, lambda e: e.scalar_tensor_tensor(out=scl_f[:], in0=adacol[:, 24:32], scalar=1.0, in1=gfc[:],
                                                      op0=ALU.add, op1=ALU.mult), [adacol, gfc], [scl_f])
            for sg, dst in ((2, gm_b), (5, gf_b)):
                for n in range(2):
                    pb = next_ps()
                    c0 = sg * D + n * 512
                    P.op(PE, lambda e, pb=pb, c0=c0: e.matmul(
                        pb[:, :], lhsT=ones_f[0:1, 0:128], rhs=ada_row[0:1, c0:c0 + 512],
                        start=True, stop=True), [ada_row, ones_f], [pb])
                    P.op(VE, lambda e, pb=pb, dst=dst, n=n: e.tensor_copy(
                        out=dst[:, n * 512:(n + 1) * 512], in_=pb[:, :]), [pb], [dst])
            if dbg and "ada" in dbg:
                P.dma(SY, dbg_aps["ada"][0:1, :], ada_row[0:1, :], reads=[ada_row], sembuf=ada_row)
            if dbg and "gm_b" in dbg:
                P.dma(SY, dbg_aps["gm_b"][:, :], gm_b[:], reads=[gm_b], sembuf=gm_b)
            if dbg and "adacol" in dbg:
                P.dma(SY, dbg_aps["adacol"][:, :], adacol[:], reads=[adacol], sembuf=adacol)

        if stop_after == "0":
            P.finish()
            return nc

        def norm_T(xt, xn, ss, rs, scl, shcol_off, dst, dst_c0):
            P.op(VE, lambda e: e.memset(ss[:], 0.0), [], [ss])
            P.op(AC, lambda e: e.activation(out=xn[:], in_=xt[:], func=AF.Square, accum_out=ss[:]),
                 [xt, ss], [xn, ss])
            P.op(VE, lambda e: e.tensor_scalar(out=rs[:], in0=ss[:], scalar1=1.0 / D, scalar2=EPS,
                                               op0=ALU.mult, op1=ALU.add), [ss], [rs])
            P.op(AC, lambda e: e.sqrt(out=rs[:], in_=rs[:]), [rs], [rs]); P.op(VE, lambda e: e.reciprocal(out=rs[:], in_=rs[:]), [rs], [rs])
            P.op(AC, lambda e: e.activation(out=xn[:], in_=xt[:], func=AF.Identity, scale=rs[:, 0:1]),
                 [xt, rs], [xn])
            pt = next_ps()
            ptb = pt[:].bitcast(BF16)
            for j in range(8):
                P.op(PE, lambda e, j=j, ptb=ptb: e.transpose(
                    ptb[:, j * 128:(j + 1) * 128], xn[:, j * 128:(j + 1) * 128], ident_b[:]),
                    [xn, ident_b], [pt], sig=(j == 7))
            for j in range(8):
                P.op(VE, lambda e, j=j, ptb=ptb: e.tensor_scalar(
                    out=dst[:, j, dst_c0:dst_c0 + 128], in0=ptb[:, j * 128:(j + 1) * 128],
                    scalar1=scl[:, j:j + 1], scalar2=adacol[:, shcol_off + j:shcol_off + j + 1],
                    op0=ALU.mult, op1=ALU.add), [pt, scl, adacol], [dst])

        def rstd_bcast(dst, src_ps, inv_n):
            P.op(VE, lambda e: e.tensor_scalar(out=dst[:], in0=src_ps[:], scalar1=inv_n, scalar2=EPS,
                                               op0=ALU.mult, op1=ALU.add), [src_ps], [dst])
            P.op(AC, lambda e: e.sqrt(out=dst[:], in_=dst[:]), [dst], [dst]); P.op(VE, lambda e: e.reciprocal(out=dst[:], in_=dst[:]), [dst], [dst])

        attn_es = ExitStack()
        es.enter_context(attn_es)
        attnT = P.sb("attnT", [128, NH, TOK], BF16, attn_es)

        with ExitStack() as sa:
            KT = P.sb("KT", [128, NH, SEQ], BF16, sa)
            Vt = P.sb("Vt", [128, 32, NH, 65], BF16, sa)
            wkv = P.sb("wkv", [128, 8, 160], BF16, sa)
            wks = P.sb("wks", [128, 8, 96], BF16, sa)
            wq = P.sb("wq", [128, 8, 256], BF16, sa)
            wuq = P.sb("wuq", [128, 2, 768], BF16, sa)
            wuqs = P.sb("wuqs", [128, 2, 768], BF16, sa)
            wuk = P.sb("wuk", [128, 512], BF16, sa)
            wuv = P.sb("wuv", [128, 512], BF16, sa)
            qg = P.sb("qg", [128, 2], F32, sa)
            kvg = P.sb("kvg", [128, 1], F32, sa)
            rpc = P.sb("rpc", [128, 2], F32, sa)
            hTg = P.sb("hTg", [128, 8, 512], BF16, sa)
            xts = [P.sb("xta%d" % i, [128, D], F32, sa) for i in range(2)]
            xn = P.sb("xna", [128, D], BF16, sa)
            ss = P.sb("ssa", [128, 1], F32, sa)
            rs = P.sb("rsa", [128, 1], F32, sa)
            posi = P.sb("posi", [128, 512], I32, sa)
            ang = P.sb("ang", [128, 512], F32, sa)
            cosT = P.sb("cosT", [128, 512], F32, sa)
            sinT = P.sb("sinT", [128, 512], F32, sa)
            tmp = [P.sb("tmpa%d" % i, [128, 512], F32, sa) for i in range(4)]
            sqb = P.sb("sqb", [128, 2, 512], BF16, sa)
            kvn = P.sb("kvn", [128, 512], BF16, sa)
            qn = P.sb("qn", [128, 2, 512], BF16, sa)
            qT = P.sb("qT", [128, NH, 512], BF16, sa)
            PT = [P.sb("PT%d" % i, [128, 512], BF16, sa) for i in range(4)]
            rsum, rbc = tmp[0], tmp[1]

            w_in_k = w_in.rearrange("(k p) n -> p k n", p=128)
            P.dma(GP, wkv[:], w_in_k[:, :, 256:416], writes=[wkv])
            P.dma(GP, wks[:], w_kpe_sw.rearrange("(k p) n -> p k n", p=128), writes=[wks])
            P.dma(GP, wq[:], w_in_k[:, :, 0:256], writes=[wq])
            P.dma(GP, wuq[:], w_uq.rearrange("(k p) n -> p k n", p=128), writes=[wuq])
            P.dma(GP, wuqs[:], w_uq_sw.rearrange("(k p) n -> p k n", p=128), writes=[wuqs])
            P.dma(GP, wuk[:], w_uk[:, :], writes=[wuk])
            P.dma(GP, wuv[:], w_uv[:, :], writes=[wuv])
            P.dma(SY, qg[:], qg_col[:, :], writes=[qg])
            P.dma(SY, kvg[:], kvg_col[:, :], writes=[kvg])
            P.dma(SY, rpc[:], rope_c[:, :], writes=[rpc])
            P.op(GP, lambda e: e.memset(Vt[:], 1.0), [], [Vt])

            R = slice(64, 96)

            def range_reduce_sin(dst, src, shift):
                t0, t1 = tmp[0], tmp[1]
                if shift != 0.0:
                    P.op(VE, lambda e: e.tensor_scalar(out=t1[R, :], in0=src[R, :], scalar1=shift, scalar2=None,
                                                       op0=ALU.add), [src], [t1])
                    a = t1
                else:
                    a = src
                P.op(VE, lambda e: e.tensor_scalar(out=t0[R, :], in0=a[R, :], scalar1=1.0 / TWO_PI, scalar2=None,
                                                   op0=ALU.mult), [a], [t0])
                P.op(VE, lambda e: e.tensor_copy(out=posi[R, :], in_=t0[R, :]), [t0], [posi])
                P.op(VE, lambda e: e.tensor_copy(out=t0[R, :], in_=posi[R, :]), [posi], [t0])
                P.op(VE, lambda e: e.scalar_tensor_tensor(out=dst[R, :], in0=t0[R, :], scalar=-CW1, in1=a[R, :],
                                                          op0=ALU.mult, op1=ALU.add), [t0, a], [dst])
                P.op(VE, lambda e: e.scalar_tensor_tensor(out=dst[R, :], in0=t0[R, :], scalar=-CW2, in1=dst[R, :],
                                                          op0=ALU.mult, op1=ALU.add), [t0, dst], [dst])
                P.op(VE, lambda e: e.tensor_scalar(out=t0[R, :], in0=dst[R, :], scalar1=float(np.pi),
                                                   scalar2=-TWO_PI, op0=ALU.is_gt, op1=ALU.mult), [dst], [t0])
                P.op(VE, lambda e: e.tensor_tensor(out=dst[R, :], in0=dst[R, :], in1=t0[R, :], op=ALU.add),
                     [dst, t0], [dst])
                P.op(VE, lambda e: e.tensor_scalar(out=t0[R, :], in0=dst[R, :], scalar1=-float(np.pi),
                                                   scalar2=TWO_PI, op0=ALU.is_lt, op1=ALU.mult), [dst], [t0])
                P.op(VE, lambda e: e.tensor_tensor(out=dst[R, :], in0=dst[R, :], in1=t0[R, :], op=ALU.add),
                     [dst, t0], [dst])
                P.op(AC, lambda e: e.activation(out=dst[R, :], in_=dst[R, :], func=AF.Sin), [dst], [dst])

            xi = 0
            for kg in range(8):
                own = kg >= 4
                g = kg - 4
                src = x_own if own else x_ctx
                row0 = (kg % 4) * 512
                k0 = kg * 512
                for tt in range(4):
                    xt = xts[xi % 2]
                    xi += 1
                    r0 = row0 + tt * 128
                    P.dma(SY, xt[:], src[r0:r0 + 128, :], writes=[xt])
                    norm_T(xt, xn, ss, rs, scl_m, 0, hTg, tt * 128)
                pA, pB, pC = next_ps(), next_ps(), next_ps()
                for k in range(8):
                    P.op(PE, lambda e, k=k: e.matmul(pA[:, :], lhsT=wkv[:, k, 0:128], rhs=hTg[:, k, :],
                                                     start=(k == 0), stop=(k == 7)), [wkv, hTg], [pA], sig=(k == 7))
                for k in range(8):
                    P.op(PE, lambda e, k=k: e.matmul(pB[0:96, :], lhsT=wkv[:, k, 64:160], rhs=hTg[:, k, :],
                                                     start=(k == 0), stop=(k == 7)), [wkv, hTg], [pB], sig=(k == 7))
                for k in range(8):
                    P.op(PE, lambda e, k=k: e.matmul(pC[0:96, :], lhsT=wks[:, k, :], rhs=hTg[:, k, :],
                                                     start=(k == 0), stop=(k == 7)), [wks, hTg], [pC], sig=(k == 7))
                P.dma(SY, posi[R, :], pos_all[0:1, k0:k0 + 512].broadcast_to([32, 512]), writes=[posi])
                P.op(VE, lambda e: e.tensor_copy(out=ang[R, :], in_=posi[R, :]), [posi], [ang])
                P.op(VE, lambda e: e.tensor_scalar(out=ang[R, :], in0=ang[R, :], scalar1=rpc[R, 0:1], scalar2=None,
                                                   op0=ALU.mult), [ang, rpc], [ang])
                range_reduce_sin(sinT, ang, 0.0)
                range_reduce_sin(cosT, ang, float(np.pi / 2))
                P.op(VE, lambda e: e.tensor_scalar(out=sinT[R, :], in0=sinT[R, :], scalar1=rpc[R, 1:2], scalar2=None,
                                                   op0=ALU.mult), [sinT, rpc], [sinT])
                t2, t3 = tmp[2], tmp[3]
                P.op(VE, lambda e: e.tensor_tensor(out=t2[R, :], in0=pB[R, :], in1=cosT[R, :], op=ALU.mult),
                     [pB, cosT], [t2])
                P.op(VE, lambda e: e.tensor_tensor(out=t3[R, :], in0=pC[R, :], in1=sinT[R, :], op=ALU.mult),
                     [pC, sinT], [t3])
                P.op(VE, lambda e: e.tensor_tensor(out=t2[R, :], in0=t2[R, :], in1=t3[R, :], op=ALU.add),
                     [t2, t3], [t2])
                for h in range(NH):
                    eng = GP if h % 2 else VE
                    P.op(eng, lambda e, h=h: e.tensor_copy(out=KT[R, h, k0:k0 + 512], in_=t2[R, :]), [t2], [KT])
                P.op(AC, lambda e: e.activation(out=sqb[:, 0, :], in_=pA[:, :], func=AF.Square), [pA], [sqb])
                pD = next_ps()
                P.op(PE, lambda e: e.matmul(pD[:, :], lhsT=ones_b[:], rhs=sqb[:, 0, :], start=True, stop=True),
                     [ones_b, sqb], [pD])
                rstd_bcast(tmp[0], pD, 1.0 / 128)
                P.op(VE, lambda e: e.scalar_tensor_tensor(out=kvn[:], in0=pA[:, :], scalar=kvg[:, 0:1], in1=tmp[0][:],
                                                          op0=ALU.mult, op1=ALU.mult), [pA, kvg, tmp[0]], [kvn])
                for h in range(NH):
                    pk = next_ps()
                    P.op(PE, lambda e, h=h, pk=pk: e.matmul(pk[0:64, :], lhsT=wuk[:, h * 64:(h + 1) * 64], rhs=kvn[:],
                                                            start=True, stop=True), [wuk, kvn], [pk])
                    if h % 2:
                        P.op(AC, lambda e, h=h, pk=pk: e.copy(out=KT[0:64, h, k0:k0 + 512], in_=pk[0:64, :]),
                             [pk], [KT])
                    else:
                        P.op(VE, lambda e, h=h, pk=pk: e.tensor_copy(out=KT[0:64, h, k0:k0 + 512], in_=pk[0:64, :]),
                             [pk], [KT])
                for tt in range(4):
                    pv = next_ps()
                    kt = kg * 4 + tt
                    P.op(PE, lambda e, tt=tt, pv=pv: e.matmul(pv[:, :], lhsT=kvn[:, tt * 128:(tt + 1) * 128], rhs=wuv[:],
                                                              start=True, stop=True), [kvn, wuv], [pv])
                    if tt % 2:
                        P.op(AC, lambda e, kt=kt, pv=pv: e.copy(
                            out=Vt[:, kt, :, 0:64], in_=pv[:, :].rearrange("p (h d) -> p h d", h=NH)), [pv], [Vt])
                    else:
                        P.op(VE, lambda e, kt=kt, pv=pv: e.tensor_copy(
                            out=Vt[:, kt, :, 0:64], in_=pv[:, :].rearrange("p (h d) -> p h d", h=NH)), [pv], [Vt])
                if not own:
                    continue
                pE = [next_ps(), next_ps()]
                for c in range(2):
                    for k in range(8):
                        P.op(PE, lambda e, c=c, k=k: e.matmul(pE[c][:, :], lhsT=wq[:, k, c * 128:(c + 1) * 128],
                                                              rhs=hTg[:, k, :], start=(k == 0), stop=(k == 7)),
                             [wq, hTg], [pE[c]], sig=(k == 7))
                    P.op(AC, lambda e, c=c: e.activation(out=sqb[:, c, :], in_=pE[c][:, :], func=AF.Square),
                         [pE[c]], [sqb])
                pD = next_ps()
                for c in range(2):
                    P.op(PE, lambda e, c=c: e.matmul(pD[:, :], lhsT=ones_b[:], rhs=sqb[:, c, :],
                                                     start=(c == 0), stop=(c == 1)), [ones_b, sqb], [pD], sig=(c == 1))
                rstd_bcast(tmp[0], pD, 1.0 / 256)
                for c in range(2):
                    P.op(VE, lambda e, c=c: e.scalar_tensor_tensor(
                        out=qn[:, c, :], in0=pE[c][:, :], scalar=qg[:, c:c + 1], in1=tmp[0][:],
                        op0=ALU.mult, op1=ALU.mult), [pE[c], qg, tmp[0]], [qn])
                for h in range(NH):
                    pF, pG = next_ps(), next_ps()
                    for c in range(2):
                        P.op(PE, lambda e, c=c, h=h, pF=pF: e.matmul(
                            pF[0:96, :], lhsT=wuq[:, c, h * 96:(h + 1) * 96], rhs=qn[:, c, :],
                            start=(c == 0), stop=(c == 1)), [wuq, qn], [pF], sig=(c == 1))
                    for c in range(2):
                        P.op(PE, lambda e, c=c, h=h, pG=pG: e.matmul(
                            pG[0:96, :], lhsT=wuqs[:, c, h * 96:(h + 1) * 96], rhs=qn[:, c, :],
                            start=(c == 0), stop=(c == 1)), [wuqs, qn], [pG], sig=(c == 1))
                    P.op(AC, lambda e, h=h, pF=pF: e.copy(out=qT[0:64, h, :], in_=pF[0:64, :]), [pF], [qT])
                    P.op(VE, lambda e, pF=pF: e.tensor_tensor(out=t2[R, :], in0=pF[R, :], in1=cosT[R, :], op=ALU.mult),
                         [pF, cosT], [t2])
                    P.op(VE, lambda e, pG=pG: e.tensor_tensor(out=t3[R, :], in0=pG[R, :], in1=sinT[R, :], op=ALU.mult),
                         [pG, sinT], [t3])
                    P.op(VE, lambda e, h=h: e.tensor_tensor(out=qT[R, h, :], in0=t2[R, :], in1=t3[R, :], op=ALU.add),
                         [t2, t3], [qT])
                pti = 0
                for h in range(NH):
                    pO = next_ps()
                    nkt = (kg + 1) * 4
                    for kt in range(nkt):
                        j = kt - kg * 4
                        qoff = max(j, 0) * 128
                        n = 512 - qoff
                        pS = next_ps()
                        if pS is pO:
                            pS = next_ps()
                        P.op(PE, lambda e, h=h, kt=kt, qoff=qoff, n=n, pS=pS: e.matmul(
                            pS[:, 0:n], lhsT=KT[0:96, h, kt * 128:(kt + 1) * 128], rhs=qT[0:96, h, qoff:512],
                            start=True, stop=True), [KT, qT], [pS])
                        pt = PT[pti % 4]
                        pti += 1
                        bias = flg[:, 0:1] if kt < 16 else zero_c[:, 0:1]
                        P.op(AC, lambda e, pt=pt, pS=pS, n=n, bias=bias: e.activation(
                            out=pt[:, 0:n], in_=pS[:, 0:n], func=AF.Exp, bias=bias, scale=SM_SCALE),
                            [pS, flg, zero_c], [pt])
                        if j >= 0:
                            P.op(GP, lambda e, pt=pt: e.memset(pt[64:128, 0:64], 0.0), [], [pt])
                        P.op(PE, lambda e, h=h, kt=kt, qoff=qoff, n=n, pt=pt, pO=pO: e.matmul(
                            pO[0:65, qoff:512], lhsT=Vt[:, kt, h, :], rhs=pt[:, 0:n],
                            start=(kt == 0), stop=(kt == nkt - 1)), [Vt, pt], [pO], sig=(kt == nkt - 1))
                    P.op(VE, lambda e, pO=pO: e.reciprocal(out=rsum[64:65, :], in_=pO[64:65, :]), [pO], [rsum])
                    pN = next_ps()
                    if pN is pO:
                        pN = next_ps()
                    P.op(PE, lambda e, pN=pN: e.matmul(pN[0:64, :], lhsT=ones_f[64:65, 0:64], rhs=rsum[64:65, :],
                                                       start=True, stop=True), [ones_f, rsum], [pN])
                    P.op(AC, lambda e, pN=pN: e.copy(out=rbc[0:64, :], in_=pN[0:64, :]), [pN], [rbc])
                    P.op(VE, lambda e, h=h, pO=pO, g=g: e.tensor_tensor(
                        out=attnT[0:64, h, g * 512:(g + 1) * 512], in0=pO[0:64, :], in1=rbc[0:64, :], op=ALU.mult),
                        [pO, rbc], [attnT])
            if dbg and "attnT" in dbg:
                for h in range(NH):
                    P.op(VE, lambda e: e.tensor_copy(out=tmp[0][0:64, :], in_=attnT[0:64, h, 0:512]), [attnT], [tmp[0]])
                    P.dma(SY, dbg_aps["attnT"][h * 64:(h + 1) * 64, :], tmp[0][0:64, :], reads=[tmp[0]], sembuf=tmp[0])

        if stop_after == "A":
            P.finish()
            return nc

        with ExitStack() as sbk:
            win = P.sb("win", [128, 8, 3584], BF16, sbk)
            wua = P.sb("wua", [64, NH, D], BF16, sbk)
            wuc = P.sb("wuc", [128, 4, D], BF16, sbk)
            wo = P.sb("wo", [128, 8, D], BF16, sbk)
            cvc = P.sb("cvc", [128, 12], F32, sbk)
            xres = P.sb("xres", [128, 4, D], F32, sbk)
            xn = P.sb("xnb", [128, D], BF16, sbk)
            ss = P.sb("ssb", [128, 1], F32, sbk)
            rs = P.sb("rsb", [128, 1], F32, sbk)
            hTg = P.sb("hTgb", [128, 8, 512], BF16, sbk)
            cu = P.sb("cu", [128, 4, 514], F32, sbk)
            bzT = P.sb("bzT", [128, 4, 512], BF16, sbk)
            mT = P.sb("mT", [128, 8, 512], BF16, sbk)
            tb = [P.sb("tmpb%d" % i, [128, 512], F32, sbk) for i in range(6)]
            x1t = [P.sb("x1t%d" % i, [128, D], F32, sbk) for i in range(2)]
            w_in_k = w_in.rearrange("(k p) n -> p k n", p=128)
            for k in range(8):
                P.dma(GP, win[:, k, :], w_in_k[:, k, 416:4000], writes=[win])
            P.dma(GP, wua[:], w_up_attn.rearrange("(h p) n -> p h n", p=64), writes=[wua])
            P.dma(GP, wuc[:], w_up_conv.rearrange("(k p) n -> p k n", p=128), writes=[wuc])
            P.dma(GP, wo[:], w_o.rearrange("(k p) n -> p k n", p=128), writes=[wo])
            P.dma(SY, cvc[:], conv_col[:, :], writes=[cvc])

            def ucb(colbase, ch, rhs_ap, nfree, reads):
                pz = next_ps()
                for k in range(8):
                    P.op(PE, lambda e, k=k, pz=pz: e.matmul(
                        pz[:, 0:nfree], lhsT=win[:, k, colbase + ch * 128:colbase + (ch + 1) * 128],
                        rhs=rhs_ap(k), start=(k == 0), stop=(k == 7)), [win] + reads, [pz], sig=(k == 7))
                return pz

            P.dma(SY, xres[:, 0, :], x_ctx[TOK - 128:TOK, :], writes=[xres])
            def norm_T_res(slot, dst_c0):
                xt = xres
                P.op(VE, lambda e: e.memset(ss[:], 0.0), [], [ss])
                P.op(AC, lambda e: e.activation(out=xn[:], in_=xt[:, slot, :], func=AF.Square, accum_out=ss[:]),
                     [xt, ss], [xn, ss])
                P.op(VE, lambda e: e.tensor_scalar(out=rs[:], in0=ss[:], scalar1=1.0 / D, scalar2=EPS,
                                                   op0=ALU.mult, op1=ALU.add), [ss], [rs])
                P.op(AC, lambda e: e.sqrt(out=rs[:], in_=rs[:]), [rs], [rs]); P.op(VE, lambda e: e.reciprocal(out=rs[:], in_=rs[:]), [rs], [rs])
                P.op(AC, lambda e: e.activation(out=xn[:], in_=xt[:, slot, :], func=AF.Identity, scale=rs[:, 0:1]),
                     [xt, rs], [xn])
                pt = next_ps()
                ptb = pt[:].bitcast(BF16)
                for j in range(8):
                    P.op(PE, lambda e, j=j, ptb=ptb: e.transpose(
                        ptb[:, j * 128:(j + 1) * 128], xn[:, j * 128:(j + 1) * 128], ident_b[:]),
                        [xn, ident_b], [pt], sig=(j == 7))
                for j in range(8):
                    P.op(VE, lambda e, j=j, ptb=ptb: e.tensor_scalar(
                        out=hTg[:, j, dst_c0:dst_c0 + 128], in0=ptb[:, j * 128:(j + 1) * 128],
                        scalar1=scl_m[:, j:j + 1], scalar2=adacol[:, j:j + 1],
                        op0=ALU.mult, op1=ALU.add), [pt, scl_m, adacol], [hTg])

            norm_T_res(0, 0)
            for ch in range(4):
                pu = ucb(0, ch, lambda k: hTg[:, k, 0:128], 128, [hTg])
                P.op(AC, lambda e, pu=pu: e.copy(out=tb[0][:, 0:128], in_=pu[:, 0:128]), [pu], [tb[0]])
                pc = ucb(512, ch, lambda k: hTg[:, k, 0:128], 128, [hTg])
                P.op(VE, lambda e, pc=pc: e.tensor_tensor(out=tb[1][:, 0:128], in0=pc[:, 0:128], in1=tb[0][:, 0:128],
                                                          op=ALU.mult), [pc, tb[0]], [tb[1]])
                P.op(VE, lambda e, ch=ch: e.tensor_scalar(out=cu[:, ch, 512:514], in0=tb[1][:, 126:128],
                                                          scalar1=flg[:, 1:2], scalar2=None, op0=ALU.mult),
                     [tb[1], flg], [cu])

            xo = 0
            for g in range(NG):
                t0 = g * 512
                for tt in range(4):
                    P.dma(SY, xres[:, tt, :], x_own[t0 + tt * 128:t0 + (tt + 1) * 128, :], writes=[xres])
                for tt in range(4):
                    norm_T_res(tt, tt * 128)
                for ch in range(4):
                    P.op(VE, lambda e, ch=ch: e.tensor_copy(out=cu[:, ch, 0:2], in_=cu[:, ch, 512:514]), [cu], [cu])
                    pu = ucb(0, ch, lambda k: hTg[:, k, :], 512, [hTg])
                    P.op(AC, lambda e, pu=pu: e.copy(out=tb[0][:], in_=pu[:, :]), [pu], [tb[0]])
                    pc = ucb(512, ch, lambda k: hTg[:, k, :], 512, [hTg])
                    P.op(VE, lambda e, pc=pc, ch=ch: e.tensor_tensor(out=cu[:, ch, 2:514], in0=pc[:, :], in1=tb[0][:],
                                                                     op=ALU.mult), [pc, tb[0]], [cu])
                    P.op(GP, lambda e, ch=ch: e.tensor_scalar(out=tb[1][:], in0=cu[:, ch, 2:514],
                                                              scalar1=cvc[:, ch * 3 + 2:ch * 3 + 3], scalar2=None,
                                                              op0=ALU.mult), [cu, cvc], [tb[1]])
                    P.op(VE, lambda e, ch=ch: e.scalar_tensor_tensor(out=tb[1][:], in0=cu[:, ch, 1:513],
                                                                     scalar=cvc[:, ch * 3 + 1:ch * 3 + 2], in1=tb[1][:],
                                                                     op0=ALU.mult, op1=ALU.add), [cu, cvc, tb[1]], [tb[1]])
                    P.op(VE, lambda e, ch=ch: e.scalar_tensor_tensor(out=tb[1][:], in0=cu[:, ch, 0:512],
                                                                     scalar=cvc[:, ch * 3:ch * 3 + 1], in1=tb[1][:],
                                                                     op0=ALU.mult, op1=ALU.add), [cu, cvc, tb[1]], [tb[1]])
                    pb = ucb(1024, ch, lambda k: hTg[:, k, :], 512, [hTg])
                    P.op(VE, lambda e, pb=pb, ch=ch: e.tensor_tensor(out=bzT[:, ch, :], in0=pb[:, :], in1=tb[1][:],
                                                                     op=ALU.mult), [pb, tb[1]], [bzT])
                for oc in range(8):
                    pga = ucb(1536, oc, lambda k: hTg[:, k, :], 512, [hTg])
                    P.op(AC, lambda e, pga=pga: e.activation(out=tb[2][:], in_=pga[:, :], func=AF.Sigmoid),
                         [pga], [tb[2]])
                    pgc = ucb(2560, oc, lambda k: hTg[:, k, :], 512, [hTg])
                    P.op(AC, lambda e, pgc=pgc: e.activation(out=tb[3][:], in_=pgc[:, :], func=AF.Sigmoid),
                         [pgc], [tb[3]])
                    pab = next_ps()
                    for h in range(NH):
                        P.op(PE, lambda e, h=h, oc=oc, pab=pab, t0=t0: e.matmul(
                            pab[:, :], lhsT=wua[0:64, h, oc * 128:(oc + 1) * 128], rhs=attnT[0:64, h, t0:t0 + 512],
                            start=(h == 0), stop=(h == NH - 1)), [wua, attnT], [pab], sig=(h == NH - 1))
                    pcb = next_ps()
                    for ch in range(4):
                        P.op(PE, lambda e, ch=ch, oc=oc, pcb=pcb: e.matmul(
                            pcb[:, :], lhsT=wuc[:, ch, oc * 128:(oc + 1) * 128], rhs=bzT[:, ch, :],
                            start=(ch == 0), stop=(ch == 3)), [wuc, bzT], [pcb], sig=(ch == 3))
                    P.op(VE, lambda e, pab=pab: e.tensor_tensor(out=tb[4][:], in0=pab[:, :], in1=tb[2][:], op=ALU.mult),
                         [pab, tb[2]], [tb[4]])
                    P.op(VE, lambda e, pcb=pcb: e.tensor_tensor(out=tb[5][:], in0=pcb[:, :], in1=tb[3][:], op=ALU.mult),
                         [pcb, tb[3]], [tb[5]])
                    P.op(GP, lambda e, oc=oc: e.tensor_tensor(out=mT[:, oc, :], in0=tb[4][:], in1=tb[5][:], op=ALU.add),
                         [tb[4], tb[5]], [mT])
                for tt in range(4):
                    xo_t = x1t[xo % 2]
                    xo += 1
                    for n in range(2):
                        pm = next_ps()
                        for oc in range(8):
                            P.op(PE, lambda e, oc=oc, tt=tt, n=n, pm=pm: e.matmul(
                                pm[:, :], lhsT=mT[:, oc, tt * 128:(tt + 1) * 128], rhs=wo[:, oc, n * 512:(n + 1) * 512],
                                start=(oc == 0), stop=(oc == 7)), [mT, wo], [pm], sig=(oc == 7))
                        P.op(VE, lambda e, pm=pm, n=n, xo_t=xo_t: e.tensor_tensor(
                            out=xo_t[:, n * 512:(n + 1) * 512], in0=pm[:, :], in1=gm_b[:, n * 512:(n + 1) * 512],
                            op=ALU.mult), [pm, gm_b], [xo_t])
                    P.op(GP, lambda e, tt=tt, xo_t=xo_t: e.tensor_tensor(out=xo_t[:], in0=xo_t[:], in1=xres[:, tt, :],
                                                                         op=ALU.add), [xo_t, xres], [xo_t])
                    r0 = t0 + tt * 128
                    P.dma(SY, x1_scr[r0:r0 + 128, :], xo_t[:], reads=[xo_t], writes=[x1d], sembuf=xo_t)
                    if dbg and "x1" in dbg:
                        P.dma(SY, dbg_aps["x1"][r0:r0 + 128, :], xo_t[:], reads=[xo_t], writes=[dbgx], sembuf=dbgx)
        attn_es.close()
        if stop_after == "B":
            P.finish()
            return nc

        with ExitStack() as sc:
            acc = P.sb("acc", [128, NT, D], F32, sc)
            h2T = P.sb("h2T", [128, 8, TOK], BF16, sc)
            gates = P.sb("gates", [128, NT, NE], F32, sc)
            gT = P.sb("gT", [32, 128], F32, sc)
            rw = P.sb("rw", [128, 8, NE], BF16, sc)
            rb = P.sb("rb", [1, NE], BF16, sc)
            bgu = P.sb("bgu", [128, NE * 16], F32, sc)
            bd = P.sb("bd", [32, D], F32, sc)
            gfin = P.sb("gfin", [128, D], F32, sc)
            xn = P.sb("xnc", [128, D], BF16, sc)
            ss = P.sb("ssc", [128, 1], F32, sc)
            rs = P.sb("rsc", [128, 1], F32, sc)
            sm = [P.sb("smc%d" % i, [128, 32], F32, sc) for i in range(4)]
            m8 = P.sb("m8", [128, 8], F32, sc)
            wg = [P.sb("wg%d" % i, [128, 8, 512], BF16, sc) for i in range(2)]
            wu = [P.sb("wu%d" % i, [128, 8, 512], BF16, sc) for i in range(2)]
            wd = [P.sb("wd%d" % i, [128, 4, D], BF16, sc) for i in range(2)]
            wst = [P.sb("wst%d" % i, [128, D], F32, sc) for i in range(2)]
            actT = [P.sb("actT%d" % i, [128, 4, 512], BF16, sc) for i in range(2)]
            tcs = [[P.sb("tc%d_%d" % (i, j), [128, 512], F32, sc) for j in range(5)] for i in range(2)]
            ot = wst

            P.dma(GP, rw[:], router_w.rearrange("(k p) n -> p k n", p=128), writes=[rw])
            P.dma(GP, rb[:], router_b[:, :], writes=[rb])
            P.dma(SY, bgu[:], bgu_col[:, :], writes=[bgu])
            P.dma(SY, bd[:], b_down[:, :], writes=[bd])
            P.dma(SY, gfin[:], gfin_b[:, :], writes=[gfin])
            P.op(VE, lambda e: e.tensor_tensor(out=bd[:], in0=bd[:], in1=gf_b[0:32, :], op=ALU.mult), [bd, gf_b], [bd])

            for t in range(NT):
                r0 = t * 128
                P.dma(SY, acc[:, t, :], x1_scr[r0:r0 + 128, :], reads=[x1d], writes=[acc])
                P.op(VE, lambda e: e.memset(ss[:], 0.0), [], [ss])
                P.op(AC, lambda e, t=t: e.activation(out=xn[:], in_=acc[:, t, :], func=AF.Square, accum_out=ss[:]),
                     [acc, ss], [xn, ss])
                P.op(VE, lambda e: e.tensor_scalar(out=rs[:], in0=ss[:], scalar1=1.0 / D, scalar2=EPS,
                                                   op0=ALU.mult, op1=ALU.add), [ss], [rs])
                P.op(AC, lambda e: e.sqrt(out=rs[:], in_=rs[:]), [rs], [rs]); P.op(VE, lambda e: e.reciprocal(out=rs[:], in_=rs[:]), [rs], [rs])
                P.op(AC, lambda e, t=t: e.activation(out=xn[:], in_=acc[:, t, :], func=AF.Identity, scale=rs[:, 0:1]),
                     [acc, rs], [xn])
                pt = next_ps()
                ptb = pt[:].bitcast(BF16)
                for j in range(8):
                    P.op(PE, lambda e, j=j, ptb=ptb: e.transpose(
                        ptb[:, j * 128:(j + 1) * 128], xn[:, j * 128:(j + 1) * 128], ident_b[:]),
                        [xn, ident_b], [pt], sig=(j == 7))
                for j in range(8):
                    P.op(VE, lambda e, j=j, ptb=ptb, r0=r0: e.tensor_scalar(
                        out=h2T[:, j, r0:r0 + 128], in0=ptb[:, j * 128:(j + 1) * 128],
                        scalar1=scl_f[:, j:j + 1], scalar2=adacol[:, 16 + j:17 + j],
                        op0=ALU.mult, op1=ALU.add), [pt, scl_f, adacol], [h2T])
                pl = next_ps()
                for k in range(8):
                    P.op(PE, lambda e, k=k, r0=r0, pl=pl: e.matmul(
                        pl[:, 0:NE], lhsT=h2T[:, k, r0:r0 + 128], rhs=rw[:, k, :], start=(k == 0), stop=False),
                        [h2T, rw], [pl], sig=False)
                P.op(PE, lambda e, pl=pl: e.matmul(pl[:, 0:NE], lhsT=ones_b[0:1, 0:128], rhs=rb[0:1, :],
                                                   start=False, stop=True), [ones_b, rb], [pl])
                lg, ex, mk, _ = sm
                P.op(VE, lambda e, pl=pl: e.tensor_copy(out=lg[:], in_=pl[:, 0:NE]), [pl], [lg])
                P.op(VE, lambda e: e.max(out=m8[:], in_=lg[:]), [lg], [m8])
                P.op(VE, lambda e: e.tensor_scalar(out=mk[:], in0=lg[:], scalar1=m8[:, 3:4], scalar2=None,
                                                   op0=ALU.is_ge), [lg, m8], [mk])
                P.op(VE, lambda e: e.tensor_scalar(out=lg[:], in0=lg[:], scalar1=m8[:, 0:1], scalar2=None,
                                                   op0=ALU.subtract), [lg, m8], [lg])
                P.op(AC, lambda e: e.activation(out=ex[:], in_=lg[:], func=AF.Exp), [lg], [ex])
                P.op(VE, lambda e: e.tensor_tensor(out=ex[:], in0=ex[:], in1=mk[:], op=ALU.mult), [ex, mk], [ex])
                P.op(VE, lambda e: e.reduce_sum(out=ss[:], in_=ex[:], axis=mybir.AxisListType.X), [ex], [ss])
                P.op(VE, lambda e: e.reciprocal(out=rs[:], in_=ss[:]), [ss], [rs])
                P.op(VE, lambda e, t=t: e.tensor_scalar(out=gates[:, t, :], in0=ex[:], scalar1=rs[:, 0:1], scalar2=None,
                                                        op0=ALU.mult), [ex, rs], [gates])
            if dbg and "gates" in dbg:
                P.dma(SY, dbg_aps["gates"][:, :], gates[:].rearrange("p t e -> p (t e)"), reads=[gates], sembuf=gates)

            if acc_in:
                for t in range(NT):
                    P.dma(SY, acc[:, t, :], acc_prev[t * 128:(t + 1) * 128, :], writes=[acc])
            w_gu_k = w_gu.rearrange("e (k p) n -> e p k n", p=128)
            w_dn_k = w_down.rearrange("e (k p) n -> e p k n", p=128)
            it = 0
            st_i = 0
            gi = 0
            for ex_i in range(e_off, e_off + n_experts):
                for hf in range(2):
                    b = it % 2
                    it += 1
                    P.dma(GP, wg[b][:], w_gu_k[ex_i, :, :, hf * 512:(hf + 1) * 512], writes=[wg[b]])
                    P.dma(GP, wu[b][:], w_gu_k[ex_i, :, :, D + hf * 512:D + (hf + 1) * 512], writes=[wu[b]])
                    for fc in range(4):
                        st = wst[st_i % 2]
                        st_i += 1
                        P.dma(SY, st[:], w_dn_k[ex_i, :, hf * 4 + fc, :], writes=[st])
                        P.op(GP, lambda e, st=st, b=b, fc=fc: e.tensor_tensor(out=wd[b][:, fc, :], in0=st[:], in1=gf_b[:],
                                                                              op=ALU.mult), [st, gf_b], [wd[b]])
                    for g in range(NG):
                        t0 = g * 512
                        aT = actT[gi % 2]
                        tc = tcs[gi % 2]
                        gi += 1
                        for fc in range(4):
                            cg = ex_i * 16 + hf * 4 + fc
                            cuu = ex_i * 16 + 8 + hf * 4 + fc
                            pG, pU = next_ps(), next_ps()
                            for k in range(8):
                                P.op(PE, lambda e, k=k, fc=fc, b=b, pG=pG, t0=t0: e.matmul(
                                    pG[:, :], lhsT=wg[b][:, k, fc * 128:(fc + 1) * 128], rhs=h2T[:, k, t0:t0 + 512],
                                    start=(k == 0), stop=(k == 7)), [wg[b], h2T], [pG], sig=(k == 7))
                            for k in range(8):
                                P.op(PE, lambda e, k=k, fc=fc, b=b, pU=pU, t0=t0: e.matmul(
                                    pU[:, :], lhsT=wu[b][:, k, fc * 128:(fc + 1) * 128], rhs=h2T[:, k, t0:t0 + 512],
                                    start=(k == 0), stop=(k == 7)), [wu[b], h2T], [pU], sig=(k == 7))
                            gcl, sg_, ub, uc, gs = tc
                            P.op(VE, lambda e, pG=pG, cg=cg, gcl=gcl: e.tensor_scalar(
                                out=gcl[:], in0=pG[:, :], scalar1=bgu[:, cg:cg + 1], scalar2=7.0,
                                op0=ALU.add, op1=ALU.min), [pG, bgu], [gcl])
                            P.op(AC, lambda e, gcl=gcl, sg_=sg_: e.activation(out=sg_[:], in_=gcl[:], func=AF.Sigmoid,
                                                                              scale=1.702), [gcl], [sg_])
                            P.op(AC, lambda e, pU=pU, cuu=cuu, ub=ub: e.activation(
                                out=ub[:], in_=pU[:, :], func=AF.Identity, bias=bgu[:, cuu:cuu + 1]), [pU, bgu], [ub])
                            P.op(GP, lambda e, ub=ub, uc=uc: e.tensor_scalar(out=uc[:], in0=ub[:], scalar1=7.0, scalar2=-7.0,
                                                                             op0=ALU.min, op1=ALU.max), [ub], [uc])
                            P.op(GP, lambda e, gcl=gcl, sg_=sg_, gs=gs: e.tensor_tensor(out=gs[:], in0=gcl[:], in1=sg_[:],
                                                                                        op=ALU.mult), [gcl, sg_], [gs])
                            P.op(VE, lambda e, uc=uc, gs=gs, aT=aT, fc=fc: e.scalar_tensor_tensor(
                                out=aT[:, fc, :], in0=uc[:], scalar=1.0, in1=gs[:], op0=ALU.add, op1=ALU.mult),
                                [uc, gs], [aT])
                        for tt in range(4):
                            t = g * 4 + tt
                            for n in range(2):
                                pY = next_ps()
                                for fc in range(4):
                                    P.op(PE, lambda e, fc=fc, tt=tt, n=n, b=b, pY=pY, aT=aT: e.matmul(
                                        pY[:, :], lhsT=aT[:, fc, tt * 128:(tt + 1) * 128],
                                        rhs=wd[b][:, fc, n * 512:(n + 1) * 512],
                                        start=(fc == 0), stop=(fc == 3)), [aT, wd[b]], [pY], sig=(fc == 3))
                                P.op(VE, lambda e, pY=pY, t=t, n=n, ex_i=ex_i: e.scalar_tensor_tensor(
                                    out=acc[:, t, n * 512:(n + 1) * 512], in0=pY[:, :],
                                    scalar=gates[:, t, ex_i:ex_i + 1], in1=acc[:, t, n * 512:(n + 1) * 512],
                                    op0=ALU.mult, op1=ALU.add), [pY, gates, acc], [acc])

            if final:
                last = None
                for t in range(NT):
                    r0 = t * 128
                    pg = next_ps()
                    P.op(PE, lambda e, t=t, pg=pg: e.transpose(pg[0:32, 0:128], gates[:, t, :], ident_f[:]),
                         [gates, ident_f], [pg])
                    P.op(VE, lambda e, pg=pg: e.tensor_copy(out=gT[:], in_=pg[0:32, 0:128]), [pg], [gT])
                    for n in range(2):
                        pbd = next_ps()
                        P.op(PE, lambda e, n=n, pbd=pbd: e.matmul(
                            pbd[:, :], lhsT=gT[0:32, :], rhs=bd[0:32, n * 512:(n + 1) * 512],
                            start=True, stop=True), [gT, bd], [pbd])
                        P.op(VE, lambda e, t=t, n=n, pbd=pbd: e.tensor_tensor(
                            out=acc[:, t, n * 512:(n + 1) * 512], in0=pbd[:, :], in1=acc[:, t, n * 512:(n + 1) * 512],
                            op=ALU.add), [pbd, acc], [acc])
                    P.op(VE, lambda e: e.memset(ss[:], 0.0), [], [ss])
                    P.op(AC, lambda e, t=t: e.activation(out=xn[:], in_=acc[:, t, :], func=AF.Square, accum_out=ss[:]),
                         [acc, ss], [xn, ss])
                    P.op(VE, lambda e: e.tensor_scalar(out=rs[:], in0=ss[:], scalar1=1.0 / D, scalar2=EPS,
                                                       op0=ALU.mult, op1=ALU.add), [ss], [rs])
                    P.op(AC, lambda e: e.sqrt(out=rs[:], in_=rs[:]), [rs], [rs]); P.op(VE, lambda e: e.reciprocal(out=rs[:], in_=rs[:]), [rs], [rs])
                    o = ot[t % 2]
                    P.op(VE, lambda e, t=t, o=o: e.scalar_tensor_tensor(out=o[:], in0=acc[:, t, :], scalar=rs[:, 0:1],
                                                                        in1=gfin[:], op0=ALU.mult, op1=ALU.mult),
                         [acc, rs, gfin], [o])
                    last = P.dma(SY, y[r0:r0 + 128, :], o[:], reads=[o], writes=[yd], sembuf=o)
            else:
                for t in range(NT):
                    r0 = t * 128
                    o = ot[t % 2]
                    P.op(GP, lambda e, t=t, o=o: e.tensor_copy(out=o[:], in_=acc[:, t, :]), [acc], [o])
                    P.dma(SY, y[r0:r0 + 128, :], o[:], reads=[o], writes=[yd], sembuf=o)
            for o in ot:
                P.wait_tok(SY, Tok(o.dsem, o.dcnt))
        P.finish()
    return nc


def _host_layout(inputs):
    f = np.float32
    x = np.asarray(inputs["x"], f)
    c = np.asarray(inputs["c"], f)
    pos = np.asarray(inputs["positions"], np.int32)
    w_in = np.ascontiguousarray(np.asarray(inputs["w_in"], f)[0])
    w_uq = np.asarray(inputs["w_uq"], f)[0]
    w_ukv = np.asarray(inputs["w_ukv"], f)[0]
    col = lambda v, n: np.ascontiguousarray(np.asarray(v, f).reshape(n, 128).T)
    w_kpe_sw = np.ascontiguousarray(np.concatenate([w_in[:, 320:384], w_in[:, 400:416], w_in[:, 384:400]], axis=1))
    wq3 = w_uq.reshape(256, NH, 96)
    w_uq_sw = np.ascontiguousarray(
        np.concatenate([wq3[:, :, 0:64], wq3[:, :, 80:96], wq3[:, :, 64:80]], axis=2).reshape(256, 768))
    wkv3 = w_ukv.reshape(128, NH, 128)
    w_uk = np.ascontiguousarray(wkv3[:, :, 0:64].reshape(128, 512))
    w_uv = np.ascontiguousarray(wkv3[:, :, 64:128].reshape(128, 512))
    conv_w = np.asarray(inputs["conv_w"], f)[0]
    conv_col = np.ascontiguousarray(conv_w.reshape(3, 4, 128).transpose(2, 1, 0).reshape(128, 12))
    b_gu = np.asarray(inputs["b_gu"], f)[0]
    bgu_col = np.ascontiguousarray(b_gu.reshape(NE, 16, 128).transpose(2, 0, 1).reshape(128, NE * 16))
    inv_freq = (1.0 / (10000.0 ** (np.arange(0, 32, 2, dtype=np.float32) / np.float32(32)))).astype(f)
    rope_c = np.zeros((128, 2), f)
    for p in range(64, 96):
        rope_c[p, 0] = inv_freq[(p - 64) % 16]
        rope_c[p, 1] = -1.0 if p < 80 else 1.0
    shared = {
        "w_ada": np.ascontiguousarray(np.asarray(inputs["w_ada"], f)[0]),
        "b_ada": np.ascontiguousarray(np.asarray(inputs["b_ada"], f)[0].reshape(1, -1)),
        "gmix_col": col(np.asarray(inputs["norm_mix_g"])[0], 8),
        "gffn_col": col(np.asarray(inputs["norm_ffn_g"])[0], 8),
        "w_in": w_in,
        "w_kpe_sw": w_kpe_sw,
        "qg_col": col(np.asarray(inputs["q_norm_g"])[0], 2),
        "kvg_col": col(np.asarray(inputs["kv_norm_g"])[0], 1),
        "w_uq": np.ascontiguousarray(w_uq),
        "w_uq_sw": w_uq_sw,
        "w_uk": w_uk,
        "w_uv": w_uv,
        "w_up_attn": np.ascontiguousarray(np.asarray(inputs["w_up_attn"], f)[0]),
        "conv_col": conv_col,
        "w_up_conv": np.ascontiguousarray(np.asarray(inputs["w_up_conv"], f)[0]),
        "w_o": np.ascontiguousarray(np.asarray(inputs["w_o"], f)[0]),
        "router_w": np.ascontiguousarray(np.asarray(inputs["router_w"], f)[0]),
        "router_b": np.ascontiguousarray(np.asarray(inputs["router_b"], f)[0].reshape(1, -1)),
        "w_gu": np.ascontiguousarray(np.asarray(inputs["w_gu"], f)[0]),
        "bgu_col": bgu_col,
        "w_down": np.ascontiguousarray(np.asarray(inputs["w_down"], f)[0]),
        "b_down": np.ascontiguousarray(np.asarray(inputs["b_down"], f)[0]),
        "gfin_b": np.ascontiguousarray(np.broadcast_to(np.asarray(inputs["norm_final_g"], f)[None, :], (128, D))),
        "rope_c": rope_c,
        "ident": np.eye(128, dtype=f),
    }
    in_maps = []
    for i in range(8):
        b, half = i // 2, i % 2
        own = slice(half * TOK, (half + 1) * TOK)
        flags = np.zeros((128, 2), f)
        flags[:, 0] = 0.0 if half == 1 else NEG
        flags[:, 1] = 1.0 if half == 1 else 0.0
        m = dict(shared)
        m["x_own"] = np.ascontiguousarray(x[b, own])
        m["x_ctx"] = np.ascontiguousarray(x[b, 0:TOK])
        m["pos_all"] = np.ascontiguousarray(np.concatenate([pos[b, 0:TOK], pos[b, own]]).reshape(1, SEQ))
        m["flags"] = flags
        m["c_col"] = col(c[b], 8)
        in_maps.append(m)
    return in_maps


EXPERTS_PER_LAUNCH = 8


def kernel(**inputs):
    in_maps = _host_layout(inputs)
    n_l = NE // EXPERTS_PER_LAUNCH
    res = None
    for j in range(n_l):
        nc = build_program(n_experts=EXPERTS_PER_LAUNCH, e_off=j * EXPERTS_PER_LAUNCH, acc_in=(j > 0),
                           final=(j == n_l - 1))
        if j > 0:
            for i in range(8):
                in_maps[i]["acc_prev"] = np.ascontiguousarray(res.results[i]["y"])
        res = run_bass_kernel_spmd(nc, in_maps, core_ids=list(range(8)))
    out = np.zeros((4, SEQ, D), np.float32)
    for i in range(8):
        b, half = i // 2, i % 2
        out[b, half * TOK:(half + 1) * TOK] = res.results[i]["y"]
    return out
```

```python
import numpy as np
from contextlib import ExitStack
import concourse.bass as bass
import concourse.mybir as mybir
from concourse.bass_utils import run_bass_kernel_spmd

F32 = mybir.dt.float32
BF16 = mybir.dt.bfloat16
I32 = mybir.dt.int32
AF = mybir.ActivationFunctionType
ALU = mybir.AluOpType

D = 1024
SEQ = 4096
TOK = 2048
NT = TOK // 128
NG = TOK // 512
NH = 8
NE = 32
EPS = 1e-6
TWO_PI = float(2 * np.pi)
CW1 = 6.28125
CW2 = float(2 * np.pi - 6.28125)
SM_SCALE = float(96 ** -0.5)
NEG = -30000.0

ENGS = ("sync", "scalar", "gpsimd", "vector", "tensor")


class Tok:
    __slots__ = ("sem", "val")

    def __init__(self, sem, val):
        self.sem = sem
        self.val = val


class Buf:
    def __init__(self, name, h):
        self.name = name
        self.h = h
        self.last_w = None
        self.reads = {}
        self.dsem = None
        self.dcnt = 0

    def __getitem__(self, idx):
        return self.h[idx]


class _Rec:
    def __init__(self):
        self.call = None

    def __getattr__(self, name):
        def f(*a, **k):
            assert self.call is None
            self.call = (name, a, k)
            return self
        return f


class Prog:
    def __init__(self, nc, es):
        self.nc = nc
        self.es = es
        self.streams = {e: [] for e in ENGS}
        self.cnt = {e: 0 for e in ENGS}
        self.pending = {e: False for e in ENGS}
        self.esem = {e: es.enter_context(nc.semaphore("c_" + e)) for e in ENGS}
        self.waited = {e: {} for e in ENGS}
        self.nsem = len(ENGS)
        self.fence = {}
        self.dbufs = {}
        self.allbufs = []

    def sb(self, name, shape, dt, es=None):
        h = (es or self.es).enter_context(self.nc.sbuf_tensor(name, list(shape), dt))
        b = Buf(name, h)
        self.allbufs.append(b)
        b.reads = dict(self.fence)
        if es is not None:
            es.callback(self.release, [b])
        return b

    def release(self, bufs):
        for b in bufs:
            toks = list(b.reads.values()) + ([b.last_w] if b.last_w is not None else [])
            for t in toks:
                k = id(t.sem)
                if k not in self.fence or self.fence[k].val < t.val:
                    self.fence[k] = t

    def ps(self, name, shape, dt, es=None):
        h = (es or self.es).enter_context(self.nc.psum_tensor(name, list(shape), dt))
        b = Buf(name, h)
        self.allbufs.append(b)
        return b

    def dram(self, name, h):
        b = Buf(name, h)
        self.allbufs.append(b)
        return b

    def _dsem(self, b):
        if b.dsem is None:
            b.dsem = self.es.enter_context(self.nc.semaphore("d_" + b.name))
            self.nsem += 1
        return b.dsem

    def _deps(self, eng, reads, writes):
        deps = []
        for b in reads:
            if b.last_w is not None:
                deps.append(b.last_w)
        for b in writes:
            if b.last_w is not None:
                deps.append(b.last_w)
            deps.extend(b.reads.values())
        out = {}
        for t in deps:
            if t.sem is self.esem[eng] and eng == "tensor":
                continue
            k = id(t.sem)
            if self.waited[eng].get(k, 0) >= t.val:
                continue
            if k not in out or out[k].val < t.val:
                out[k] = t
        for k, t in out.items():
            self.waited[eng][k] = t.val
        return list(out.values())

    def _record(self, tok, reads, writes):
        for b in reads:
            b.reads[id(tok.sem)] = tok
        for b in writes:
            b.last_w = tok
            b.reads = {}

    def _push(self, eng, deps, act):
        self.streams[eng].append({"deps": [[t.sem, t.val, 0] for t in deps], "act": act})

    def op(self, eng, fn, reads=(), writes=(), sig=True):
        deps = self._deps(eng, reads, writes)
        sem = self.esem[eng]
        if sig:
            self.cnt[eng] += 1
            self.pending[eng] = False
            tok = Tok(sem, self.cnt[eng])
        else:
            self.pending[eng] = True
            tok = Tok(sem, self.cnt[eng] + 1)
        rec = _Rec()
        fn(rec)
        name, a, k = rec.call

        def act(e, i, name=name, a=a, k=k, sig=sig, sem=sem):
            ins = getattr(e, name)(*a, **k)
            if sig:
                ins.then_inc(sem, 1)

        self._push(eng, deps, act)
        self._record(tok, reads, writes)

    def dma(self, eng, out, in_, reads=(), writes=(), sembuf=None):
        deps = self._deps(eng, reads, writes)
        sb = sembuf or (writes[0] if writes else reads[0])
        sem = self._dsem(sb)
        sb.dcnt += 16
        tok = Tok(sem, sb.dcnt)
        self.dbufs[id(sb)] = sb

        def act(e, i, out=out, in_=in_, sem=sem):
            src = in_(i) if callable(in_) else in_
            e.dma_start(out=out, in_=src).then_inc(sem, 16)

        self._push(eng, deps, act)
        self._record(tok, reads, writes)
        return tok

    def wait_tok(self, eng, tok):
        self._push(eng, [tok], lambda e, i: None)

    def mark(self):
        return {"len": {e: len(self.streams[e]) for e in ENGS},
                "cnt": dict(self.cnt),
                "dcnt": {k: b.dcnt for k, b in self.dbufs.items()}}

    def make_loop(self, m_a, m_b, it_b, it_end):
        extra = it_end - it_b - 1
        for e in ENGS:
            a0, b0, b1 = m_a["len"][e], m_b["len"][e], len(self.streams[e])
            A, B = self.streams[e][a0:b0], self.streams[e][b0:b1]
            assert len(A) == len(B), (e, len(A), len(B))
            for x, y_ in zip(A, B):
                assert len(x["deps"]) == len(y_["deps"]), (e, x["deps"], y_["deps"])
                for dx, dy in zip(x["deps"], y_["deps"]):
                    assert dx[0] is dy[0], e
                    dy[2] = dy[1] - dx[1]
                    assert dy[2] >= 0
            self.streams[e][b0:b1] = [{"loop": (it_b, it_end), "body": B}]
        shift = {}
        for e in ENGS:
            d = self.cnt[e] - m_b["cnt"][e]
            assert d == m_b["cnt"][e] - m_a["cnt"][e], e
            shift[id(self.esem[e])] = (m_b["cnt"][e], d * extra)
            self.cnt[e] += d * extra
        for k, b in self.dbufs.items():
            d = b.dcnt - m_b["dcnt"].get(k, 0)
            assert d == m_b["dcnt"].get(k, 0) - m_a["dcnt"].get(k, 0), b.name
            if d:
                shift[id(b.dsem)] = (m_b["dcnt"].get(k, 0), d * extra)
                b.dcnt += d * extra
        seen = set()

        def bump(t):
            if id(t) in seen:
                return
            seen.add(id(t))
            sh = shift.get(id(t.sem))
            if sh and t.val > sh[0]:
                t.val += sh[1]

        for b in self.allbufs:
            if b.last_w is not None:
                bump(b.last_w)
            for t in b.reads.values():
                bump(t)
        for t in self.fence.values():
            bump(t)
        for e in ENGS:
            for k, v in list(self.waited[e].items()):
                sh = shift.get(k)
                if sh and v > sh[0]:
                    self.waited[e][k] = v + sh[1]

    def finish(self):
        nc = self.nc
        for e in ENGS:
            assert not self.pending[e], e

        def run(eng_name, e):
            def emit(item, bases, tmp, it0):
                for sem, val, dv in item["deps"]:
                    if bases is not None and dv:
                        e.reg_add(tmp, bases[dv], val - it0 * dv)
                        e.wait_ge(sem, tmp)
                    else:
                        e.wait_ge(sem, val)

            for item in self.streams[eng_name]:
                if "loop" in item:
                    it0, it1 = item["loop"]
                    dvs = sorted({d[2] for sub in item["body"] for d in sub["deps"] if d[2]})
                    tmp = e.alloc_register("wtmp")
                    bases = {dv: e.alloc_register("wb%d" % dv) for dv in dvs}
                    with e.Fori(it0, it1) as i:
                        for dv, r in bases.items():
                            e.reg_mul(r, i, dv)
                        for sub in item["body"]:
                            emit(sub, bases, tmp, it0)
                            sub["act"](e, i)
                else:
                    emit(item, None, None, 0)
                    item["act"](e, None)

        with nc.Block() as block:
            @block.sync
            def _(e):
                run("sync", e)

            @block.scalar
            def _(e):
                run("scalar", e)

            @block.gpsimd
            def _(e):
                run("gpsimd", e)

            @block.vector
            def _(e):
                run("vector", e)

            @block.tensor
            def _(e):
                run("tensor", e)


def build_program(dbg=None, n_experts=NE, stop_after=None, e_off=0, acc_in=False, final=True):
    nc = bass.Bass("TRN2", target_bir_lowering=False)

    def din(name, shape, dt=F32):
        return nc.dram_tensor(name, list(shape), dt, kind="ExternalInput").ap()

    x_own = din("x_own", [TOK, D])
    x_ctx = din("x_ctx", [TOK, D])
    pos_all = din("pos_all", [1, SEQ], I32)
    flags = din("flags", [128, 2])
    c_col = din("c_col", [128, 8])
    w_ada = din("w_ada", [D, 6 * D])
    b_ada = din("b_ada", [1, 6 * D])
    gmix_col = din("gmix_col", [128, 8])
    gffn_col = din("gffn_col", [128, 8])
    w_in = din("w_in", [D, 4000])
    w_kpe_sw = din("w_kpe_sw", [D, 96])
    qg_col = din("qg_col", [128, 2])
    kvg_col = din("kvg_col", [128, 1])
    w_uq = din("w_uq", [256, 768])
    w_uq_sw = din("w_uq_sw", [256, 768])
    w_uk = din("w_uk", [128, 512])
    w_uv = din("w_uv", [128, 512])
    w_up_attn = din("w_up_attn", [512, D])
    conv_col = din("conv_col", [128, 12])
    w_up_conv = din("w_up_conv", [512, D])
    w_o = din("w_o", [D, D])
    router_w = din("router_w", [D, NE])
    router_b = din("router_b", [1, NE])
    w_gu = din("w_gu", [NE, D, 2 * D])
    bgu_col = din("bgu_col", [128, NE * 16])
    w_down = din("w_down", [NE, D, D])
    b_down = din("b_down", [NE, D])
    gfin_b = din("gfin_b", [128, D])
    rope_c = din("rope_c", [128, 2])
    ident_in = din("ident", [128, 128])
    y = nc.dram_tensor("y", [TOK, D], F32, kind="ExternalOutput").ap()
    acc_prev = din("acc_prev", [TOK, D]) if acc_in else None
    x1_scr = nc.dram_tensor("x1_scr", [TOK, D], F32, kind="Internal").ap()
    g_scr = nc.dram_tensor("g_scr", [128, NE * NT], F32, kind="Internal").ap()
    dbg_aps = {}
    if dbg:
        for k, shp in dbg.items():
            dbg_aps[k] = nc.dram_tensor("dbg_" + k, list(shp), F32, kind="ExternalOutput").ap()

    es = ExitStack()
    with es:
        P = Prog(nc, es)
        SY, AC, GP, VE, PE = "sync", "scalar", "gpsimd", "vector", "tensor"
        x1d = P.dram("x1d", None)
        gsd = P.dram("gsd", None)
        dbgx = P.dram("dbgx", None)
        yd = P.dram("yd", None)

        psb = [P.ps("ps%d" % i, [128, 512], F32) for i in range(8)]
        ps_rr = [0]

        def next_ps():
            b = psb[ps_rr[0] % 8]
            ps_rr[0] += 1
            return b

        ident_f = P.sb("ident_f", [128, 128], F32)
        ident_b = P.sb("ident_b", [128, 128], BF16)
        ones_f = P.sb("ones_f", [128, 128], F32)
        ones_b = P.sb("ones_b", [128, 128], BF16)
        zero_c = P.sb("zero_c", [128, 1], F32)
        flg = P.sb("flg", [128, 2], F32)
        adacol = P.sb("adacol", [128, 32], F32)
        scl_m = P.sb("scl_m", [128, 8], F32)
        scl_f = P.sb("scl_f", [128, 8], F32)
        gm_b = P.sb("gm_b", [128, D], F32)
        gf_b = P.sb("gf_b", [128, D], F32)

        P.dma(SY, ident_f[:], ident_in[:, :], writes=[ident_f])
        P.dma(SY, flg[:], flags[:, :], writes=[flg])
        P.op(VE, lambda e: e.tensor_copy(out=ident_b[:], in_=ident_f[:]), [ident_f], [ident_b])
        P.op(VE, lambda e: e.memset(ones_f[:], 1.0), [], [ones_f])
        P.op(VE, lambda e: e.memset(ones_b[:], 1.0), [], [ones_b])
        P.op(VE, lambda e: e.memset(zero_c[:], 0.0), [], [zero_c])

        with ExitStack() as s0:
            ccol = P.sb("ccol", [128, 8], F32, s0)
            cact = P.sb("cact", [128, 8], F32, s0)
            gmc = P.sb("gmc", [128, 8], F32, s0)
            gfc = P.sb("gfc", [128, 8], F32, s0)
            ada_row = P.sb("ada_row", [1, 6 * D], F32, s0)
            bada = P.sb("bada", [1, 6 * D], F32, s0)
            wa = [P.sb("wa%d" % i, [128, 3072], F32, s0) for i in range(2)]
            P.dma(SY, ccol[:], c_col[:, :], writes=[ccol])
            P.dma(SY, gmc[:], gmix_col[:, :], writes=[gmc])
            P.dma(SY, gfc[:], gffn_col[:, :], writes=[gfc])
            P.dma(SY, bada[:], b_ada[:, :], writes=[bada])
            P.op(AC, lambda e: e.activation(out=cact[:], in_=ccol[:], func=AF.Silu), [ccol], [cact])
            it = 0
            for hh in range(2):
                banks = [next_ps() for _ in range(6)]
                for k in range(8):
                    w = wa[it % 2]
                    it += 1
                    P.dma(SY, w[:], w_ada[k * 128:(k + 1) * 128, hh * 3072:(hh + 1) * 3072], writes=[w])
                    for n in range(6):
                        P.op(PE, lambda e, b=banks[n], w=w, k=k, n=n: e.matmul(
                            b[0:1, :], lhsT=cact[:, k:k + 1], rhs=w[:, n * 512:(n + 1) * 512],
                            start=(k == 0), stop=(k == 7)),
                            [cact, w], [banks[n]], sig=True)
                for n in range(6):
                    c0 = hh * 3072 + n * 512
                    P.op(VE, lambda e, b=banks[n], c0=c0: e.tensor_tensor(
                        out=ada_row[0:1, c0:c0 + 512], in0=b[0:1, :], in1=bada[0:1, c0:c0 + 512], op=ALU.add),
                        [banks[n], bada], [ada_row])
            pcol = next_ps()
            segs = [0, 1, 3, 4]
            for si, sg in enumerate(segs):
                for j in range(8):
                    c0 = sg * D + j * 128
                    idx = si * 8 + j
                    P.op(PE, lambda e, c0=c0, idx=idx: e.matmul(
                        pcol[:, idx:idx + 1], lhsT=ada_row[0:1, c0:c0 + 128], rhs=ones_f[0:1, 0:1],
                        start=True, stop=True), [ada_row, ones_f], [pcol])
            P.op(VE, lambda e: e.tensor_copy(out=adacol[:], in_=pcol[:, 0:32]), [pcol], [adacol])
            P.op(VE, lambda e: e.scalar_tensor_tensor(out=scl_m[:], in0=adacol[:, 8:16], scalar=1.0, in1=gmc[:],
                                                      op0=ALU.add, op1=ALU.mult), [adacol, gmc], [scl_m])
            P.op(VE, lambda e: e.scalar_tensor_tensor(out=scl_f[:], in0=adacol[:, 24:32], scalar=1.0, in1=gfc[:],
                                                      op0=ALU.add, op1=ALU.mult), [adacol, gfc], [scl_f])
            for sg, dst in ((2, gm_b), (5, gf_b)):
                for n in range(2):
                    pb = next_ps()
                    c0 = sg * D + n * 512
                    P.op(PE, lambda e, pb=pb, c0=c0: e.matmul(
                        pb[:, :], lhsT=ones_f[0:1, 0:128], rhs=ada_row[0:1, c0:c0 + 512],
                        start=True, stop=True), [ada_row, ones_f], [pb])
                    P.op(VE, lambda e, pb=pb, dst=dst, n=n: e.tensor_copy(
                        out=dst[:, n * 512:(n + 1) * 512], in_=pb[:, :]), [pb], [dst])
            if dbg and "ada" in dbg:
                P.dma(SY, dbg_aps["ada"][0:1, :], ada_row[0:1, :], reads=[ada_row], sembuf=ada_row)
            if dbg and "gm_b" in dbg:
                P.dma(SY, dbg_aps["gm_b"][:, :], gm_b[:], reads=[gm_b], sembuf=gm_b)
            if dbg and "adacol" in dbg:
                P.dma(SY, dbg_aps["adacol"][:, :], adacol[:], reads=[adacol], sembuf=adacol)

        if stop_after == "0":
            P.finish()
            return nc

        def norm_T(xt, xn, ss, rs, scl, shcol_off, dst, dst_c0):
            P.op(VE, lambda e: e.memset(ss[:], 0.0), [], [ss])
            P.op(AC, lambda e: e.activation(out=xn[:], in_=xt[:], func=AF.Square, accum_out=ss[:]),
                 [xt, ss], [xn, ss])
            P.op(VE, lambda e: e.tensor_scalar(out=rs[:], in0=ss[:], scalar1=1.0 / D, scalar2=EPS,
                                               op0=ALU.mult, op1=ALU.add), [ss], [rs])
            P.op(AC, lambda e: e.sqrt(out=rs[:], in_=rs[:]), [rs], [rs]); P.op(VE, lambda e: e.reciprocal(out=rs[:], in_=rs[:]), [rs], [rs])
            P.op(AC, lambda e: e.activation(out=xn[:], in_=xt[:], func=AF.Identity, scale=rs[:, 0:1]),
                 [xt, rs], [xn])
            pt = next_ps()
            ptb = pt[:].bitcast(BF16)
            for j in range(8):
                P.op(PE, lambda e, j=j, ptb=ptb: e.transpose(
                    ptb[:, j * 128:(j + 1) * 128], xn[:, j * 128:(j + 1) * 128], ident_b[:]),
                    [xn, ident_b], [pt], sig=(j == 7))
            for j in range(8):
                P.op(VE, lambda e, j=j, ptb=ptb: e.tensor_scalar(
                    out=dst[:, j, dst_c0:dst_c0 + 128], in0=ptb[:, j * 128:(j + 1) * 128],
                    scalar1=scl[:, j:j + 1], scalar2=adacol[:, shcol_off + j:shcol_off + j + 1],
                    op0=ALU.mult, op1=ALU.add), [pt, scl, adacol], [dst])

        def rstd_bcast(dst, src_ps, inv_n):
            P.op(VE, lambda e: e.tensor_scalar(out=dst[:], in0=src_ps[:], scalar1=inv_n, scalar2=EPS,
                                               op0=ALU.mult, op1=ALU.add), [src_ps], [dst])
            P.op(AC, lambda e: e.sqrt(out=dst[:], in_=dst[:]), [dst], [dst]); P.op(VE, lambda e: e.reciprocal(out=dst[:], in_=dst[:]), [dst], [dst])

        attn_es = ExitStack()
        es.enter_context(attn_es)
        attnT = P.sb("attnT", [128, NH, TOK], BF16, attn_es)

        with ExitStack() as sa:
            KT = P.sb("KT", [128, NH, SEQ], BF16, sa)
            Vt = P.sb("Vt", [128, 32, NH, 65], BF16, sa)
            wkv = P.sb("wkv", [128, 8, 160], BF16, sa)
            wks = P.sb("wks", [128, 8, 96], BF16, sa)
            wq = P.sb("wq", [128, 8, 256], BF16, sa)
            wuq = P.sb("wuq", [128, 2, 768], BF16, sa)
            wuqs = P.sb("wuqs", [128, 2, 768], BF16, sa)
            wuk = P.sb("wuk", [128, 512], BF16, sa)
            wuv = P.sb("wuv", [128, 512], BF16, sa)
            qg = P.sb("qg", [128, 2], F32, sa)
            kvg = P.sb("kvg", [128, 1], F32, sa)
            rpc = P.sb("rpc", [128, 2], F32, sa)
            hTg = P.sb("hTg", [128, 8, 512], BF16, sa)
            xts = [P.sb("xta%d" % i, [128, D], F32, sa) for i in range(2)]
            xn = P.sb("xna", [128, D], BF16, sa)
            ss = P.sb("ssa", [128, 1], F32, sa)
            rs = P.sb("rsa", [128, 1], F32, sa)
            posi = P.sb("posi", [128, 512], I32, sa)
            ang = P.sb("ang", [128, 512], F32, sa)
            cosT = P.sb("cosT", [128, 512], F32, sa)
            sinT = P.sb("sinT", [128, 512], F32, sa)
            tmp = [P.sb("tmpa%d" % i, [128, 512], F32, sa) for i in range(4)]
            sqb = P.sb("sqb", [128, 2, 512], BF16, sa)
            kvn = P.sb("kvn", [128, 512], BF16, sa)
            qn = P.sb("qn", [128, 2, 512], BF16, sa)
            qT = P.sb("qT", [128, NH, 512], BF16, sa)
            PT = [P.sb("PT%d" % i, [128, 512], BF16, sa) for i in range(4)]
            rsum, rbc = tmp[0], tmp[1]

            w_in_k = w_in.rearrange("(k p) n -> p k n", p=128)
            P.dma(GP, wkv[:], w_in_k[:, :, 256:416], writes=[wkv])
            P.dma(GP, wks[:], w_kpe_sw.rearrange("(k p) n -> p k n", p=128), writes=[wks])
            P.dma(GP, wq[:], w_in_k[:, :, 0:256], writes=[wq])
            P.dma(GP, wuq[:], w_uq.rearrange("(k p) n -> p k n", p=128), writes=[wuq])
            P.dma(GP, wuqs[:], w_uq_sw.rearrange("(k p) n -> p k n", p=128), writes=[wuqs])
            P.dma(GP, wuk[:], w_uk[:, :], writes=[wuk])
            P.dma(GP, wuv[:], w_uv[:, :], writes=[wuv])
            P.dma(SY, qg[:], qg_col[:, :], writes=[qg])
            P.dma(SY, kvg[:], kvg_col[:, :], writes=[kvg])
            P.dma(SY, rpc[:], rope_c[:, :], writes=[rpc])
            P.op(GP, lambda e: e.memset(Vt[:], 1.0), [], [Vt])

            R = slice(64, 96)

            def range_reduce_sin(dst, src, shift):
                t0, t1 = tmp[0], tmp[1]
                if shift != 0.0:
                    P.op(VE, lambda e: e.tensor_scalar(out=t1[R, :], in0=src[R, :], scalar1=shift, scalar2=None,
                                                       op0=ALU.add), [src], [t1])
                    a = t1
                else:
                    a = src
                P.op(VE, lambda e: e.tensor_scalar(out=t0[R, :], in0=a[R, :], scalar1=1.0 / TWO_PI, scalar2=None,
                                                   op0=ALU.mult), [a], [t0])
                P.op(VE, lambda e: e.tensor_copy(out=posi[R, :], in_=t0[R, :]), [t0], [posi])
                P.op(VE, lambda e: e.tensor_copy(out=t0[R, :], in_=posi[R, :]), [posi], [t0])
                P.op(VE, lambda e: e.scalar_tensor_tensor(out=dst[R, :], in0=t0[R, :], scalar=-CW1, in1=a[R, :],
                                                          op0=ALU.mult, op1=ALU.add), [t0, a], [dst])
                P.op(VE, lambda e: e.scalar_tensor_tensor(out=dst[R, :], in0=t0[R, :], scalar=-CW2, in1=dst[R, :],
                                                          op0=ALU.mult, op1=ALU.add), [t0, dst], [dst])
                P.op(VE, lambda e: e.tensor_scalar(out=t0[R, :], in0=dst[R, :], scalar1=float(np.pi),
                                                   scalar2=-TWO_PI, op0=ALU.is_gt, op1=ALU.mult), [dst], [t0])
                P.op(VE, lambda e: e.tensor_tensor(out=dst[R, :], in0=dst[R, :], in1=t0[R, :], op=ALU.add),
                     [dst, t0], [dst])
                P.op(VE, lambda e: e.tensor_scalar(out=t0[R, :], in0=dst[R, :], scalar1=-float(np.pi),
                                                   scalar2=TWO_PI, op0=ALU.is_lt, op1=ALU.mult), [dst], [t0])
                P.op(VE, lambda e: e.tensor_tensor(out=dst[R, :], in0=dst[R, :], in1=t0[R, :], op=ALU.add),
                     [dst, t0], [dst])
                P.op(AC, lambda e: e.activation(out=dst[R, :], in_=dst[R, :], func=AF.Sin), [dst], [dst])

            xi = 0
            for kg in range(8):
                own = kg >= 4
                g = kg - 4
                src = x_own if own else x_ctx
                row0 = (kg % 4) * 512
                k0 = kg * 512
                for tt in range(4):
                    xt = xts[xi % 2]
                    xi += 1
                    r0 = row0 + tt * 128
                    P.dma(SY, xt[:], src[r0:r0 + 128, :], writes=[xt])
                    norm_T(xt, xn, ss, rs, scl_m, 0, hTg, tt * 128)
                pA, pB, pC = next_ps(), next_ps(), next_ps()
                for k in range(8):
                    P.op(PE, lambda e, k=k: e.matmul(pA[:, :], lhsT=wkv[:, k, 0:128], rhs=hTg[:, k, :],
                                                     start=(k == 0), stop=(k == 7)), [wkv, hTg], [pA], sig=(k == 7))
                for k in range(8):
                    P.op(PE, lambda e, k=k: e.matmul(pB[0:96, :], lhsT=wkv[:, k, 64:160], rhs=hTg[:, k, :],
                                                     start=(k == 0), stop=(k == 7)), [wkv, hTg], [pB], sig=(k == 7))
                for k in range(8):
                    P.op(PE, lambda e, k=k: e.matmul(pC[0:96, :], lhsT=wks[:, k, :], rhs=hTg[:, k, :],
                                                     start=(k == 0), stop=(k == 7)), [wks, hTg], [pC], sig=(k == 7))
                P.dma(SY, posi[R, :], pos_all[0:1, k0:k0 + 512].broadcast_to([32, 512]), writes=[posi])
                P.op(VE, lambda e: e.tensor_copy(out=ang[R, :], in_=posi[R, :]), [posi], [ang])
                P.op(VE, lambda e: e.tensor_scalar(out=ang[R, :], in0=ang[R, :], scalar1=rpc[R, 0:1], scalar2=None,
                                                   op0=ALU.mult), [ang, rpc], [ang])
                range_reduce_sin(sinT, ang, 0.0)
                range_reduce_sin(cosT, ang, float(np.pi / 2))
                P.op(VE, lambda e: e.tensor_scalar(out=sinT[R, :], in0=sinT[R, :], scalar1=rpc[R, 1:2], scalar2=None,
                                                   op0=ALU.mult), [sinT, rpc], [sinT])
                t2, t3 = tmp[2], tmp[3]
                P.op(VE, lambda e: e.tensor_tensor(out=t2[R, :], in0=pB[R, :], in1=cosT[R, :], op=ALU.mult),
                     [pB, cosT], [t2])
                P.op(VE, lambda e: e.tensor_tensor(out=t3[R, :], in0=pC[R, :], in1=sinT[R, :], op=ALU.mult),
                     [pC, sinT], [t3])
                P.op(VE, lambda e: e.tensor_tensor(out=t2[R, :], in0=t2[R, :], in1=t3[R, :], op=ALU.add),
                     [t2, t3], [t2])
                for h in range(NH):
                    eng = GP if h % 2 else VE
                    P.op(eng, lambda e, h=h: e.tensor_copy(out=KT[R, h, k0:k0 + 512], in_=t2[R, :]), [t2], [KT])
                P.op(AC, lambda e: e.activation(out=sqb[:, 0, :], in_=pA[:, :], func=AF.Square), [pA], [sqb])
                pD = next_ps()
                P.op(PE, lambda e: e.matmul(pD[:, :], lhsT=ones_b[:], rhs=sqb[:, 0, :], start=True, stop=True),
                     [ones_b, sqb], [pD])
                rstd_bcast(tmp[0], pD, 1.0 / 128)
                P.op(VE, lambda e: e.scalar_tensor_tensor(out=kvn[:], in0=pA[:, :], scalar=kvg[:, 0:1], in1=tmp[0][:],
                                                          op0=ALU.mult, op1=ALU.mult), [pA, kvg, tmp[0]], [kvn])
                for h in range(NH):
                    pk = next_ps()
                    P.op(PE, lambda e, h=h, pk=pk: e.matmul(pk[0:64, :], lhsT=wuk[:, h * 64:(h + 1) * 64], rhs=kvn[:],
                                                            start=True, stop=True), [wuk, kvn], [pk])
                    if h % 2:
                        P.op(AC, lambda e, h=h, pk=pk: e.copy(out=KT[0:64, h, k0:k0 + 512], in_=pk[0:64, :]),
                             [pk], [KT])
                    else:
                        P.op(VE, lambda e, h=h, pk=pk: e.tensor_copy(out=KT[0:64, h, k0:k0 + 512], in_=pk[0:64, :]),
                             [pk], [KT])
                for tt in range(4):
                    pv = next_ps()
                    kt = kg * 4 + tt
                    P.op(PE, lambda e, tt=tt, pv=pv: e.matmul(pv[:, :], lhsT=kvn[:, tt * 128:(tt + 1) * 128], rhs=wuv[:],
                                                              start=True, stop=True), [kvn, wuv], [pv])
                    if tt % 2:
                        P.op(AC, lambda e, kt=kt, pv=pv: e.copy(
                            out=Vt[:, kt, :, 0:64], in_=pv[:, :].rearrange("p (h d) -> p h d", h=NH)), [pv], [Vt])
                    else:
                        P.op(VE, lambda e, kt=kt, pv=pv: e.tensor_copy(
                            out=Vt[:, kt, :, 0:64], in_=pv[:, :].rearrange("p (h d) -> p h d", h=NH)), [pv], [Vt])
                if not own:
                    continue
                pE = [next_ps(), next_ps()]
                for c in range(2):
                    for k in range(8):
                        P.op(PE, lambda e, c=c, k=k: e.matmul(pE[c][:, :], lhsT=wq[:, k, c * 128:(c + 1) * 128],
                                                              rhs=hTg[:, k, :], start=(k == 0), stop=(k == 7)),
                             [wq, hTg], [pE[c]], sig=(k == 7))
                    P.op(AC, lambda e, c=c: e.activation(out=sqb[:, c, :], in_=pE[c][:, :], func=AF.Square),
                         [pE[c]], [sqb])
                pD = next_ps()
                for c in range(2):
                    P.op(PE, lambda e, c=c: e.matmul(pD[:, :], lhsT=ones_b[:], rhs=sqb[:, c, :],
                                                     start=(c == 0), stop=(c == 1)), [ones_b, sqb], [pD], sig=(c == 1))
                rstd_bcast(tmp[0], pD, 1.0 / 256)
                for c in range(2):
                    P.op(VE, lambda e, c=c: e.scalar_tensor_tensor(
                        out=qn[:, c, :], in0=pE[c][:, :], scalar=qg[:, c:c + 1], in1=tmp[0][:],
                        op0=ALU.mult, op1=ALU.mult), [pE[c], qg, tmp[0]], [qn])
                for h in range(NH):
                    pF, pG = next_ps(), next_ps()
                    for c in range(2):
                        P.op(PE, lambda e, c=c, h=h, pF=pF: e.matmul(
                            pF[0:96, :], lhsT=wuq[:, c, h * 96:(h + 1) * 96], rhs=qn[:, c, :],
                            start=(c == 0), stop=(c == 1)), [wuq, qn], [pF], sig=(c == 1))
                    for c in range(2):
                        P.op(PE, lambda e, c=c, h=h, pG=pG: e.matmul(
                            pG[0:96, :], lhsT=wuqs[:, c, h * 96:(h + 1) * 96], rhs=qn[:, c, :],
                            start=(c == 0), stop=(c == 1)), [wuqs, qn], [pG], sig=(c == 1))
                    P.op(AC, lambda e, h=h, pF=pF: e.copy(out=qT[0:64, h, :], in_=pF[0:64, :]), [pF], [qT])
                    P.op(VE, lambda e, pF=pF: e.tensor_tensor(out=t2[R, :], in0=pF[R, :], in1=cosT[R, :], op=ALU.mult),
                         [pF, cosT], [t2])
                    P.op(VE, lambda e, pG=pG: e.tensor_tensor(out=t3[R, :], in0=pG[R, :], in1=sinT[R, :], op=ALU.mult),
                         [pG, sinT], [t3])
                    P.op(VE, lambda e, h=h: e.tensor_tensor(out=qT[R, h, :], in0=t2[R, :], in1=t3[R, :], op=ALU.add),
                         [t2, t3], [qT])
                pti = 0
                for h in range(NH):
                    pO = next_ps()
                    nkt = (kg + 1) * 4
                    for kt in range(nkt):
                        j = kt - kg * 4
                        qoff = max(j, 0) * 128
                        n = 512 - qoff
                        pS = next_ps()
                        if pS is pO:
                            pS = next_ps()
                        P.op(PE, lambda e, h=h, kt=kt, qoff=qoff, n=n, pS=pS: e.matmul(
                            pS[:, 0:n], lhsT=KT[0:96, h, kt * 128:(kt + 1) * 128], rhs=qT[0:96, h, qoff:512],
                            start=True, stop=True), [KT, qT], [pS])
                        pt = PT[pti % 4]
                        pti += 1
                        bias = flg[:, 0:1] if kt < 16 else zero_c[:, 0:1]
                        P.op(AC, lambda e, pt=pt, pS=pS, n=n, bias=bias: e.activation(
                            out=pt[:, 0:n], in_=pS[:, 0:n], func=AF.Exp, bias=bias, scale=SM_SCALE),
                            [pS, flg, zero_c], [pt])
                        if j >= 0:
                            P.op(GP, lambda e, pt=pt: e.memset(pt[64:128, 0:64], 0.0), [], [pt])
                        P.op(PE, lambda e, h=h, kt=kt, qoff=qoff, n=n, pt=pt, pO=pO: e.matmul(
                            pO[0:65, qoff:512], lhsT=Vt[:, kt, h, :], rhs=pt[:, 0:n],
                            start=(kt == 0), stop=(kt == nkt - 1)), [Vt, pt], [pO], sig=(kt == nkt - 1))
                    P.op(VE, lambda e, pO=pO: e.reciprocal(out=rsum[64:65, :], in_=pO[64:65, :]), [pO], [rsum])
                    pN = next_ps()
                    if pN is pO:
                        pN = next_ps()
                    P.op(PE, lambda e, pN=pN: e.matmul(pN[0:64, :], lhsT=ones_f[64:65, 0:64], rhs=rsum[64:65, :],
                                                       start=True, stop=True), [ones_f, rsum], [pN])
                    P.op(AC, lambda e, pN=pN: e.copy(out=rbc[0:64, :], in_=pN[0:64, :]), [pN], [rbc])
                    P.op(VE, lambda e, h=h, pO=pO, g=g: e.tensor_tensor(
                        out=attnT[0:64, h, g * 512:(g + 1) * 512], in0=pO[0:64, :], in1=rbc[0:64, :], op=ALU.mult),
                        [pO, rbc], [attnT])
            if dbg and "attnT" in dbg:
                for h in range(NH):
                    P.op(VE, lambda e: e.tensor_copy(out=tmp[0][0:64, :], in_=attnT[0:64, h, 0:512]), [attnT], [tmp[0]])
                    P.dma(SY, dbg_aps["attnT"][h * 64:(h + 1) * 64, :], tmp[0][0:64, :], reads=[tmp[0]], sembuf=tmp[0])

        if stop_after == "A":
            P.finish()
            return nc

        with ExitStack() as sbk:
            win = P.sb("win", [128, 8, 3584], BF16, sbk)
            wua = P.sb("wua", [64, NH, D], BF16, sbk)
            wuc = P.sb("wuc", [128, 4, D], BF16, sbk)
            wo = P.sb("wo", [128, 8, D], BF16, sbk)
            cvc = P.sb("cvc", [128, 12], F32, sbk)
            xres = P.sb("xres", [128, 4, D], F32, sbk)
            xn = P.sb("xnb", [128, D], BF16, sbk)
            ss = P.sb("ssb", [128, 1], F32, sbk)
            rs = P.sb("rsb", [128, 1], F32, sbk)
            hTg = P.sb("hTgb", [128, 8, 512], BF16, sbk)
            cu = P.sb("cu", [128, 4, 514], F32, sbk)
            bzT = P.sb("bzT", [128, 4, 512], BF16, sbk)
            mT = P.sb("mT", [128, 8, 512], BF16, sbk)
            tb = [P.sb("tmpb%d" % i, [128, 512], F32, sbk) for i in range(6)]
            x1t = [P.sb("x1t%d" % i, [128, D], F32, sbk) for i in range(2)]
            w_in_k = w_in.rearrange("(k p) n -> p k n", p=128)
            for k in range(8):
                P.dma(GP, win[:, k, :], w_in_k[:, k, 416:4000], writes=[win])
            P.dma(GP, wua[:], w_up_attn.rearrange("(h p) n -> p h n", p=64), writes=[wua])
            P.dma(GP, wuc[:], w_up_conv.rearrange("(k p) n -> p k n", p=128), writes=[wuc])
            P.dma(GP, wo[:], w_o.rearrange("(k p) n -> p k n", p=128), writes=[wo])
            P.dma(SY, cvc[:], conv_col[:, :], writes=[cvc])

            def ucb(colbase, ch, rhs_ap, nfree, reads):
                pz = next_ps()
                for k in range(8):
                    P.op(PE, lambda e, k=k, pz=pz: e.matmul(
                        pz[:, 0:nfree], lhsT=win[:, k, colbase + ch * 128:colbase + (ch + 1) * 128],
                        rhs=rhs_ap(k), start=(k == 0), stop=(k == 7)), [win] + reads, [pz], sig=(k == 7))
                return pz

            P.dma(SY, xres[:, 0, :], x_ctx[TOK - 128:TOK, :], writes=[xres])
            def norm_T_res(slot, dst_c0):
                xt = xres
                P.op(VE, lambda e: e.memset(ss[:], 0.0), [], [ss])
                P.op(AC, lambda e: e.activation(out=xn[:], in_=xt[:, slot, :], func=AF.Square, accum_out=ss[:]),
                     [xt, ss], [xn, ss])
                P.op(VE, lambda e: e.tensor_scalar(out=rs[:], in0=ss[:], scalar1=1.0 / D, scalar2=EPS,
                                                   op0=ALU.mult, op1=ALU.add), [ss], [rs])
                P.op(AC, lambda e: e.sqrt(out=rs[:], in_=rs[:]), [rs], [rs]); P.op(VE, lambda e: e.reciprocal(out=rs[:], in_=rs[:]), [rs], [rs])
                P.op(AC, lambda e: e.activation(out=xn[:], in_=xt[:, slot, :], func=AF.Identity, scale=rs[:, 0:1]),
                     [xt, rs], [xn])
                pt = next_ps()
                ptb = pt[:].bitcast(BF16)
                for j in range(8):
                    P.op(PE, lambda e, j=j, ptb=ptb: e.transpose(
                        ptb[:, j * 128:(j + 1) * 128], xn[:, j * 128:(j + 1) * 128], ident_b[:]),
                        [xn, ident_b], [pt], sig=(j == 7))
                for j in range(8):
                    P.op(VE, lambda e, j=j, ptb=ptb: e.tensor_scalar(
                        out=hTg[:, j, dst_c0:dst_c0 + 128], in0=ptb[:, j * 128:(j + 1) * 128],
                        scalar1=scl_m[:, j:j + 1], scalar2=adacol[:, j:j + 1],
                        op0=ALU.mult, op1=ALU.add), [pt, scl_m, adacol], [hTg])

            norm_T_res(0, 0)
            for ch in range(4):
                pu = ucb(0, ch, lambda k: hTg[:, k, 0:128], 128, [hTg])
                P.op(AC, lambda e, pu=pu: e.copy(out=tb[0][:, 0:128], in_=pu[:, 0:128]), [pu], [tb[0]])
                pc = ucb(512, ch, lambda k: hTg[:, k, 0:128], 128, [hTg])
                P.op(VE, lambda e, pc=pc: e.tensor_tensor(out=tb[1][:, 0:128], in0=pc[:, 0:128], in1=tb[0][:, 0:128],
                                                          op=ALU.mult), [pc, tb[0]], [tb[1]])
                P.op(VE, lambda e, ch=ch: e.tensor_scalar(out=cu[:, ch, 512:514], in0=tb[1][:, 126:128],
                                                          scalar1=flg[:, 1:2], scalar2=None, op0=ALU.mult),
                     [tb[1], flg], [cu])

            xo = 0
            for g in range(NG):
                t0 = g * 512
                for tt in range(4):
                    P.dma(SY, xres[:, tt, :], x_own[t0 + tt * 128:t0 + (tt + 1) * 128, :], writes=[xres])
                for tt in range(4):
                    norm_T_res(tt, tt * 128)
                for ch in range(4):
                    P.op(VE, lambda e, ch=ch: e.tensor_copy(out=cu[:, ch, 0:2], in_=cu[:, ch, 512:514]), [cu], [cu])
                    pu = ucb(0, ch, lambda k: hTg[:, k, :], 512, [hTg])
                    P.op(AC, lambda e, pu=pu: e.copy(out=tb[0][:], in_=pu[:, :]), [pu], [tb[0]])
                    pc = ucb(512, ch, lambda k: hTg[:, k, :], 512, [hTg])
                    P.op(VE, lambda e, pc=pc, ch=ch: e.tensor_tensor(out=cu[:, ch, 2:514], in0=pc[:, :], in1=tb[0][:],
                                                                     op=ALU.mult), [pc, tb[0]], [cu])
                    P.op(GP, lambda e, ch=ch: e.tensor_scalar(out=tb[1][:], in0=cu[:, ch, 2:514],
                                                              scalar1=cvc[:, ch * 3 + 2:ch * 3 + 3], scalar2=None,
                                                              op0=ALU.mult), [cu, cvc], [tb[1]])
                    P.op(VE, lambda e, ch=ch: e.scalar_tensor_tensor(out=tb[1][:], in0=cu[:, ch, 1:513],
                                                                     scalar=cvc[:, ch * 3 + 1:ch * 3 + 2], in1=tb[1][:],
                                                                     op0=ALU.mult, op1=ALU.add), [cu, cvc, tb[1]], [tb[1]])
                    P.op(VE, lambda e, ch=ch: e.scalar_tensor_tensor(out=tb[1][:], in0=cu[:, ch, 0:512],
                                                                     scalar=cvc[:, ch * 3:ch * 3 + 1], in1=tb[1][:],
                                                                     op0=ALU.mult, op1=ALU.add), [cu, cvc, tb[1]], [tb[1]])
                    pb = ucb(1024, ch, lambda k: hTg[:, k, :], 512, [hTg])
                    P.op(VE, lambda e, pb=pb, ch=ch: e.tensor_tensor(out=bzT[:, ch, :], in0=pb[:, :], in1=tb[1][:],
                                                                     op=ALU.mult), [pb, tb[1]], [bzT])
                for oc in range(8):
                    pga = ucb(1536, oc, lambda k: hTg[:, k, :], 512, [hTg])
                    P.op(AC, lambda e, pga=pga: e.activation(out=tb[2][:], in_=pga[:, :], func=AF.Sigmoid),
                         [pga], [tb[2]])
                    pgc = ucb(2560, oc, lambda k: hTg[:, k, :], 512, [hTg])
                    P.op(AC, lambda e, pgc=pgc: e.activation(out=tb[3][:], in_=pgc[:, :], func=AF.Sigmoid),
                         [pgc], [tb[3]])
                    pab = next_ps()
                    for h in range(NH):
                        P.op(PE, lambda e, h=h, oc=oc, pab=pab, t0=t0: e.matmul(
                            pab[:, :], lhsT=wua[0:64, h, oc * 128:(oc + 1) * 128], rhs=attnT[0:64, h, t0:t0 + 512],
                            start=(h == 0), stop=(h == NH - 1)), [wua, attnT], [pab], sig=(h == NH - 1))
                    pcb = next_ps()
                    for ch in range(4):
                        P.op(PE, lambda e, ch=ch, oc=oc, pcb=pcb: e.matmul(
                            pcb[:, :], lhsT=wuc[:, ch, oc * 128:(oc + 1) * 128], rhs=bzT[:, ch, :],
                            start=(ch == 0), stop=(ch == 3)), [wuc, bzT], [pcb], sig=(ch == 3))
                    P.op(VE, lambda e, pab=pab: e.tensor_tensor(out=tb[4][:], in0=pab[:, :], in1=tb[2][:], op=ALU.mult),
                         [pab, tb[2]], [tb[4]])
                    P.op(VE, lambda e, pcb=pcb: e.tensor_tensor(out=tb[5][:], in0=pcb[:, :], in1=tb[3][:], op=ALU.mult),
                         [pcb, tb[3]], [tb[5]])
                    P.op(GP, lambda e, oc=oc: e.tensor_tensor(out=mT[:, oc, :], in0=tb[4][:], in1=tb[5][:], op=ALU.add),
                         [tb[4], tb[5]], [mT])
                for tt in range(4):
                    xo_t = x1t[xo % 2]
                    xo += 1
                    for n in range(2):
                        pm = next_ps()
                        for oc in range(8):
                            P.op(PE, lambda e, oc=oc, tt=tt, n=n, pm=pm: e.matmul(
                                pm[:, :], lhsT=mT[:, oc, tt * 128:(tt + 1) * 128], rhs=wo[:, oc, n * 512:(n + 1) * 512],
                                start=(oc == 0), stop=(oc == 7)), [mT, wo], [pm], sig=(oc == 7))
                        P.op(VE, lambda e, pm=pm, n=n, xo_t=xo_t: e.tensor_tensor(
                            out=xo_t[:, n * 512:(n + 1) * 512], in0=pm[:, :], in1=gm_b[:, n * 512:(n + 1) * 512],
                            op=ALU.mult), [pm, gm_b], [xo_t])
                    P.op(GP, lambda e, tt=tt, xo_t=xo_t: e.tensor_tensor(out=xo_t[:], in0=xo_t[:], in1=xres[:, tt, :],
                                                                         op=ALU.add), [xo_t, xres], [xo_t])
                    r0 = t0 + tt * 128
                    P.dma(SY, x1_scr[r0:r0 + 128, :], xo_t[:], reads=[xo_t], writes=[x1d], sembuf=xo_t)
                    if dbg and "x1" in dbg:
                        P.dma(SY, dbg_aps["x1"][r0:r0 + 128, :], xo_t[:], reads=[xo_t], writes=[dbgx], sembuf=dbgx)
        attn_es.close()
        if stop_after == "B":
            P.finish()
            return nc

        with ExitStack() as sc:
            acc = P.sb("acc", [128, NT, D], F32, sc)
            h2T = P.sb("h2T", [128, 8, TOK], BF16, sc)
            gates = P.sb("gates", [128, NT, NE], F32, sc)
            gT = P.sb("gT", [32, 128], F32, sc)
            gatesE = P.sb("gatesE", [128, NE, NT], F32, sc)
            bge = P.sb("bge", [128, 16], F32, sc)
            gce = P.sb("gce", [128, NT], F32, sc)
            rw = P.sb("rw", [128, 8, NE], BF16, sc)
            rb = P.sb("rb", [1, NE], BF16, sc)
            bd = P.sb("bd", [32, D], F32, sc)
            gfin = P.sb("gfin", [128, D], F32, sc)
            xn = P.sb("xnc", [128, D], BF16, sc)
            ss = P.sb("ssc", [128, 1], F32, sc)
            rs = P.sb("rsc", [128, 1], F32, sc)
            sm = [P.sb("smc%d" % i, [128, 32], F32, sc) for i in range(4)]
            m8 = P.sb("m8", [128, 8], F32, sc)
            wg = [P.sb("wg%d" % i, [128, 8, 512], BF16, sc) for i in range(2)]
            wu = [P.sb("wu%d" % i, [128, 8, 512], BF16, sc) for i in range(2)]
            wd = [P.sb("wd%d" % i, [128, 4, D], BF16, sc) for i in range(2)]
            wst = [P.sb("wst%d" % i, [128, D], F32, sc) for i in range(2)]
            actT = [P.sb("actT%d" % i, [128, 4, 512], BF16, sc) for i in range(2)]
            tcs = [[P.sb("tc%d_%d" % (i, j), [128, 512], F32, sc) for j in range(5)] for i in range(2)]
            ot = wst

            P.dma(GP, rw[:], router_w.rearrange("(k p) n -> p k n", p=128), writes=[rw])
            P.dma(GP, rb[:], router_b[:, :], writes=[rb])
            P.dma(SY, bd[:], b_down[:, :], writes=[bd])
            P.dma(SY, gfin[:], gfin_b[:, :], writes=[gfin])
            P.op(VE, lambda e: e.tensor_tensor(out=bd[:], in0=bd[:], in1=gf_b[0:32, :], op=ALU.mult), [bd, gf_b], [bd])

            for t in range(NT):
                r0 = t * 128
                P.dma(SY, acc[:, t, :], x1_scr[r0:r0 + 128, :], reads=[x1d], writes=[acc])
                P.op(VE, lambda e: e.memset(ss[:], 0.0), [], [ss])
                P.op(AC, lambda e, t=t: e.activation(out=xn[:], in_=acc[:, t, :], func=AF.Square, accum_out=ss[:]),
                     [acc, ss], [xn, ss])
                P.op(VE, lambda e: e.tensor_scalar(out=rs[:], in0=ss[:], scalar1=1.0 / D, scalar2=EPS,
                                                   op0=ALU.mult, op1=ALU.add), [ss], [rs])
                P.op(AC, lambda e: e.sqrt(out=rs[:], in_=rs[:]), [rs], [rs]); P.op(VE, lambda e: e.reciprocal(out=rs[:], in_=rs[:]), [rs], [rs])
                P.op(AC, lambda e, t=t: e.activation(out=xn[:], in_=acc[:, t, :], func=AF.Identity, scale=rs[:, 0:1]),
                     [acc, rs], [xn])
                pt = next_ps()
                ptb = pt[:].bitcast(BF16)
                for j in range(8):
                    P.op(PE, lambda e, j=j, ptb=ptb: e.transpose(
                        ptb[:, j * 128:(j + 1) * 128], xn[:, j * 128:(j + 1) * 128], ident_b[:]),
                        [xn, ident_b], [pt], sig=(j == 7))
                for j in range(8):
                    P.op(VE, lambda e, j=j, ptb=ptb, r0=r0: e.tensor_scalar(
                        out=h2T[:, j, r0:r0 + 128], in0=ptb[:, j * 128:(j + 1) * 128],
                        scalar1=scl_f[:, j:j + 1], scalar2=adacol[:, 16 + j:17 + j],
                        op0=ALU.mult, op1=ALU.add), [pt, scl_f, adacol], [h2T])
                pl = next_ps()
                for k in range(8):
                    P.op(PE, lambda e, k=k, r0=r0, pl=pl: e.matmul(
                        pl[:, 0:NE], lhsT=h2T[:, k, r0:r0 + 128], rhs=rw[:, k, :], start=(k == 0), stop=False),
                        [h2T, rw], [pl], sig=False)
                P.op(PE, lambda e, pl=pl: e.matmul(pl[:, 0:NE], lhsT=ones_b[0:1, 0:128], rhs=rb[0:1, :],
                                                   start=False, stop=True), [ones_b, rb], [pl])
                lg, ex, mk, _ = sm
                P.op(VE, lambda e, pl=pl: e.tensor_copy(out=lg[:], in_=pl[:, 0:NE]), [pl], [lg])
                P.op(VE, lambda e: e.max(out=m8[:], in_=lg[:]), [lg], [m8])
                P.op(VE, lambda e: e.tensor_scalar(out=mk[:], in0=lg[:], scalar1=m8[:, 3:4], scalar2=None,
                                                   op0=ALU.is_ge), [lg, m8], [mk])
                P.op(VE, lambda e: e.tensor_scalar(out=lg[:], in0=lg[:], scalar1=m8[:, 0:1], scalar2=None,
                                                   op0=ALU.subtract), [lg, m8], [lg])
                P.op(AC, lambda e: e.activation(out=ex[:], in_=lg[:], func=AF.Exp), [lg], [ex])
                P.op(VE, lambda e: e.tensor_tensor(out=ex[:], in0=ex[:], in1=mk[:], op=ALU.mult), [ex, mk], [ex])
                P.op(VE, lambda e: e.reduce_sum(out=ss[:], in_=ex[:], axis=mybir.AxisListType.X), [ex], [ss])
                P.op(VE, lambda e: e.reciprocal(out=rs[:], in_=ss[:]), [ss], [rs])
                P.op(VE, lambda e, t=t: e.tensor_scalar(out=gates[:, t, :], in0=ex[:], scalar1=rs[:, 0:1], scalar2=None,
                                                        op0=ALU.mult), [ex, rs], [gates])
                P.op(GP, lambda e, t=t: e.tensor_copy(out=gatesE[:, :, t], in_=gates[:, t, :]), [gates], [gatesE])
            if dbg and "gates" in dbg:
                P.dma(SY, dbg_aps["gates"][:, :], gates[:].rearrange("p t e -> p (t e)"), reads=[gates], sembuf=gates)

            if acc_in:
                for t in range(NT):
                    P.dma(SY, acc[:, t, :], acc_prev[t * 128:(t + 1) * 128, :], writes=[acc])
            P.dma(SY, g_scr[:, :], gatesE[:].rearrange("p e t -> p (e t)"), reads=[gatesE], writes=[gsd], sembuf=gatesE)
            w_gu_k = w_gu.rearrange("e (k p) n -> e p k n", p=128)
            w_dn_k = w_down.rearrange("e (k p) n -> e p k n", p=128)
            cnts = {"it": 0, "st": 0, "gi": 0}

            def expert_iter(ex_s):
                def esel(i):
                    return ex_s if i is None else i

                P.dma(SY, bge[:], lambda i: bgu_col[:, bass.ds(esel(i) * 16, 16)], writes=[bge])
                P.dma(SY, gce[:], lambda i: g_scr[:, bass.ds(esel(i) * NT, NT)], reads=[gsd], writes=[gce])
                for hf in range(2):
                    b = cnts["it"] % 2
                    cnts["it"] += 1
                    P.dma(GP, wg[b][:], lambda i, hf=hf: w_gu_k[bass.ds(esel(i), 1), :, :, hf * 512:(hf + 1) * 512]
                          .rearrange("o p k n -> p (o k) n"), writes=[wg[b]])
                    P.dma(GP, wu[b][:], lambda i, hf=hf: w_gu_k[bass.ds(esel(i), 1), :, :, D + hf * 512:D + (hf + 1) * 512]
                          .rearrange("o p k n -> p (o k) n"), writes=[wu[b]])
                    for fc in range(4):
                        st = wst[cnts["st"] % 2]
                        cnts["st"] += 1
                        P.dma(SY, st[:], lambda i, hf=hf, fc=fc: w_dn_k[bass.ds(esel(i), 1), :, hf * 4 + fc, :]
                              .rearrange("o p n -> p (o n)"), writes=[st])
                        P.op(GP, lambda e, st=st, b=b, fc=fc: e.tensor_tensor(out=wd[b][:, fc, :], in0=st[:], in1=gf_b[:],
                                                                              op=ALU.mult), [st, gf_b], [wd[b]])
                    for g in range(NG):
                        t0 = g * 512
                        aT = actT[cnts["gi"] % 2]
                        tc = tcs[cnts["gi"] % 2]
                        cnts["gi"] += 1
                        for fc in range(4):
                            cg = hf * 4 + fc
                            cuu = 8 + hf * 4 + fc
                            pG, pU = next_ps(), next_ps()
                            for k in range(8):
                                P.op(PE, lambda e, k=k, fc=fc, b=b, pG=pG, t0=t0: e.matmul(
                                    pG[:, :], lhsT=wg[b][:, k, fc * 128:(fc + 1) * 128], rhs=h2T[:, k, t0:t0 + 512],
                                    start=(k == 0), stop=(k == 7)), [wg[b], h2T], [pG], sig=(k == 7))
                            for k in range(8):
                                P.op(PE, lambda e, k=k, fc=fc, b=b, pU=pU, t0=t0: e.matmul(
                                    pU[:, :], lhsT=wu[b][:, k, fc * 128:(fc + 1) * 128], rhs=h2T[:, k, t0:t0 + 512],
                                    start=(k == 0), stop=(k == 7)), [wu[b], h2T], [pU], sig=(k == 7))
                            gcl, sg_, ub, uc, gs = tc
                            P.op(VE, lambda e, pG=pG, cg=cg, gcl=gcl: e.tensor_scalar(
                                out=gcl[:], in0=pG[:, :], scalar1=bge[:, cg:cg + 1], scalar2=7.0,
                                op0=ALU.add, op1=ALU.min), [pG, bge], [gcl])
                            P.op(AC, lambda e, gcl=gcl, sg_=sg_: e.activation(out=sg_[:], in_=gcl[:], func=AF.Sigmoid,
                                                                              scale=1.702), [gcl], [sg_])
                            P.op(AC, lambda e, pU=pU, cuu=cuu, ub=ub: e.activation(
                                out=ub[:], in_=pU[:, :], func=AF.Identity, bias=bge[:, cuu:cuu + 1]), [pU, bge], [ub])
                            P.op(GP, lambda e, ub=ub, uc=uc: e.tensor_scalar(out=uc[:], in0=ub[:], scalar1=7.0, scalar2=-7.0,
                                                                             op0=ALU.min, op1=ALU.max), [ub], [uc])
                            P.op(GP, lambda e, gcl=gcl, sg_=sg_, gs=gs: e.tensor_tensor(out=gs[:], in0=gcl[:], in1=sg_[:],
                                                                                        op=ALU.mult), [gcl, sg_], [gs])
                            P.op(VE, lambda e, uc=uc, gs=gs, aT=aT, fc=fc: e.scalar_tensor_tensor(
                                out=aT[:, fc, :], in0=uc[:], scalar=1.0, in1=gs[:], op0=ALU.add, op1=ALU.mult),
                                [uc, gs], [aT])
                        for tt in range(4):
                            t = g * 4 + tt
                            for n in range(2):
                                pY = next_ps()
                                for fc in range(4):
                                    P.op(PE, lambda e, fc=fc, tt=tt, n=n, b=b, pY=pY, aT=aT: e.matmul(
                                        pY[:, :], lhsT=aT[:, fc, tt * 128:(tt + 1) * 128],
                                        rhs=wd[b][:, fc, n * 512:(n + 1) * 512],
                                        start=(fc == 0), stop=(fc == 3)), [aT, wd[b]], [pY], sig=(fc == 3))
                                P.op(VE, lambda e, pY=pY, t=t, n=n: e.scalar_tensor_tensor(
                                    out=acc[:, t, n * 512:(n + 1) * 512], in0=pY[:, :],
                                    scalar=gce[:, t:t + 1], in1=acc[:, t, n * 512:(n + 1) * 512],
                                    op0=ALU.mult, op1=ALU.add), [pY, gce, acc], [acc])

            e_end = e_off + n_experts
            if n_experts <= 4:
                for ex_i in range(e_off, e_end):
                    expert_iter(ex_i)
            else:
                expert_iter(e_off)
                expert_iter(e_off + 1)
                m_a = P.mark()
                expert_iter(e_off + 2)
                m_b = P.mark()
                expert_iter(e_off + 3)
                P.make_loop(m_a, m_b, e_off + 3, e_end)

            if final:
                last = None
                for t in range(NT):
                    r0 = t * 128
                    pg = next_ps()
                    P.op(PE, lambda e, t=t, pg=pg: e.transpose(pg[0:32, 0:128], gates[:, t, :], ident_f[:]),
                         [gates, ident_f], [pg])
                    P.op(VE, lambda e, pg=pg: e.tensor_copy(out=gT[:], in_=pg[0:32, 0:128]), [pg], [gT])
                    for n in range(2):
                        pbd = next_ps()
                        P.op(PE, lambda e, n=n, pbd=pbd: e.matmul(
                            pbd[:, :], lhsT=gT[0:32, :], rhs=bd[0:32, n * 512:(n + 1) * 512],
                            start=True, stop=True), [gT, bd], [pbd])
                        P.op(VE, lambda e, t=t, n=n, pbd=pbd: e.tensor_tensor(
                            out=acc[:, t, n * 512:(n + 1) * 512], in0=pbd[:, :], in1=acc[:, t, n * 512:(n + 1) * 512],
                            op=ALU.add), [pbd, acc], [acc])
                    P.op(VE, lambda e: e.memset(ss[:], 0.0), [], [ss])
                    P.op(AC, lambda e, t=t: e.activation(out=xn[:], in_=acc[:, t, :], func=AF.Square, accum_out=ss[:]),
                         [acc, ss], [xn, ss])
                    P.op(VE, lambda e: e.tensor_scalar(out=rs[:], in0=ss[:], scalar1=1.0 / D, scalar2=EPS,
                                                       op0=ALU.mult, op1=ALU.add), [ss], [rs])
                    P.op(AC, lambda e: e.sqrt(out=rs[:], in_=rs[:]), [rs], [rs]); P.op(VE, lambda e: e.reciprocal(out=rs[:], in_=rs[:]), [rs], [rs])
                    o = ot[t % 2]
                    P.op(VE, lambda e, t=t, o=o: e.scalar_tensor_tensor(out=o[:], in0=acc[:, t, :], scalar=rs[:, 0:1],
                                                                        in1=gfin[:], op0=ALU.mult, op1=ALU.mult),
                         [acc, rs, gfin], [o])
                    last = P.dma(SY, y[r0:r0 + 128, :], o[:], reads=[o], writes=[yd], sembuf=o)
            else:
                for t in range(NT):
                    r0 = t * 128
                    o = ot[t % 2]
                    P.op(GP, lambda e, t=t, o=o: e.tensor_copy(out=o[:], in_=acc[:, t, :]), [acc], [o])
                    P.dma(SY, y[r0:r0 + 128, :], o[:], reads=[o], writes=[yd], sembuf=o)
            for o in ot:
                P.wait_tok(SY, Tok(o.dsem, o.dcnt))
        P.finish()
    return nc


def _host_layout(inputs):
    f = np.float32
    x = np.asarray(inputs["x"], f)
    c = np.asarray(inputs["c"], f)
    pos = np.asarray(inputs["positions"], np.int32)
    w_in = np.ascontiguousarray(np.asarray(inputs["w_in"], f)[0])
    w_uq = np.asarray(inputs["w_uq"], f)[0]
    w_ukv = np.asarray(inputs["w_ukv"], f)[0]
    col = lambda v, n: np.ascontiguousarray(np.asarray(v, f).reshape(n, 128).T)
    w_kpe_sw = np.ascontiguousarray(np.concatenate([w_in[:, 320:384], w_in[:, 400:416], w_in[:, 384:400]], axis=1))
    wq3 = w_uq.reshape(256, NH, 96)
    w_uq_sw = np.ascontiguousarray(
        np.concatenate([wq3[:, :, 0:64], wq3[:, :, 80:96], wq3[:, :, 64:80]], axis=2).reshape(256, 768))
    wkv3 = w_ukv.reshape(128, NH, 128)
    w_uk = np.ascontiguousarray(wkv3[:, :, 0:64].reshape(128, 512))
    w_uv = np.ascontiguousarray(wkv3[:, :, 64:128].reshape(128, 512))
    conv_w = np.asarray(inputs["conv_w"], f)[0]
    conv_col = np.ascontiguousarray(conv_w.reshape(3, 4, 128).transpose(2, 1, 0).reshape(128, 12))
    b_gu = np.asarray(inputs["b_gu"], f)[0]
    bgu_col = np.ascontiguousarray(b_gu.reshape(NE, 16, 128).transpose(2, 0, 1).reshape(128, NE * 16))
    inv_freq = (1.0 / (10000.0 ** (np.arange(0, 32, 2, dtype=np.float32) / np.float32(32)))).astype(f)
    rope_c = np.zeros((128, 2), f)
    for p in range(64, 96):
        rope_c[p, 0] = inv_freq[(p - 64) % 16]
        rope_c[p, 1] = -1.0 if p < 80 else 1.0
    shared = {
        "w_ada": np.ascontiguousarray(np.asarray(inputs["w_ada"], f)[0]),
        "b_ada": np.ascontiguousarray(np.asarray(inputs["b_ada"], f)[0].reshape(1, -1)),
        "gmix_col": col(np.asarray(inputs["norm_mix_g"])[0], 8),
        "gffn_col": col(np.asarray(inputs["norm_ffn_g"])[0], 8),
        "w_in": w_in,
        "w_kpe_sw": w_kpe_sw,
        "qg_col": col(np.asarray(inputs["q_norm_g"])[0], 2),
        "kvg_col": col(np.asarray(inputs["kv_norm_g"])[0], 1),
        "w_uq": np.ascontiguousarray(w_uq),
        "w_uq_sw": w_uq_sw,
        "w_uk": w_uk,
        "w_uv": w_uv,
        "w_up_attn": np.ascontiguousarray(np.asarray(inputs["w_up_attn"], f)[0]),
        "conv_col": conv_col,
        "w_up_conv": np.ascontiguousarray(np.asarray(inputs["w_up_conv"], f)[0]),
        "w_o": np.ascontiguousarray(np.asarray(inputs["w_o"], f)[0]),
        "router_w": np.ascontiguousarray(np.asarray(inputs["router_w"], f)[0]),
        "router_b": np.ascontiguousarray(np.asarray(inputs["router_b"], f)[0].reshape(1, -1)),
        "w_gu": np.ascontiguousarray(np.asarray(inputs["w_gu"], f)[0]),
        "bgu_col": bgu_col,
        "w_down": np.ascontiguousarray(np.asarray(inputs["w_down"], f)[0]),
        "b_down": np.ascontiguousarray(np.asarray(inputs["b_down"], f)[0]),
        "gfin_b": np.ascontiguousarray(np.broadcast_to(np.asarray(inputs["norm_final_g"], f)[None, :], (128, D))),
        "rope_c": rope_c,
        "ident": np.eye(128, dtype=f),
    }
    in_maps = []
    for i in range(8):
        b, half = i // 2, i % 2
        own = slice(half * TOK, (half + 1) * TOK)
        flags = np.zeros((128, 2), f)
        flags[:, 0] = 0.0 if half == 1 else NEG
        flags[:, 1] = 1.0 if half == 1 else 0.0
        m = dict(shared)
        m["x_own"] = np.ascontiguousarray(x[b, own])
        m["x_ctx"] = np.ascontiguousarray(x[b, 0:TOK])
        m["pos_all"] = np.ascontiguousarray(np.concatenate([pos[b, 0:TOK], pos[b, own]]).reshape(1, SEQ))
        m["flags"] = flags
        m["c_col"] = col(c[b], 8)
        in_maps.append(m)
    return in_maps


def kernel(**inputs):
    in_maps = _host_layout(inputs)
    nc = build_program()
    res = run_bass_kernel_spmd(nc, in_maps, core_ids=list(range(8)))
    out = np.zeros((4, SEQ, D), np.float32)
    for i in range(8):
        b, half = i // 2, i % 2
        out[b, half * TOK:(half + 1) * TOK] = res.results[i]["y"]
    return out
```

```python
import numpy as np
from contextlib import ExitStack
import concourse.bass as bass
import concourse.mybir as mybir
from concourse.bass_utils import run_bass_kernel_spmd

F32 = mybir.dt.float32
BF16 = mybir.dt.bfloat16
I32 = mybir.dt.int32
AF = mybir.ActivationFunctionType
ALU = mybir.AluOpType

D = 1024
SEQ = 4096
TOK = 2048
NT = TOK // 128
NG = TOK // 512
NH = 8
NE = 32
EPS = 1e-6
TWO_PI = float(2 * np.pi)
CW1 = 6.28125
CW2 = float(2 * np.pi - 6.28125)
SM_SCALE = float(96 ** -0.5)
NEG = -30000.0

ENGS = ("sync", "scalar", "gpsimd", "vector", "tensor")


class Tok:
    __slots__ = ("sem", "val")

    def __init__(self, sem, val):
        self.sem = sem
        self.val = val


class Buf:
    def __init__(self, name, h):
        self.name = name
        self.h = h
        self.last_w = None
        self.reads = {}
        self.dsem = None
        self.dcnt = 0

    def __getitem__(self, idx):
        return self.h[idx]


class _Rec:
    def __init__(self):
        self.call = None

    def __getattr__(self, name):
        def f(*a, **k):
            assert self.call is None
            self.call = (name, a, k)
            return self
        return f


class Prog:
    def __init__(self, nc, es):
        self.nc = nc
        self.es = es
        self.streams = {e: [] for e in ENGS}
        self.cnt = {e: 0 for e in ENGS}
        self.pending = {e: False for e in ENGS}
        self.esem = {e: es.enter_context(nc.semaphore("c_" + e)) for e in ENGS}
        self.waited = {e: {} for e in ENGS}
        self.nsem = len(ENGS)
        self.fence = {}
        self.dbufs = {}
        self.allbufs = []

    def sb(self, name, shape, dt, es=None):
        h = (es or self.es).enter_context(self.nc.sbuf_tensor(name, list(shape), dt))
        b = Buf(name, h)
        self.allbufs.append(b)
        b.reads = dict(self.fence)
        if es is not None:
            es.callback(self.release, [b])
        return b

    def release(self, bufs):
        for b in bufs:
            toks = list(b.reads.values()) + ([b.last_w] if b.last_w is not None else [])
            for t in toks:
                k = id(t.sem)
                if k not in self.fence or self.fence[k].val < t.val:
                    self.fence[k] = t

    def ps(self, name, shape, dt, es=None):
        h = (es or self.es).enter_context(self.nc.psum_tensor(name, list(shape), dt))
        b = Buf(name, h)
        self.allbufs.append(b)
        return b

    def dram(self, name, h):
        b = Buf(name, h)
        self.allbufs.append(b)
        return b

    def _dsem(self, b):
        if b.dsem is None:
            b.dsem = self.es.enter_context(self.nc.semaphore("d_" + b.name))
            self.nsem += 1
        return b.dsem

    def _deps(self, eng, reads, writes):
        deps = []
        for b in reads:
            if b.last_w is not None:
                deps.append(b.last_w)
        for b in writes:
            if b.last_w is not None:
                deps.append(b.last_w)
            deps.extend(b.reads.values())
        out = {}
        for t in deps:
            if t.sem is self.esem[eng] and eng == "tensor":
                continue
            k = id(t.sem)
            if self.waited[eng].get(k, 0) >= t.val:
                continue
            if k not in out or out[k].val < t.val:
                out[k] = t
        for k, t in out.items():
            self.waited[eng][k] = t.val
        return list(out.values())

    def _record(self, tok, reads, writes):
        for b in reads:
            b.reads[id(tok.sem)] = tok
        for b in writes:
            b.last_w = tok
            b.reads = {}

    def _push(self, eng, deps, act):
        self.streams[eng].append({"deps": [[t.sem, t.val, 0] for t in deps], "act": act})

    def op(self, eng, fn, reads=(), writes=(), sig=True):
        deps = self._deps(eng, reads, writes)
        sem = self.esem[eng]
        if sig:
            self.cnt[eng] += 1
            self.pending[eng] = False
            tok = Tok(sem, self.cnt[eng])
        else:
            self.pending[eng] = True
            tok = Tok(sem, self.cnt[eng] + 1)
        rec = _Rec()
        fn(rec)
        name, a, k = rec.call

        def act(e, i, name=name, a=a, k=k, sig=sig, sem=sem):
            ins = getattr(e, name)(*a, **k)
            if sig:
                ins.then_inc(sem, 1)

        self._push(eng, deps, act)
        self._record(tok, reads, writes)

    def dma(self, eng, out, in_, reads=(), writes=(), sembuf=None):
        deps = self._deps(eng, reads, writes)
        sb = sembuf or (writes[0] if writes else reads[0])
        sem = self._dsem(sb)
        sb.dcnt += 16
        tok = Tok(sem, sb.dcnt)
        self.dbufs[id(sb)] = sb

        def act(e, i, out=out, in_=in_, sem=sem):
            src = in_(i) if callable(in_) else in_
            e.dma_start(out=out, in_=src).then_inc(sem, 16)

        self._push(eng, deps, act)
        self._record(tok, reads, writes)
        return tok

    def wait_tok(self, eng, tok):
        self._push(eng, [tok], lambda e, i: None)

    def mark(self):
        return {"len": {e: len(self.streams[e]) for e in ENGS},
                "cnt": dict(self.cnt),
                "dcnt": {k: b.dcnt for k, b in self.dbufs.items()}}

    def make_loop(self, m_a, m_b, it_b, it_end):
        extra = it_end - it_b - 1
        for e in ENGS:
            a0, b0, b1 = m_a["len"][e], m_b["len"][e], len(self.streams[e])
            A, B = self.streams[e][a0:b0], self.streams[e][b0:b1]
            assert len(A) == len(B), (e, len(A), len(B))
            for x, y_ in zip(A, B):
                assert len(x["deps"]) == len(y_["deps"]), (e, x["deps"], y_["deps"])
                for dx, dy in zip(x["deps"], y_["deps"]):
                    assert dx[0] is dy[0], e
                    dy[2] = dy[1] - dx[1]
                    assert dy[2] >= 0
            self.streams[e][b0:b1] = [{"loop": (it_b, it_end), "body": B}]
        shift = {}
        for e in ENGS:
            d = self.cnt[e] - m_b["cnt"][e]
            assert d == m_b["cnt"][e] - m_a["cnt"][e], e
            shift[id(self.esem[e])] = (m_b["cnt"][e], d * extra)
            self.cnt[e] += d * extra
        for k, b in self.dbufs.items():
            d = b.dcnt - m_b["dcnt"].get(k, 0)
            assert d == m_b["dcnt"].get(k, 0) - m_a["dcnt"].get(k, 0), b.name
            if d:
                shift[id(b.dsem)] = (m_b["dcnt"].get(k, 0), d * extra)
                b.dcnt += d * extra
        seen = set()

        def bump(t):
            if id(t) in seen:
                return
            seen.add(id(t))
            sh = shift.get(id(t.sem))
            if sh and t.val > sh[0]:
                t.val += sh[1]

        for b in self.allbufs:
            if b.last_w is not None:
                bump(b.last_w)
            for t in b.reads.values():
                bump(t)
        for t in self.fence.values():
            bump(t)
        for e in ENGS:
            for k, v in list(self.waited[e].items()):
                sh = shift.get(k)
                if sh and v > sh[0]:
                    self.waited[e][k] = v + sh[1]

    def finish(self):
        nc = self.nc
        for e in ENGS:
            assert not self.pending[e], e

        def run(eng_name, e):
            def emit(item, bases, tmp, it0):
                for sem, val, dv in item["deps"]:
                    if bases is not None and dv:
                        e.reg_add(tmp, bases[dv], val - it0 * dv)
                        e.wait_ge(sem, tmp)
                    else:
                        e.wait_ge(sem, val)

            for item in self.streams[eng_name]:
                if "loop" in item:
                    it0, it1 = item["loop"]
                    dvs = sorted({d[2] for sub in item["body"] for d in sub["deps"] if d[2]})
                    tmp = e.alloc_register("wtmp")
                    bases = {dv: e.alloc_register("wb%d" % dv) for dv in dvs}
                    with e.Fori(it0, it1) as i:
                        for dv, r in bases.items():
                            e.reg_mul(r, i, dv)
                        for sub in item["body"]:
                            emit(sub, bases, tmp, it0)
                            sub["act"](e, i)
                else:
                    emit(item, None, None, 0)
                    item["act"](e, None)

        with nc.Block() as block:
            @block.sync
            def _(e):
                run("sync", e)

            @block.scalar
            def _(e):
                run("scalar", e)

            @block.gpsimd
            def _(e):
                run("gpsimd", e)

            @block.vector
            def _(e):
                run("vector", e)

            @block.tensor
            def _(e):
                run("tensor", e)


def build_program(dbg=None, n_experts=NE, stop_after=None, e_off=0, acc_in=False, final=True):
    nc = bass.Bass("TRN2", target_bir_lowering=False)

    def din(name, shape, dt=F32):
        return nc.dram_tensor(name, list(shape), dt, kind="ExternalInput").ap()

    x_own = din("x_own", [TOK, D])
    x_ctx = din("x_ctx", [TOK, D])
    pos_all = din("pos_all", [1, SEQ], I32)
    flags = din("flags", [128, 2])
    c_col = din("c_col", [128, 8])
    w_ada = din("w_ada", [D, 6 * D])
    b_ada = din("b_ada", [1, 6 * D])
    gmix_col = din("gmix_col", [128, 8])
    gffn_col = din("gffn_col", [128, 8])
    w_in = din("w_in", [D, 4000])
    w_kpe_sw = din("w_kpe_sw", [D, 96])
    qg_col = din("qg_col", [128, 2])
    kvg_col = din("kvg_col", [128, 1])
    w_uq = din("w_uq", [256, 768])
    w_uq_sw = din("w_uq_sw", [256, 768])
    w_uk = din("w_uk", [128, 512])
    w_uv = din("w_uv", [128, 512])
    w_up_attn = din("w_up_attn", [512, D])
    conv_col = din("conv_col", [128, 12])
    w_up_conv = din("w_up_conv", [512, D])
    w_o = din("w_o", [D, D])
    router_w = din("router_w", [D, NE])
    router_b = din("router_b", [1, NE])
    w_gu = din("w_gu", [NE, D, 2 * D])
    bgu_col = din("bgu_col", [128, NE * 16])
    w_down = din("w_down", [NE, D, D])
    b_down = din("b_down", [NE, D])
    gfin_b = din("gfin_b", [128, D])
    rope_c = din("rope_c", [128, 2])
    ident_in = din("ident", [128, 128])
    y = nc.dram_tensor("y", [TOK, D], F32, kind="ExternalOutput").ap()
    acc_prev = din("acc_prev", [TOK, D]) if acc_in else None
    x1_scr = nc.dram_tensor("x1_scr", [TOK, D], F32, kind="Internal").ap()
    g_scr = nc.dram_tensor("g_scr", [128, NE * NT], F32, kind="Internal").ap()
    dbg_aps = {}
    if dbg:
        for k, shp in dbg.items():
            dbg_aps[k] = nc.dram_tensor("dbg_" + k, list(shp), F32, kind="ExternalOutput").ap()

    es = ExitStack()
    with es:
        P = Prog(nc, es)
        SY, AC, GP, VE, PE = "sync", "scalar", "gpsimd", "vector", "tensor"
        x1d = P.dram("x1d", None)
        gsd = P.dram("gsd", None)
        dbgx = P.dram("dbgx", None)
        yd = P.dram("yd", None)

        psb = [P.ps("ps%d" % i, [128, 512], F32) for i in range(8)]
        ps_rr = [0]

        def next_ps():
            b = psb[ps_rr[0] % 8]
            ps_rr[0] += 1
            return b

        ident_f = P.sb("ident_f", [128, 128], F32)
        ident_b = P.sb("ident_b", [128, 128], BF16)
        ones_f = P.sb("ones_f", [128, 128], F32)
        ones_b = P.sb("ones_b", [128, 128], BF16)
        zero_c = P.sb("zero_c", [128, 1], F32)
        flg = P.sb("flg", [128, 2], F32)
        adacol = P.sb("adacol", [128, 32], F32)
        scl_m = P.sb("scl_m", [128, 8], F32)
        scl_f = P.sb("scl_f", [128, 8], F32)
        gm_b = P.sb("gm_b", [128, D], F32)
        gf_b = P.sb("gf_b", [128, D], F32)

        P.dma(SY, ident_f[:], ident_in[:, :], writes=[ident_f])
        P.dma(SY, flg[:], flags[:, :], writes=[flg])
        P.op(VE, lambda e: e.tensor_copy(out=ident_b[:], in_=ident_f[:]), [ident_f], [ident_b])
        P.op(VE, lambda e: e.memset(ones_f[:], 1.0), [], [ones_f])
        P.op(VE, lambda e: e.memset(ones_b[:], 1.0), [], [ones_b])
        P.op(VE, lambda e: e.memset(zero_c[:], 0.0), [], [zero_c])

        with ExitStack() as s0:
            ccol = P.sb("ccol", [128, 8], F32, s0)
            cact = P.sb("cact", [128, 8], F32, s0)
            gmc = P.sb("gmc", [128, 8], F32, s0)
            gfc = P.sb("gfc", [128, 8], F32, s0)
            ada_row = P.sb("ada_row", [1, 6 * D], F32, s0)
            bada = P.sb("bada", [1, 6 * D], F32, s0)
            wa = [P.sb("wa%d" % i, [128, 3072], F32, s0) for i in range(4)]
            P.dma(SY, ccol[:], c_col[:, :], writes=[ccol])
            P.dma(SY, gmc[:], gmix_col[:, :], writes=[gmc])
            P.dma(SY, gfc[:], gffn_col[:, :], writes=[gfc])
            P.dma(SY, bada[:], b_ada[:, :], writes=[bada])
            P.op(AC, lambda e: e.activation(out=cact[:], in_=ccol[:], func=AF.Silu), [ccol], [cact])
            it = 0
            for hh in range(2):
                banks = [next_ps() for _ in range(6)]
                for k in range(8):
                    w = wa[it % 4]
                    it += 1
                    P.dma(SY, w[:], w_ada[k * 128:(k + 1) * 128, hh * 3072:(hh + 1) * 3072], writes=[w])
                    for n in range(6):
                        P.op(PE, lambda e, b=banks[n], w=w, k=k, n=n: e.matmul(
                            b[0:1, :], lhsT=cact[:, k:k + 1], rhs=w[:, n * 512:(n + 1) * 512],
                            start=(k == 0), stop=(k == 7)),
                            [cact, w], [banks[n]], sig=True)
                for n in range(6):
                    c0 = hh * 3072 + n * 512
                    P.op(VE, lambda e, b=banks[n], c0=c0: e.tensor_tensor(
                        out=ada_row[0:1, c0:c0 + 512], in0=b[0:1, :], in1=bada[0:1, c0:c0 + 512], op=ALU.add),
                        [banks[n], bada], [ada_row])
            pcol = next_ps()
            segs = [0, 1, 3, 4]
            for si, sg in enumerate(segs):
                for j in range(8):
                    c0 = sg * D + j * 128
                    idx = si * 8 + j
                    P.op(PE, lambda e, c0=c0, idx=idx: e.matmul(
                        pcol[:, idx:idx + 1], lhsT=ada_row[0:1, c0:c0 + 128], rhs=ones_f[0:1, 0:1],
                        start=True, stop=True), [ada_row, ones_f], [pcol])
            P.op(VE, lambda e: e.tensor_copy(out=adacol[:], in_=pcol[:, 0:32]), [pcol], [adacol])
            P.op(VE, lambda e: e.scalar_tensor_tensor(out=scl_m[:], in0=adacol[:, 8:16], scalar=1.0, in1=gmc[:],
                                                      op0=ALU.add, op1=ALU.mult), [adacol, gmc], [scl_m])
            P.op(VE, lambda e: e.scalar_tensor_tensor(out=scl_f[:], in0=adacol[:, 24:32], scalar=1.0, in1=gfc[:],
                                                      op0=ALU.add, op1=ALU.mult), [adacol, gfc], [scl_f])
            for sg, dst in ((2, gm_b), (5, gf_b)):
                for n in range(2):
                    pb = next_ps()
                    c0 = sg * D + n * 512
                    P.op(PE, lambda e, pb=pb, c0=c0: e.matmul(
                        pb[:, :], lhsT=ones_f[0:1, 0:128], rhs=ada_row[0:1, c0:c0 + 512],
                        start=True, stop=True), [ada_row, ones_f], [pb])
                    P.op(VE, lambda e, pb=pb, dst=dst, n=n: e.tensor_copy(
                        out=dst[:, n * 512:(n + 1) * 512], in_=pb[:, :]), [pb], [dst])
            if dbg and "ada" in dbg:
                P.dma(SY, dbg_aps["ada"][0:1, :], ada_row[0:1, :], reads=[ada_row], sembuf=ada_row)
            if dbg and "gm_b" in dbg:
                P.dma(SY, dbg_aps["gm_b"][:, :], gm_b[:], reads=[gm_b], sembuf=gm_b)
            if dbg and "adacol" in dbg:
                P.dma(SY, dbg_aps["adacol"][:, :], adacol[:], reads=[adacol], sembuf=adacol)

        if stop_after == "0":
            P.finish()
            return nc

        def norm_T(xt, xn, ss, rs, scl, shcol_off, dst, dst_c0):
            P.op(VE, lambda e: e.memset(ss[:], 0.0), [], [ss])
            P.op(AC, lambda e: e.activation(out=xn[:], in_=xt[:], func=AF.Square, accum_out=ss[:]),
                 [xt, ss], [xn, ss])
            P.op(VE, lambda e: e.tensor_scalar(out=rs[:], in0=ss[:], scalar1=1.0 / D, scalar2=EPS,
                                               op0=ALU.mult, op1=ALU.add), [ss], [rs])
            P.op(AC, lambda e: e.sqrt(out=rs[:], in_=rs[:]), [rs], [rs]); P.op(VE, lambda e: e.reciprocal(out=rs[:], in_=rs[:]), [rs], [rs])
            P.op(AC, lambda e: e.activation(out=xn[:], in_=xt[:], func=AF.Identity, scale=rs[:, 0:1]),
                 [xt, rs], [xn])
            pt = next_ps()
            ptb = pt[:].bitcast(BF16)
            for j in range(8):
                P.op(PE, lambda e, j=j, ptb=ptb: e.transpose(
                    ptb[:, j * 128:(j + 1) * 128], xn[:, j * 128:(j + 1) * 128], ident_b[:]),
                    [xn, ident_b], [pt], sig=(j == 7))
            for j in range(8):
                P.op(VE, lambda e, j=j, ptb=ptb: e.tensor_scalar(
                    out=dst[:, j, dst_c0:dst_c0 + 128], in0=ptb[:, j * 128:(j + 1) * 128],
                    scalar1=scl[:, j:j + 1], scalar2=adacol[:, shcol_off + j:shcol_off + j + 1],
                    op0=ALU.mult, op1=ALU.add), [pt, scl, adacol], [dst])

        def rstd_bcast(dst, src_ps, inv_n):
            P.op(VE, lambda e: e.tensor_scalar(out=dst[:], in0=src_ps[:], scalar1=inv_n, scalar2=EPS,
                                               op0=ALU.mult, op1=ALU.add), [src_ps], [dst])
            P.op(AC, lambda e: e.sqrt(out=dst[:], in_=dst[:]), [dst], [dst]); P.op(VE, lambda e: e.reciprocal(out=dst[:], in_=dst[:]), [dst], [dst])

        attn_es = ExitStack()
        es.enter_context(attn_es)
        attnT = P.sb("attnT", [128, NH, TOK], BF16, attn_es)

        with ExitStack() as sa:
            KT = P.sb("KT", [128, NH, SEQ], BF16, sa)
            Vt = P.sb("Vt", [128, 32, NH, 65], BF16, sa)
            wkv = P.sb("wkv", [128, 8, 160], BF16, sa)
            wks = P.sb("wks", [128, 8, 96], BF16, sa)
            wq = P.sb("wq", [128, 8, 256], BF16, sa)
            wuq = P.sb("wuq", [128, 2, 768], BF16, sa)
            wuqs = P.sb("wuqs", [128, 2, 768], BF16, sa)
            wuk = P.sb("wuk", [128, 512], BF16, sa)
            wuv = P.sb("wuv", [128, 512], BF16, sa)
            qg = P.sb("qg", [128, 2], F32, sa)
            kvg = P.sb("kvg", [128, 1], F32, sa)
            rpc = P.sb("rpc", [128, 2], F32, sa)
            hTg = P.sb("hTg", [128, 8, 512], BF16, sa)
            xts = [P.sb("xta%d" % i, [128, D], F32, sa) for i in range(2)]
            xn = P.sb("xna", [128, D], BF16, sa)
            ss = P.sb("ssa", [128, 1], F32, sa)
            rs = P.sb("rsa", [128, 1], F32, sa)
            posi = P.sb("posi", [128, 512], I32, sa)
            ang = P.sb("ang", [128, 512], F32, sa)
            cosT = P.sb("cosT", [128, 512], F32, sa)
            sinT = P.sb("sinT", [128, 512], F32, sa)
            tmp = [P.sb("tmpa%d" % i, [128, 512], F32, sa) for i in range(4)]
            sqb = P.sb("sqb", [128, 2, 512], BF16, sa)
            kvn = P.sb("kvn", [128, 512], BF16, sa)
            qn = P.sb("qn", [128, 2, 512], BF16, sa)
            qT = P.sb("qT", [128, NH, 512], BF16, sa)
            PT = [P.sb("PT%d" % i, [128, 512], BF16, sa) for i in range(4)]
            rsum, rbc = tmp[0], tmp[1]

            w_in_k = w_in.rearrange("(k p) n -> p k n", p=128)
            P.dma(GP, wkv[:], w_in_k[:, :, 256:416], writes=[wkv])
            P.dma(GP, wks[:], w_kpe_sw.rearrange("(k p) n -> p k n", p=128), writes=[wks])
            P.dma(GP, wq[:], w_in_k[:, :, 0:256], writes=[wq])
            P.dma(GP, wuq[:], w_uq.rearrange("(k p) n -> p k n", p=128), writes=[wuq])
            P.dma(GP, wuqs[:], w_uq_sw.rearrange("(k p) n -> p k n", p=128), writes=[wuqs])
            P.dma(GP, wuk[:], w_uk[:, :], writes=[wuk])
            P.dma(GP, wuv[:], w_uv[:, :], writes=[wuv])
            P.dma(SY, qg[:], qg_col[:, :], writes=[qg])
            P.dma(SY, kvg[:], kvg_col[:, :], writes=[kvg])
            P.dma(SY, rpc[:], rope_c[:, :], writes=[rpc])
            P.op(GP, lambda e: e.memset(Vt[:], 1.0), [], [Vt])

            R = slice(64, 96)

            def range_reduce_sin(dst, src, shift):
                t0, t1 = tmp[0], tmp[1]
                if shift != 0.0:
                    P.op(VE, lambda e: e.tensor_scalar(out=t1[R, :], in0=src[R, :], scalar1=shift, scalar2=None,
                                                       op0=ALU.add), [src], [t1])
                    a = t1
                else:
                    a = src
                P.op(VE, lambda e: e.tensor_scalar(out=t0[R, :], in0=a[R, :], scalar1=1.0 / TWO_PI, scalar2=None,
                                                   op0=ALU.mult), [a], [t0])
                P.op(VE, lambda e: e.tensor_copy(out=posi[R, :], in_=t0[R, :]), [t0], [posi])
                P.op(VE, lambda e: e.tensor_copy(out=t0[R, :], in_=posi[R, :]), [posi], [t0])
                P.op(VE, lambda e: e.scalar_tensor_tensor(out=dst[R, :], in0=t0[R, :], scalar=-CW1, in1=a[R, :],
                                                          op0=ALU.mult, op1=ALU.add), [t0, a], [dst])
                P.op(VE, lambda e: e.scalar_tensor_tensor(out=dst[R, :], in0=t0[R, :], scalar=-CW2, in1=dst[R, :],
                                                          op0=ALU.mult, op1=ALU.add), [t0, dst], [dst])
                P.op(VE, lambda e: e.tensor_scalar(out=t0[R, :], in0=dst[R, :], scalar1=float(np.pi),
                                                   scalar2=-TWO_PI, op0=ALU.is_gt, op1=ALU.mult), [dst], [t0])
                P.op(VE, lambda e: e.tensor_tensor(out=dst[R, :], in0=dst[R, :], in1=t0[R, :], op=ALU.add),
                     [dst, t0], [dst])
                P.op(VE, lambda e: e.tensor_scalar(out=t0[R, :], in0=dst[R, :], scalar1=-float(np.pi),
                                                   scalar2=TWO_PI, op0=ALU.is_lt, op1=ALU.mult), [dst], [t0])
                P.op(VE, lambda e: e.tensor_tensor(out=dst[R, :], in0=dst[R, :], in1=t0[R, :], op=ALU.add),
                     [dst, t0], [dst])
                P.op(AC, lambda e: e.activation(out=dst[R, :], in_=dst[R, :], func=AF.Sin), [dst], [dst])

            xi = 0
            for kg in range(8):
                own = kg >= 4
                g = kg - 4
                src = x_own if own else x_ctx
                row0 = (kg % 4) * 512
                k0 = kg * 512
                for tt in range(4):
                    xt = xts[xi % 2]
                    xi += 1
                    r0 = row0 + tt * 128
                    P.dma(SY, xt[:], src[r0:r0 + 128, :], writes=[xt])
                    norm_T(xt, xn, ss, rs, scl_m, 0, hTg, tt * 128)
                pA, pB, pC = next_ps(), next_ps(), next_ps()
                for k in range(8):
                    P.op(PE, lambda e, k=k: e.matmul(pA[:, :], lhsT=wkv[:, k, 0:128], rhs=hTg[:, k, :],
                                                     start=(k == 0), stop=(k == 7)), [wkv, hTg], [pA], sig=(k == 7))
                for k in range(8):
                    P.op(PE, lambda e, k=k: e.matmul(pB[0:96, :], lhsT=wkv[:, k, 64:160], rhs=hTg[:, k, :],
                                                     start=(k == 0), stop=(k == 7)), [wkv, hTg], [pB], sig=(k == 7))
                for k in range(8):
                    P.op(PE, lambda e, k=k: e.matmul(pC[0:96, :], lhsT=wks[:, k, :], rhs=hTg[:, k, :],
                                                     start=(k == 0), stop=(k == 7)), [wks, hTg], [pC], sig=(k == 7))
                P.dma(SY, posi[R, :], pos_all[0:1, k0:k0 + 512].broadcast_to([32, 512]), writes=[posi])
                P.op(VE, lambda e: e.tensor_copy(out=ang[R, :], in_=posi[R, :]), [posi], [ang])
                P.op(VE, lambda e: e.tensor_scalar(out=ang[R, :], in0=ang[R, :], scalar1=rpc[R, 0:1], scalar2=None,
                                                   op0=ALU.mult), [ang, rpc], [ang])
                range_reduce_sin(sinT, ang, 0.0)
                range_reduce_sin(cosT, ang, float(np.pi / 2))
                P.op(VE, lambda e: e.tensor_scalar(out=sinT[R, :], in0=sinT[R, :], scalar1=rpc[R, 1:2], scalar2=None,
                                                   op0=ALU.mult), [sinT, rpc], [sinT])
                t2, t3 = tmp[2], tmp[3]
                P.op(VE, lambda e: e.tensor_tensor(out=t2[R, :], in0=pB[R, :], in1=cosT[R, :], op=ALU.mult),
                     [pB, cosT], [t2])
                P.op(VE, lambda e: e.tensor_tensor(out=t3[R, :], in0=pC[R, :], in1=sinT[R, :], op=ALU.mult),
                     [pC, sinT], [t3])
                P.op(VE, lambda e: e.tensor_tensor(out=t2[R, :], in0=t2[R, :], in1=t3[R, :], op=ALU.add),
                     [t2, t3], [t2])
                for h in range(NH):
                    eng = GP if h % 2 else VE
                    P.op(eng, lambda e, h=h: e.tensor_copy(out=KT[R, h, k0:k0 + 512], in_=t2[R, :]), [t2], [KT])
                P.op(AC, lambda e: e.activation(out=sqb[:, 0, :], in_=pA[:, :], func=AF.Square), [pA], [sqb])
                pD = next_ps()
                P.op(PE, lambda e: e.matmul(pD[:, :], lhsT=ones_b[:], rhs=sqb[:, 0, :], start=True, stop=True),
                     [ones_b, sqb], [pD])
                rstd_bcast(tmp[0], pD, 1.0 / 128)
                P.op(VE, lambda e: e.scalar_tensor_tensor(out=kvn[:], in0=pA[:, :], scalar=kvg[:, 0:1], in1=tmp[0][:],
                                                          op0=ALU.mult, op1=ALU.mult), [pA, kvg, tmp[0]], [kvn])
                for h in range(NH):
                    pk = next_ps()
                    P.op(PE, lambda e, h=h, pk=pk: e.matmul(pk[0:64, :], lhsT=wuk[:, h * 64:(h + 1) * 64], rhs=kvn[:],
                                                            start=True, stop=True), [wuk, kvn], [pk])
                    if h % 2:
                        P.op(AC, lambda e, h=h, pk=pk: e.copy(out=KT[0:64, h, k0:k0 + 512], in_=pk[0:64, :]),
                             [pk], [KT])
                    else:
                        P.op(VE, lambda e, h=h, pk=pk: e.tensor_copy(out=KT[0:64, h, k0:k0 + 512], in_=pk[0:64, :]),
                             [pk], [KT])
                for tt in range(4):
                    pv = next_ps()
                    kt = kg * 4 + tt
                    P.op(PE, lambda e, tt=tt, pv=pv: e.matmul(pv[:, :], lhsT=kvn[:, tt * 128:(tt + 1) * 128], rhs=wuv[:],
                                                              start=True, stop=True), [kvn, wuv], [pv])
                    if tt % 2:
                        P.op(AC, lambda e, kt=kt, pv=pv: e.copy(
                            out=Vt[:, kt, :, 0:64], in_=pv[:, :].rearrange("p (h d) -> p h d", h=NH)), [pv], [Vt])
                    else:
                        P.op(VE, lambda e, kt=kt, pv=pv: e.tensor_copy(
                            out=Vt[:, kt, :, 0:64], in_=pv[:, :].rearrange("p (h d) -> p h d", h=NH)), [pv], [Vt])
                if not own:
                    continue
                pE = [next_ps(), next_ps()]
                for c in range(2):
                    for k in range(8):
                        P.op(PE, lambda e, c=c, k=k: e.matmul(pE[c][:, :], lhsT=wq[:, k, c * 128:(c + 1) * 128],
                                                              rhs=hTg[:, k, :], start=(k == 0), stop=(k == 7)),
                             [wq, hTg], [pE[c]], sig=(k == 7))
                    P.op(AC, lambda e, c=c: e.activation(out=sqb[:, c, :], in_=pE[c][:, :], func=AF.Square),
                         [pE[c]], [sqb])
                pD = next_ps()
                for c in range(2):
                    P.op(PE, lambda e, c=c: e.matmul(pD[:, :], lhsT=ones_b[:], rhs=sqb[:, c, :],
                                                     start=(c == 0), stop=(c == 1)), [ones_b, sqb], [pD], sig=(c == 1))
                rstd_bcast(tmp[0], pD, 1.0 / 256)
                for c in range(2):
                    P.op(VE, lambda e, c=c: e.scalar_tensor_tensor(
                        out=qn[:, c, :], in0=pE[c][:, :], scalar=qg[:, c:c + 1], in1=tmp[0][:],
                        op0=ALU.mult, op1=ALU.mult), [pE[c], qg, tmp[0]], [qn])
                for h in range(NH):
                    pF, pG = next_ps(), next_ps()
                    for c in range(2):
                        P.op(PE, lambda e, c=c, h=h, pF=pF: e.matmul(
                            pF[0:96, :], lhsT=wuq[:, c, h * 96:(h + 1) * 96], rhs=qn[:, c, :],
                            start=(c == 0), stop=(c == 1)), [wuq, qn], [pF], sig=(c == 1))
                    for c in range(2):
                        P.op(PE, lambda e, c=c, h=h, pG=pG: e.matmul(
                            pG[0:96, :], lhsT=wuqs[:, c, h * 96:(h + 1) * 96], rhs=qn[:, c, :],
                            start=(c == 0), stop=(c == 1)), [wuqs, qn], [pG], sig=(c == 1))
                    P.op(AC, lambda e, h=h, pF=pF: e.copy(out=qT[0:64, h, :], in_=pF[0:64, :]), [pF], [qT])
                    P.op(VE, lambda e, pF=pF: e.tensor_tensor(out=t2[R, :], in0=pF[R, :], in1=cosT[R, :], op=ALU.mult),
                         [pF, cosT], [t2])
                    P.op(VE, lambda e, pG=pG: e.tensor_tensor(out=t3[R, :], in0=pG[R, :], in1=sinT[R, :], op=ALU.mult),
                         [pG, sinT], [t3])
                    P.op(VE, lambda e, h=h: e.tensor_tensor(out=qT[R, h, :], in0=t2[R, :], in1=t3[R, :], op=ALU.add),
                         [t2, t3], [qT])
                pti = 0
                for h in range(NH):
                    pO = next_ps()
                    nkt = (kg + 1) * 4
                    for kt in range(nkt):
                        j = kt - kg * 4
                        qoff = max(j, 0) * 128
                        n = 512 - qoff
                        pS = next_ps()
                        if pS is pO:
                            pS = next_ps()
                        P.op(PE, lambda e, h=h, kt=kt, qoff=qoff, n=n, pS=pS: e.matmul(
                            pS[:, 0:n], lhsT=KT[0:96, h, kt * 128:(kt + 1) * 128], rhs=qT[0:96, h, qoff:512],
                            start=True, stop=True), [KT, qT], [pS])
                        pt = PT[pti % 4]
                        pti += 1
                        bias = flg[:, 0:1] if kt < 16 else zero_c[:, 0:1]
                        P.op(AC, lambda e, pt=pt, pS=pS, n=n, bias=bias: e.activation(
                            out=pt[:, 0:n], in_=pS[:, 0:n], func=AF.Exp, bias=bias, scale=SM_SCALE),
                            [pS, flg, zero_c], [pt])
                        if j >= 0:
                            P.op(GP, lambda e, pt=pt: e.memset(pt[64:128, 0:64], 0.0), [], [pt])
                        P.op(PE, lambda e, h=h, kt=kt, qoff=qoff, n=n, pt=pt, pO=pO: e.matmul(
                            pO[0:65, qoff:512], lhsT=Vt[:, kt, h, :], rhs=pt[:, 0:n],
                            start=(kt == 0), stop=(kt == nkt - 1)), [Vt, pt], [pO], sig=(kt == nkt - 1))
                    P.op(VE, lambda e, pO=pO: e.reciprocal(out=rsum[64:65, :], in_=pO[64:65, :]), [pO], [rsum])
                    pN = next_ps()
                    if pN is pO:
                        pN = next_ps()
                    P.op(PE, lambda e, pN=pN: e.matmul(pN[0:64, :], lhsT=ones_f[64:65, 0:64], rhs=rsum[64:65, :],
                                                       start=True, stop=True), [ones_f, rsum], [pN])
                    P.op(AC, lambda e, pN=pN: e.copy(out=rbc[0:64, :], in_=pN[0:64, :]), [pN], [rbc])
                    P.op(VE, lambda e, h=h, pO=pO, g=g: e.tensor_tensor(
                        out=attnT[0:64, h, g * 512:(g + 1) * 512], in0=pO[0:64, :], in1=rbc[0:64, :], op=ALU.mult),
                        [pO, rbc], [attnT])
            if dbg and "attnT" in dbg:
                for h in range(NH):
                    P.op(VE, lambda e: e.tensor_copy(out=tmp[0][0:64, :], in_=attnT[0:64, h, 0:512]), [attnT], [tmp[0]])
                    P.dma(SY, dbg_aps["attnT"][h * 64:(h + 1) * 64, :], tmp[0][0:64, :], reads=[tmp[0]], sembuf=tmp[0])

        if stop_after == "A":
            P.finish()
            return nc

        with ExitStack() as sbk:
            win = P.sb("win", [128, 8, 3584], BF16, sbk)
            wua = P.sb("wua", [64, NH, D], BF16, sbk)
            wuc = P.sb("wuc", [128, 4, D], BF16, sbk)
            wo = P.sb("wo", [128, 8, D], BF16, sbk)
            cvc = P.sb("cvc", [128, 12], F32, sbk)
            xres = P.sb("xres", [128, 4, D], F32, sbk)
            xn = P.sb("xnb", [128, D], BF16, sbk)
            ss = P.sb("ssb", [128, 1], F32, sbk)
            rs = P.sb("rsb", [128, 1], F32, sbk)
            hTg = P.sb("hTgb", [128, 8, 512], BF16, sbk)
            cu = P.sb("cu", [128, 4, 514], F32, sbk)
            bzT = P.sb("bzT", [128, 4, 512], BF16, sbk)
            mT = P.sb("mT", [128, 8, 512], BF16, sbk)
            tb = [P.sb("tmpb%d" % i, [128, 512], F32, sbk) for i in range(6)]
            x1t = [P.sb("x1t%d" % i, [128, D], F32, sbk) for i in range(2)]
            w_in_k = w_in.rearrange("(k p) n -> p k n", p=128)
            for k in range(8):
                P.dma(GP, win[:, k, :], w_in_k[:, k, 416:4000], writes=[win])
            P.dma(GP, wua[:], w_up_attn.rearrange("(h p) n -> p h n", p=64), writes=[wua])
            P.dma(GP, wuc[:], w_up_conv.rearrange("(k p) n -> p k n", p=128), writes=[wuc])
            P.dma(GP, wo[:], w_o.rearrange("(k p) n -> p k n", p=128), writes=[wo])
            P.dma(SY, cvc[:], conv_col[:, :], writes=[cvc])

            def ucb(colbase, ch, rhs_ap, nfree, reads):
                pz = next_ps()
                for k in range(8):
                    P.op(PE, lambda e, k=k, pz=pz: e.matmul(
                        pz[:, 0:nfree], lhsT=win[:, k, colbase + ch * 128:colbase + (ch + 1) * 128],
                        rhs=rhs_ap(k), start=(k == 0), stop=(k == 7)), [win] + reads, [pz], sig=(k == 7))
                return pz

            P.dma(SY, xres[:, 0, :], x_ctx[TOK - 128:TOK, :], writes=[xres])
            def norm_T_res(slot, dst_c0):
                xt = xres
                P.op(VE, lambda e: e.memset(ss[:], 0.0), [], [ss])
                P.op(AC, lambda e: e.activation(out=xn[:], in_=xt[:, slot, :], func=AF.Square, accum_out=ss[:]),
                     [xt, ss], [xn, ss])
                P.op(VE, lambda e: e.tensor_scalar(out=rs[:], in0=ss[:], scalar1=1.0 / D, scalar2=EPS,
                                                   op0=ALU.mult, op1=ALU.add), [ss], [rs])
                P.op(AC, lambda e: e.sqrt(out=rs[:], in_=rs[:]), [rs], [rs]); P.op(VE, lambda e: e.reciprocal(out=rs[:], in_=rs[:]), [rs], [rs])
                P.op(AC, lambda e: e.activation(out=xn[:], in_=xt[:, slot, :], func=AF.Identity, scale=rs[:, 0:1]),
                     [xt, rs], [xn])
                pt = next_ps()
                ptb = pt[:].bitcast(BF16)
                for j in range(8):
                    P.op(PE, lambda e, j=j, ptb=ptb: e.transpose(
                        ptb[:, j * 128:(j + 1) * 128], xn[:, j * 128:(j + 1) * 128], ident_b[:]),
                        [xn, ident_b], [pt], sig=(j == 7))
                for j in range(8):
                    P.op(VE, lambda e, j=j, ptb=ptb: e.tensor_scalar(
                        out=hTg[:, j, dst_c0:dst_c0 + 128], in0=ptb[:, j * 128:(j + 1) * 128],
                        scalar1=scl_m[:, j:j + 1], scalar2=adacol[:, j:j + 1],
                        op0=ALU.mult, op1=ALU.add), [pt, scl_m, adacol], [hTg])

            norm_T_res(0, 0)
            for ch in range(4):
                pu = ucb(0, ch, lambda k: hTg[:, k, 0:128], 128, [hTg])
                P.op(AC, lambda e, pu=pu: e.copy(out=tb[0][:, 0:128], in_=pu[:, 0:128]), [pu], [tb[0]])
                pc = ucb(512, ch, lambda k: hTg[:, k, 0:128], 128, [hTg])
                P.op(VE, lambda e, pc=pc: e.tensor_tensor(out=tb[1][:, 0:128], in0=pc[:, 0:128], in1=tb[0][:, 0:128],
                                                          op=ALU.mult), [pc, tb[0]], [tb[1]])
                P.op(VE, lambda e, ch=ch: e.tensor_scalar(out=cu[:, ch, 512:514], in0=tb[1][:, 126:128],
                                                          scalar1=flg[:, 1:2], scalar2=None, op0=ALU.mult),
                     [tb[1], flg], [cu])

            xo = 0
            for g in range(NG):
                t0 = g * 512
                for tt in range(4):
                    P.dma(SY, xres[:, tt, :], x_own[t0 + tt * 128:t0 + (tt + 1) * 128, :], writes=[xres])
                for tt in range(4):
                    norm_T_res(tt, tt * 128)
                for ch in range(4):
                    P.op(VE, lambda e, ch=ch: e.tensor_copy(out=cu[:, ch, 0:2], in_=cu[:, ch, 512:514]), [cu], [cu])
                    pu = ucb(0, ch, lambda k: hTg[:, k, :], 512, [hTg])
                    P.op(AC, lambda e, pu=pu: e.copy(out=tb[0][:], in_=pu[:, :]), [pu], [tb[0]])
                    pc = ucb(512, ch, lambda k: hTg[:, k, :], 512, [hTg])
                    P.op(VE, lambda e, pc=pc, ch=ch: e.tensor_tensor(out=cu[:, ch, 2:514], in0=pc[:, :], in1=tb[0][:],
                                                                     op=ALU.mult), [pc, tb[0]], [cu])
                    P.op(GP, lambda e, ch=ch: e.tensor_scalar(out=tb[1][:], in0=cu[:, ch, 2:514],
                                                              scalar1=cvc[:, ch * 3 + 2:ch * 3 + 3], scalar2=None,
                                                              op0=ALU.mult), [cu, cvc], [tb[1]])
                    P.op(VE, lambda e, ch=ch: e.scalar_tensor_tensor(out=tb[1][:], in0=cu[:, ch, 1:513],
                                                                     scalar=cvc[:, ch * 3 + 1:ch * 3 + 2], in1=tb[1][:],
                                                                     op0=ALU.mult, op1=ALU.add), [cu, cvc, tb[1]], [tb[1]])
                    P.op(VE, lambda e, ch=ch: e.scalar_tensor_tensor(out=tb[1][:], in0=cu[:, ch, 0:512],
                                                                     scalar=cvc[:, ch * 3:ch * 3 + 1], in1=tb[1][:],
                                                                     op0=ALU.mult, op1=ALU.add), [cu, cvc, tb[1]], [tb[1]])
                    pb = ucb(1024, ch, lambda k: hTg[:, k, :], 512, [hTg])
                    P.op(VE, lambda e, pb=pb, ch=ch: e.tensor_tensor(out=bzT[:, ch, :], in0=pb[:, :], in1=tb[1][:],
                                                                     op=ALU.mult), [pb, tb[1]], [bzT])
                for oc in range(8):
                    pga = ucb(1536, oc, lambda k: hTg[:, k, :], 512, [hTg])
                    P.op(AC, lambda e, pga=pga: e.activation(out=tb[2][:], in_=pga[:, :], func=AF.Sigmoid),
                         [pga], [tb[2]])
                    pgc = ucb(2560, oc, lambda k: hTg[:, k, :], 512, [hTg])
                    P.op(AC, lambda e, pgc=pgc: e.activation(out=tb[3][:], in_=pgc[:, :], func=AF.Sigmoid),
                         [pgc], [tb[3]])
                    pab = next_ps()
                    for h in range(NH):
                        P.op(PE, lambda e, h=h, oc=oc, pab=pab, t0=t0: e.matmul(
                            pab[:, :], lhsT=wua[0:64, h, oc * 128:(oc + 1) * 128], rhs=attnT[0:64, h, t0:t0 + 512],
                            start=(h == 0), stop=(h == NH - 1)), [wua, attnT], [pab], sig=(h == NH - 1))
                    pcb = next_ps()
                    for ch in range(4):
                        P.op(PE, lambda e, ch=ch, oc=oc, pcb=pcb: e.matmul(
                            pcb[:, :], lhsT=wuc[:, ch, oc * 128:(oc + 1) * 128], rhs=bzT[:, ch, :],
                            start=(ch == 0), stop=(ch == 3)), [wuc, bzT], [pcb], sig=(ch == 3))
                    P.op(VE, lambda e, pab=pab: e.tensor_tensor(out=tb[4][:], in0=pab[:, :], in1=tb[2][:], op=ALU.mult),
                         [pab, tb[2]], [tb[4]])
                    P.op(VE, lambda e, pcb=pcb: e.tensor_tensor(out=tb[5][:], in0=pcb[:, :], in1=tb[3][:], op=ALU.mult),
                         [pcb, tb[3]], [tb[5]])
                    P.op(GP, lambda e, oc=oc: e.tensor_tensor(out=mT[:, oc, :], in0=tb[4][:], in1=tb[5][:], op=ALU.add),
                         [tb[4], tb[5]], [mT])
                for tt in range(4):
                    xo_t = x1t[xo % 2]
                    xo += 1
                    for n in range(2):
                        pm = next_ps()
                        for oc in range(8):
                            P.op(PE, lambda e, oc=oc, tt=tt, n=n, pm=pm: e.matmul(
                                pm[:, :], lhsT=mT[:, oc, tt * 128:(tt + 1) * 128], rhs=wo[:, oc, n * 512:(n + 1) * 512],
                                start=(oc == 0), stop=(oc == 7)), [mT, wo], [pm], sig=(oc == 7))
                        P.op(VE, lambda e, pm=pm, n=n, xo_t=xo_t: e.tensor_tensor(
                            out=xo_t[:, n * 512:(n + 1) * 512], in0=pm[:, :], in1=gm_b[:, n * 512:(n + 1) * 512],
                            op=ALU.mult), [pm, gm_b], [xo_t])
                    P.op(GP, lambda e, tt=tt, xo_t=xo_t: e.tensor_tensor(out=xo_t[:], in0=xo_t[:], in1=xres[:, tt, :],
                                                                         op=ALU.add), [xo_t, xres], [xo_t])
                    r0 = t0 + tt * 128
                    P.dma(SY, x1_scr[r0:r0 + 128, :], xo_t[:], reads=[xo_t], writes=[x1d], sembuf=xo_t)
                    if dbg and "x1" in dbg:
                        P.dma(SY, dbg_aps["x1"][r0:r0 + 128, :], xo_t[:], reads=[xo_t], writes=[dbgx], sembuf=dbgx)
        attn_es.close()
        if stop_after == "B":
            P.finish()
            return nc

        with ExitStack() as sc:
            acc = P.sb("acc", [128, NT, D], F32, sc)
            h2T = P.sb("h2T", [128, 8, TOK], BF16, sc)
            gates = P.sb("gates", [128, NT, NE], F32, sc)
            gT = P.sb("gT", [32, 128], F32, sc)
            gatesE = P.sb("gatesE", [128, NE, NT], F32, sc)
            bge = P.sb("bge", [128, 16], F32, sc)
            gce = P.sb("gce", [128, NT], F32, sc)
            rw = P.sb("rw", [128, 8, NE], BF16, sc)
            rb = P.sb("rb", [1, NE], BF16, sc)
            xn = P.sb("xnc", [128, D], BF16, sc)
            ss = P.sb("ssc", [128, 1], F32, sc)
            rs = P.sb("rsc", [128, 1], F32, sc)
            sm = [P.sb("smc%d" % i, [128, 32], F32, sc) for i in range(4)]
            m8 = P.sb("m8", [128, 8], F32, sc)
            wg = [P.sb("wg%d" % i, [128, 8, 512], BF16, sc) for i in range(2)]
            wu = [P.sb("wu%d" % i, [128, 8, 512], BF16, sc) for i in range(2)]
            wd = [P.sb("wd%d" % i, [128, 4, D], BF16, sc) for i in range(2)]
            wst = [P.sb("wst%d" % i, [128, D], F32, sc) for i in range(4)]
            actT = [P.sb("actT%d" % i, [128, 4, 512], BF16, sc) for i in range(2)]
            tcs = [[P.sb("tc%d_%d" % (i, j), [128, 512], F32, sc) for j in range(5)] for i in range(2)]
            ot = wst[0:2]

            P.dma(GP, rw[:], router_w.rearrange("(k p) n -> p k n", p=128), writes=[rw])
            P.dma(GP, rb[:], router_b[:, :], writes=[rb])

            for t in range(NT):
                P.dma(SY, acc[:, t, :], x1_scr[t * 128:(t + 1) * 128, :], reads=[x1d], writes=[acc])
            for t in range(NT):
                r0 = t * 128
                P.op(VE, lambda e: e.memset(ss[:], 0.0), [], [ss])
                P.op(AC, lambda e, t=t: e.activation(out=xn[:], in_=acc[:, t, :], func=AF.Square, accum_out=ss[:]),
                     [acc, ss], [xn, ss])
                P.op(VE, lambda e: e.tensor_scalar(out=rs[:], in0=ss[:], scalar1=1.0 / D, scalar2=EPS,
                                                   op0=ALU.mult, op1=ALU.add), [ss], [rs])
                P.op(AC, lambda e: e.sqrt(out=rs[:], in_=rs[:]), [rs], [rs]); P.op(VE, lambda e: e.reciprocal(out=rs[:], in_=rs[:]), [rs], [rs])
                P.op(AC, lambda e, t=t: e.activation(out=xn[:], in_=acc[:, t, :], func=AF.Identity, scale=rs[:, 0:1]),
                     [acc, rs], [xn])
                pt = next_ps()
                ptb = pt[:].bitcast(BF16)
                for j in range(8):
                    P.op(PE, lambda e, j=j, ptb=ptb: e.transpose(
                        ptb[:, j * 128:(j + 1) * 128], xn[:, j * 128:(j + 1) * 128], ident_b[:]),
                        [xn, ident_b], [pt], sig=(j == 7))
                for j in range(8):
                    P.op(VE, lambda e, j=j, ptb=ptb, r0=r0: e.tensor_scalar(
                        out=h2T[:, j, r0:r0 + 128], in0=ptb[:, j * 128:(j + 1) * 128],
                        scalar1=scl_f[:, j:j + 1], scalar2=adacol[:, 16 + j:17 + j],
                        op0=ALU.mult, op1=ALU.add), [pt, scl_f, adacol], [h2T])
                pl = next_ps()
                for k in range(8):
                    P.op(PE, lambda e, k=k, r0=r0, pl=pl: e.matmul(
                        pl[:, 0:NE], lhsT=h2T[:, k, r0:r0 + 128], rhs=rw[:, k, :], start=(k == 0), stop=False),
                        [h2T, rw], [pl], sig=False)
                P.op(PE, lambda e, pl=pl: e.matmul(pl[:, 0:NE], lhsT=ones_b[0:1, 0:128], rhs=rb[0:1, :],
                                                   start=False, stop=True), [ones_b, rb], [pl])
                lg, ex, mk, _ = sm
                P.op(VE, lambda e, pl=pl: e.tensor_copy(out=lg[:], in_=pl[:, 0:NE]), [pl], [lg])
                P.op(VE, lambda e: e.max(out=m8[:], in_=lg[:]), [lg], [m8])
                P.op(VE, lambda e: e.tensor_scalar(out=mk[:], in0=lg[:], scalar1=m8[:, 3:4], scalar2=None,
                                                   op0=ALU.is_ge), [lg, m8], [mk])
                P.op(VE, lambda e: e.tensor_scalar(out=lg[:], in0=lg[:], scalar1=m8[:, 0:1], scalar2=None,
                                                   op0=ALU.subtract), [lg, m8], [lg])
                P.op(AC, lambda e: e.activation(out=ex[:], in_=lg[:], func=AF.Exp), [lg], [ex])
                P.op(VE, lambda e: e.tensor_tensor(out=ex[:], in0=ex[:], in1=mk[:], op=ALU.mult), [ex, mk], [ex])
                P.op(VE, lambda e: e.reduce_sum(out=ss[:], in_=ex[:], axis=mybir.AxisListType.X), [ex], [ss])
                P.op(VE, lambda e: e.reciprocal(out=rs[:], in_=ss[:]), [ss], [rs])
                P.op(VE, lambda e, t=t: e.tensor_scalar(out=gates[:, t, :], in0=ex[:], scalar1=rs[:, 0:1], scalar2=None,
                                                        op0=ALU.mult), [ex, rs], [gates])
                P.op(GP, lambda e, t=t: e.tensor_copy(out=gatesE[:, :, t], in_=gates[:, t, :]), [gates], [gatesE])
            if dbg and "gates" in dbg:
                P.dma(SY, dbg_aps["gates"][:, :], gates[:].rearrange("p t e -> p (t e)"), reads=[gates], sembuf=gates)

            if acc_in:
                for t in range(NT):
                    P.dma(SY, acc[:, t, :], acc_prev[t * 128:(t + 1) * 128, :], writes=[acc])
            P.dma(SY, g_scr[:, :], gatesE[:].rearrange("p e t -> p (e t)"), reads=[gatesE], writes=[gsd], sembuf=gatesE)
            w_gu_k = w_gu.rearrange("e (k p) n -> e p k n", p=128)
            w_dn_k = w_down.rearrange("e (k p) n -> e p k n", p=128)
            cnts = {"it": 0, "st": 0, "gi": 0}

            pend = [None]

            def down_stage(g, b, aT):
                for tt in range(4):
                    t = g * 4 + tt
                    for n in range(2):
                        pY = next_ps()
                        for fc in range(4):
                            P.op(PE, lambda e, fc=fc, tt=tt, n=n, b=b, pY=pY, aT=aT: e.matmul(
                                pY[:, :], lhsT=aT[:, fc, tt * 128:(tt + 1) * 128],
                                rhs=wd[b][:, fc, n * 512:(n + 1) * 512],
                                start=(fc == 0), stop=(fc == 3)), [aT, wd[b]], [pY], sig=(fc == 3))
                        P.op(VE, lambda e, pY=pY, t=t, n=n: e.scalar_tensor_tensor(
                            out=acc[:, t, n * 512:(n + 1) * 512], in0=pY[:, :],
                            scalar=gce[:, t:t + 1], in1=acc[:, t, n * 512:(n + 1) * 512],
                            op0=ALU.mult, op1=ALU.add), [pY, gce, acc], [acc])

            def expert_iter(ex_s):
                def esel(i):
                    return ex_s if i is None else i

                P.dma(SY, bge[:], lambda i: bgu_col[:, bass.ds(esel(i) * 16, 16)], writes=[bge])
                P.dma(SY, gce[:], lambda i: g_scr[:, bass.ds(esel(i) * NT, NT)], reads=[gsd], writes=[gce])
                for hf in range(2):
                    b = cnts["it"] % 2
                    cnts["it"] += 1
                    P.dma(GP, wg[b][:], lambda i, hf=hf: w_gu_k[bass.ds(esel(i), 1), :, :, hf * 512:(hf + 1) * 512]
                          .rearrange("o p k n -> p (o k) n"), writes=[wg[b]])
                    P.dma(GP, wu[b][:], lambda i, hf=hf: w_gu_k[bass.ds(esel(i), 1), :, :, D + hf * 512:D + (hf + 1) * 512]
                          .rearrange("o p k n -> p (o k) n"), writes=[wu[b]])
                    for fc in range(4):
                        st = wst[cnts["st"] % 4]
                        cnts["st"] += 1
                        P.dma(SY, st[:], lambda i, hf=hf, fc=fc: w_dn_k[bass.ds(esel(i), 1), :, hf * 4 + fc, :]
                              .rearrange("o p n -> p (o n)"), writes=[st])
                        P.op(GP, lambda e, st=st, b=b, fc=fc: e.tensor_tensor(out=wd[b][:, fc, :], in0=st[:], in1=gf_b[:],
                                                                              op=ALU.mult), [st, gf_b], [wd[b]])
                    for g in range(NG):
                        t0 = g * 512
                        aT = actT[cnts["gi"] % 2]
                        tc = tcs[cnts["gi"] % 2]
                        cnts["gi"] += 1
                        for fc in range(4):
                            cg = hf * 4 + fc
                            cuu = 8 + hf * 4 + fc
                            pG, pU = next_ps(), next_ps()
                            for k in range(8):
                                P.op(PE, lambda e, k=k, fc=fc, b=b, pG=pG, t0=t0: e.matmul(
                                    pG[:, :], lhsT=wg[b][:, k, fc * 128:(fc + 1) * 128], rhs=h2T[:, k, t0:t0 + 512],
                                    start=(k == 0), stop=(k == 7)), [wg[b], h2T], [pG], sig=(k == 7))
                            for k in range(8):
                                P.op(PE, lambda e, k=k, fc=fc, b=b, pU=pU, t0=t0: e.matmul(
                                    pU[:, :], lhsT=wu[b][:, k, fc * 128:(fc + 1) * 128], rhs=h2T[:, k, t0:t0 + 512],
                                    start=(k == 0), stop=(k == 7)), [wu[b], h2T], [pU], sig=(k == 7))
                            gcl, sg_, ub, uc, gs = tc
                            P.op(VE, lambda e, pG=pG, cg=cg, gcl=gcl: e.tensor_scalar(
                                out=gcl[:], in0=pG[:, :], scalar1=bge[:, cg:cg + 1], scalar2=7.0,
                                op0=ALU.add, op1=ALU.min), [pG, bge], [gcl])
                            P.op(AC, lambda e, gcl=gcl, sg_=sg_: e.activation(out=sg_[:], in_=gcl[:], func=AF.Sigmoid,
                                                                              scale=1.702), [gcl], [sg_])
                            P.op(AC, lambda e, pU=pU, cuu=cuu, ub=ub: e.activation(
                                out=ub[:], in_=pU[:, :], func=AF.Identity, bias=bge[:, cuu:cuu + 1]), [pU, bge], [ub])
                            P.op(GP, lambda e, ub=ub, uc=uc: e.tensor_scalar(out=uc[:], in0=ub[:], scalar1=7.0, scalar2=-7.0,
                                                                             op0=ALU.min, op1=ALU.max), [ub], [uc])
                            P.op(GP, lambda e, gcl=gcl, sg_=sg_, gs=gs: e.tensor_tensor(out=gs[:], in0=gcl[:], in1=sg_[:],
                                                                                        op=ALU.mult), [gcl, sg_], [gs])
                            P.op(VE, lambda e, uc=uc, gs=gs, aT=aT, fc=fc: e.scalar_tensor_tensor(
                                out=aT[:, fc, :], in0=uc[:], scalar=1.0, in1=gs[:], op0=ALU.add, op1=ALU.mult),
                                [uc, gs], [aT])
                        if pend[0] is not None:
                            down_stage(*pend[0])
                        pend[0] = (g, b, aT)
                if pend[0] is not None:
                    down_stage(*pend[0])
                    pend[0] = None

            e_end = e_off + n_experts
            if n_experts <= 4:
                for ex_i in range(e_off, e_end):
                    expert_iter(ex_i)
            else:
                expert_iter(e_off)
                expert_iter(e_off + 1)
                m_a = P.mark()
                expert_iter(e_off + 2)
                m_b = P.mark()
                expert_iter(e_off + 3)
                P.make_loop(m_a, m_b, e_off + 3, e_end)

            if final:
                last = None
                gfin, bd = wst[2], wst[3]
                P.dma(SY, gfin[:], gfin_b[:, :], writes=[gfin])
                P.dma(SY, bd[0:32, :], b_down[:, :], writes=[bd])
                P.op(VE, lambda e: e.tensor_tensor(out=bd[0:32, :], in0=bd[0:32, :], in1=gf_b[0:32, :], op=ALU.mult),
                     [bd, gf_b], [bd])
                for t in range(NT):
                    r0 = t * 128
                    pg = next_ps()
                    P.op(PE, lambda e, t=t, pg=pg: e.transpose(pg[0:32, 0:128], gates[:, t, :], ident_f[:]),
                         [gates, ident_f], [pg])
                    P.op(VE, lambda e, pg=pg: e.tensor_copy(out=gT[:], in_=pg[0:32, 0:128]), [pg], [gT])
                    for n in range(2):
                        pbd = next_ps()
                        P.op(PE, lambda e, n=n, pbd=pbd: e.matmul(
                            pbd[:, :], lhsT=gT[0:32, :], rhs=bd[0:32, n * 512:(n + 1) * 512],
                            start=True, stop=True), [gT, bd], [pbd])
                        P.op(VE, lambda e, t=t, n=n, pbd=pbd: e.tensor_tensor(
                            out=acc[:, t, n * 512:(n + 1) * 512], in0=pbd[:, :], in1=acc[:, t, n * 512:(n + 1) * 512],
                            op=ALU.add), [pbd, acc], [acc])
                    P.op(VE, lambda e: e.memset(ss[:], 0.0), [], [ss])
                    P.op(AC, lambda e, t=t: e.activation(out=xn[:], in_=acc[:, t, :], func=AF.Square, accum_out=ss[:]),
                         [acc, ss], [xn, ss])
                    P.op(VE, lambda e: e.tensor_scalar(out=rs[:], in0=ss[:], scalar1=1.0 / D, scalar2=EPS,
                                                       op0=ALU.mult, op1=ALU.add), [ss], [rs])
                    P.op(AC, lambda e: e.sqrt(out=rs[:], in_=rs[:]), [rs], [rs]); P.op(VE, lambda e: e.reciprocal(out=rs[:], in_=rs[:]), [rs], [rs])
                    o = ot[t % 2]
                    P.op(VE, lambda e, t=t, o=o: e.scalar_tensor_tensor(out=o[:], in0=acc[:, t, :], scalar=rs[:, 0:1],
                                                                        in1=gfin[:], op0=ALU.mult, op1=ALU.mult),
                         [acc, rs, gfin], [o])
                    last = P.dma(SY, y[r0:r0 + 128, :], o[:], reads=[o], writes=[yd], sembuf=o)
            else:
                for t in range(NT):
                    r0 = t * 128
                    o = ot[t % 2]
                    P.op(GP, lambda e, t=t, o=o: e.tensor_copy(out=o[:], in_=acc[:, t, :]), [acc], [o])
                    P.dma(SY, y[r0:r0 + 128, :], o[:], reads=[o], writes=[yd], sembuf=o)
            for o in wst:
                P.wait_tok(SY, Tok(o.dsem, o.dcnt))
        P.finish()
    return nc


def _host_layout(inputs):
    f = np.float32
    x = np.asarray(inputs["x"], f)
    c = np.asarray(inputs["c"], f)
    pos = np.asarray(inputs["positions"], np.int32)
    w_in = np.ascontiguousarray(np.asarray(inputs["w_in"], f)[0])
    w_uq = np.asarray(inputs["w_uq"], f)[0]
    w_ukv = np.asarray(inputs["w_ukv"], f)[0]
    col = lambda v, n: np.ascontiguousarray(np.asarray(v, f).reshape(n, 128).T)
    w_kpe_sw = np.ascontiguousarray(np.concatenate([w_in[:, 320:384], w_in[:, 400:416], w_in[:, 384:400]], axis=1))
    wq3 = w_uq.reshape(256, NH, 96)
    w_uq_sw = np.ascontiguousarray(
        np.concatenate([wq3[:, :, 0:64], wq3[:, :, 80:96], wq3[:, :, 64:80]], axis=2).reshape(256, 768))
    wkv3 = w_ukv.reshape(128, NH, 128)
    w_uk = np.ascontiguousarray(wkv3[:, :, 0:64].reshape(128, 512))
    w_uv = np.ascontiguousarray(wkv3[:, :, 64:128].reshape(128, 512))
    conv_w = np.asarray(inputs["conv_w"], f)[0]
    conv_col = np.ascontiguousarray(conv_w.reshape(3, 4, 128).transpose(2, 1, 0).reshape(128, 12))
    b_gu = np.asarray(inputs["b_gu"], f)[0]
    bgu_col = np.ascontiguousarray(b_gu.reshape(NE, 16, 128).transpose(2, 0, 1).reshape(128, NE * 16))
    inv_freq = (1.0 / (10000.0 ** (np.arange(0, 32, 2, dtype=np.float32) / np.float32(32)))).astype(f)
    rope_c = np.zeros((128, 2), f)
    for p in range(64, 96):
        rope_c[p, 0] = inv_freq[(p - 64) % 16]
        rope_c[p, 1] = -1.0 if p < 80 else 1.0
    shared = {
        "w_ada": np.ascontiguousarray(np.asarray(inputs["w_ada"], f)[0]),
        "b_ada": np.ascontiguousarray(np.asarray(inputs["b_ada"], f)[0].reshape(1, -1)),
        "gmix_col": col(np.asarray(inputs["norm_mix_g"])[0], 8),
        "gffn_col": col(np.asarray(inputs["norm_ffn_g"])[0], 8),
        "w_in": w_in,
        "w_kpe_sw": w_kpe_sw,
        "qg_col": col(np.asarray(inputs["q_norm_g"])[0], 2),
        "kvg_col": col(np.asarray(inputs["kv_norm_g"])[0], 1),
        "w_uq": np.ascontiguousarray(w_uq),
        "w_uq_sw": w_uq_sw,
        "w_uk": w_uk,
        "w_uv": w_uv,
        "w_up_attn": np.ascontiguousarray(np.asarray(inputs["w_up_attn"], f)[0]),
        "conv_col": conv_col,
        "w_up_conv": np.ascontiguousarray(np.asarray(inputs["w_up_conv"], f)[0]),
        "w_o": np.ascontiguousarray(np.asarray(inputs["w_o"], f)[0]),
        "router_w": np.ascontiguousarray(np.asarray(inputs["router_w"], f)[0]),
        "router_b": np.ascontiguousarray(np.asarray(inputs["router_b"], f)[0].reshape(1, -1)),
        "w_gu": np.ascontiguousarray(np.asarray(inputs["w_gu"], f)[0]),
        "bgu_col": bgu_col,
        "w_down": np.ascontiguousarray(np.asarray(inputs["w_down"], f)[0]),
        "b_down": np.ascontiguousarray(np.asarray(inputs["b_down"], f)[0]),
        "gfin_b": np.ascontiguousarray(np.broadcast_to(np.asarray(inputs["norm_final_g"], f)[None, :], (128, D))),
        "rope_c": rope_c,
        "ident": np.eye(128, dtype=f),
    }
    in_maps = []
    for i in range(8):
        b, half = i // 2, i % 2
        own = slice(half * TOK, (half + 1) * TOK)
        flags = np.zeros((128, 2), f)
        flags[:, 0] = 0.0 if half == 1 else NEG
        flags[:, 1] = 1.0 if half == 1 else 0.0
        m = dict(shared)
        m["x_own"] = np.ascontiguousarray(x[b, own])
        m["x_ctx"] = np.ascontiguousarray(x[b, 0:TOK])
        m["pos_all"] = np.ascontiguousarray(np.concatenate([pos[b, 0:TOK], pos[b, own]]).reshape(1, SEQ))
        m["flags"] = flags
        m["c_col"] = col(c[b], 8)
        in_maps.append(m)
    return in_maps


def kernel(**inputs):
    in_maps = _host_layout(inputs)
    nc = build_program()
    res = run_bass_kernel_spmd(nc, in_maps, core_ids=list(range(8)))
    out = np.zeros((4, SEQ, D), np.float32)
    for i in range(8):
        b, half = i // 2, i % 2
        out[b, half * TOK:(half + 1) * TOK] = res.results[i]["y"]
    return out
```

```python
import numpy as np
from contextlib import ExitStack
import concourse.bass as bass
import concourse.mybir as mybir
from concourse.bass_utils import run_bass_kernel_spmd

F32 = mybir.dt.float32
BF16 = mybir.dt.bfloat16
I32 = mybir.dt.int32
AF = mybir.ActivationFunctionType
ALU = mybir.AluOpType

D = 1024
SEQ = 4096
TOK = 2048
NT = TOK // 128
NG = TOK // 512
NH = 8
NE = 32
EPS = 1e-6
TWO_PI = float(2 * np.pi)
CW1 = 6.28125
CW2 = float(2 * np.pi - 6.28125)
SM_SCALE = float(96 ** -0.5)
NEG = -30000.0

ENGS = ("sync", "scalar", "gpsimd", "vector", "tensor")


class Tok:
    __slots__ = ("sem", "val")

    def __init__(self, sem, val):
        self.sem = sem
        self.val = val


class Buf:
    def __init__(self, name, h):
        self.name = name
        self.h = h
        self.last_w = None
        self.reads = {}
        self.dsem = None
        self.dcnt = 0

    def __getitem__(self, idx):
        return self.h[idx]


class _Rec:
    def __init__(self):
        self.call = None

    def __getattr__(self, name):
        def f(*a, **k):
            assert self.call is None
            self.call = (name, a, k)
            return self
        return f


class Prog:
    def __init__(self, nc, es):
        self.nc = nc
        self.es = es
        self.streams = {e: [] for e in ENGS}
        self.cnt = {e: 0 for e in ENGS}
        self.pending = {e: False for e in ENGS}
        self.esem = {e: es.enter_context(nc.semaphore("c_" + e)) for e in ENGS}
        self.waited = {e: {} for e in ENGS}
        self.nsem = len(ENGS)
        self.fence = {}
        self.dbufs = {}
        self.allbufs = []

    def sb(self, name, shape, dt, es=None):
        h = (es or self.es).enter_context(self.nc.sbuf_tensor(name, list(shape), dt))
        b = Buf(name, h)
        self.allbufs.append(b)
        b.reads = dict(self.fence)
        if es is not None:
            es.callback(self.release, [b])
        return b

    def release(self, bufs):
        for b in bufs:
            toks = list(b.reads.values()) + ([b.last_w] if b.last_w is not None else [])
            for t in toks:
                k = id(t.sem)
                if k not in self.fence or self.fence[k].val < t.val:
                    self.fence[k] = t

    def ps(self, name, shape, dt, es=None):
        h = (es or self.es).enter_context(self.nc.psum_tensor(name, list(shape), dt))
        b = Buf(name, h)
        self.allbufs.append(b)
        return b

    def dram(self, name, h):
        b = Buf(name, h)
        self.allbufs.append(b)
        return b

    def _dsem(self, b):
        if b.dsem is None:
            b.dsem = self.es.enter_context(self.nc.semaphore("d_" + b.name))
            self.nsem += 1
        return b.dsem

    def _deps(self, eng, reads, writes):
        deps = []
        for b in reads:
            if b.last_w is not None:
                deps.append(b.last_w)
        for b in writes:
            if b.last_w is not None:
                deps.append(b.last_w)
            deps.extend(b.reads.values())
        out = {}
        for t in deps:
            if t.sem is self.esem[eng] and eng == "tensor":
                continue
            k = id(t.sem)
            if self.waited[eng].get(k, 0) >= t.val:
                continue
            if k not in out or out[k].val < t.val:
                out[k] = t
        for k, t in out.items():
            self.waited[eng][k] = t.val
        return list(out.values())

    def _record(self, tok, reads, writes):
        for b in reads:
            b.reads[id(tok.sem)] = tok
        for b in writes:
            b.last_w = tok
            b.reads = {}

    def _push(self, eng, deps, act):
        self.streams[eng].append({"deps": [[t.sem, t.val, 0] for t in deps], "act": act})

    def op(self, eng, fn, reads=(), writes=(), sig=True):
        deps = self._deps(eng, reads, writes)
        sem = self.esem[eng]
        if sig:
            self.cnt[eng] += 1
            self.pending[eng] = False
            tok = Tok(sem, self.cnt[eng])
        else:
            self.pending[eng] = True
            tok = Tok(sem, self.cnt[eng] + 1)
        rec = _Rec()
        fn(rec)
        name, a, k = rec.call

        def act(e, i, name=name, a=a, k=k, sig=sig, sem=sem):
            ins = getattr(e, name)(*a, **k)
            if sig:
                ins.then_inc(sem, 1)

        self._push(eng, deps, act)
        self._record(tok, reads, writes)

    def dma(self, eng, out, in_, reads=(), writes=(), sembuf=None):
        deps = self._deps(eng, reads, writes)
        sb = sembuf or (writes[0] if writes else reads[0])
        sem = self._dsem(sb)
        sb.dcnt += 16
        tok = Tok(sem, sb.dcnt)
        self.dbufs[id(sb)] = sb

        def act(e, i, out=out, in_=in_, sem=sem):
            src = in_(i) if callable(in_) else in_
            e.dma_start(out=out, in_=src).then_inc(sem, 16)

        self._push(eng, deps, act)
        self._record(tok, reads, writes)
        return tok

    def wait_tok(self, eng, tok):
        self._push(eng, [tok], lambda e, i: None)

    def mark(self):
        return {"len": {e: len(self.streams[e]) for e in ENGS},
                "cnt": dict(self.cnt),
                "dcnt": {k: b.dcnt for k, b in self.dbufs.items()}}

    def make_loop(self, m_a, m_b, it_b, it_end):
        extra = it_end - it_b - 1
        for e in ENGS:
            a0, b0, b1 = m_a["len"][e], m_b["len"][e], len(self.streams[e])
            A, B = self.streams[e][a0:b0], self.streams[e][b0:b1]
            assert len(A) == len(B), (e, len(A), len(B))
            for x, y_ in zip(A, B):
                assert len(x["deps"]) == len(y_["deps"]), (e, x["deps"], y_["deps"])
                for dx, dy in zip(x["deps"], y_["deps"]):
                    assert dx[0] is dy[0], e
                    dy[2] = dy[1] - dx[1]
                    assert dy[2] >= 0
            self.streams[e][b0:b1] = [{"loop": (it_b, it_end), "body": B}]
        shift = {}
        for e in ENGS:
            d = self.cnt[e] - m_b["cnt"][e]
            assert d == m_b["cnt"][e] - m_a["cnt"][e], e
            shift[id(self.esem[e])] = (m_b["cnt"][e], d * extra)
            self.cnt[e] += d * extra
        for k, b in self.dbufs.items():
            d = b.dcnt - m_b["dcnt"].get(k, 0)
            assert d == m_b["dcnt"].get(k, 0) - m_a["dcnt"].get(k, 0), b.name
            if d:
                shift[id(b.dsem)] = (m_b["dcnt"].get(k, 0), d * extra)
                b.dcnt += d * extra
        seen = set()

        def bump(t):
            if id(t) in seen:
                return
            seen.add(id(t))
            sh = shift.get(id(t.sem))
            if sh and t.val > sh[0]:
                t.val += sh[1]

        for b in self.allbufs:
            if b.last_w is not None:
                bump(b.last_w)
            for t in b.reads.values():
                bump(t)
        for t in self.fence.values():
            bump(t)
        for e in ENGS:
            for k, v in list(self.waited[e].items()):
                sh = shift.get(k)
                if sh and v > sh[0]:
                    self.waited[e][k] = v + sh[1]

    def finish(self):
        nc = self.nc
        for e in ENGS:
            assert not self.pending[e], e

        def run(eng_name, e):
            def emit(item, bases, tmp, it0):
                for sem, val, dv in item["deps"]:
                    if bases is not None and dv:
                        e.reg_add(tmp, bases[dv], val - it0 * dv)
                        e.wait_ge(sem, tmp)
                    else:
                        e.wait_ge(sem, val)

            for item in self.streams[eng_name]:
                if "loop" in item:
                    it0, it1 = item["loop"]
                    dvs = sorted({d[2] for sub in item["body"] for d in sub["deps"] if d[2]})
                    tmp = e.alloc_register("wtmp")
                    bases = {dv: e.alloc_register("wb%d" % dv) for dv in dvs}
                    with e.Fori(it0, it1) as i:
                        for dv, r in bases.items():
                            e.reg_mul(r, i, dv)
                        for sub in item["body"]:
                            emit(sub, bases, tmp, it0)
                            sub["act"](e, i)
                else:
                    emit(item, None, None, 0)
                    item["act"](e, None)

        with nc.Block() as block:
            @block.sync
            def _(e):
                run("sync", e)

            @block.scalar
            def _(e):
                run("scalar", e)

            @block.gpsimd
            def _(e):
                run("gpsimd", e)

            @block.vector
            def _(e):
                run("vector", e)

            @block.tensor
            def _(e):
                run("tensor", e)


def build_program(dbg=None, n_experts=NE, stop_after=None, e_off=0, acc_in=False, final=True):
    nc = bass.Bass("TRN2", target_bir_lowering=False)

    def din(name, shape, dt=F32):
        return nc.dram_tensor(name, list(shape), dt, kind="ExternalInput").ap()

    x_own = din("x_own", [TOK, D])
    x_ctx = din("x_ctx", [TOK, D])
    pos_all = din("pos_all", [1, SEQ], I32)
    flags = din("flags", [128, 2])
    c_col = din("c_col", [128, 8])
    w_ada = din("w_ada", [D, 6 * D])
    b_ada = din("b_ada", [1, 6 * D])
    gmix_col = din("gmix_col", [128, 8])
    gffn_col = din("gffn_col", [128, 8])
    w_in = din("w_in", [D, 4000])
    w_kpe_sw = din("w_kpe_sw", [D, 96])
    qg_col = din("qg_col", [128, 2])
    kvg_col = din("kvg_col", [128, 1])
    w_uq = din("w_uq", [256, 768])
    w_uq_sw = din("w_uq_sw", [256, 768])
    w_uk = din("w_uk", [128, 512])
    w_uv = din("w_uv", [128, 512])
    w_up_attn = din("w_up_attn", [512, D])
    conv_col = din("conv_col", [128, 12])
    w_up_conv = din("w_up_conv", [512, D])
    w_o = din("w_o", [D, D])
    router_w = din("router_w", [D, NE])
    router_b = din("router_b", [1, NE])
    w_gu = din("w_gu", [NE, D, 2 * D])
    bgu_col = din("bgu_col", [128, NE * 16])
    w_down = din("w_down", [NE, D, D])
    b_down = din("b_down", [NE, D])
    gfin_b = din("gfin_b", [128, D])
    rope_c = din("rope_c", [128, 2])
    ident_in = din("ident", [128, 128])
    y = nc.dram_tensor("y", [TOK, D], F32, kind="ExternalOutput").ap()
    acc_prev = din("acc_prev", [TOK, D]) if acc_in else None
    x1_scr = nc.dram_tensor("x1_scr", [TOK, D], F32, kind="Internal").ap()
    g_scr = nc.dram_tensor("g_scr", [128, NE * NT], F32, kind="Internal").ap()
    dbg_aps = {}
    if dbg:
        for k, shp in dbg.items():
            dbg_aps[k] = nc.dram_tensor("dbg_" + k, list(shp), F32, kind="ExternalOutput").ap()

    es = ExitStack()
    with es:
        P = Prog(nc, es)
        SY, AC, GP, VE, PE = "sync", "scalar", "gpsimd", "vector", "tensor"
        x1d = P.dram("x1d", None)
        gsd = P.dram("gsd", None)
        dbgx = P.dram("dbgx", None)
        yd = P.dram("yd", None)

        psb = [P.ps("ps%d" % i, [128, 512], F32) for i in range(8)]
        ps_rr = [0]

        def next_ps():
            b = psb[ps_rr[0] % 8]
            ps_rr[0] += 1
            return b

        ident_f = P.sb("ident_f", [128, 128], F32)
        ident_b = P.sb("ident_b", [128, 128], BF16)
        ones_f = P.sb("ones_f", [128, 128], F32)
        ones_b = P.sb("ones_b", [128, 128], BF16)
        zero_c = P.sb("zero_c", [128, 1], F32)
        flg = P.sb("flg", [128, 2], F32)
        adacol = P.sb("adacol", [128, 32], F32)
        scl_m = P.sb("scl_m", [128, 8], F32)
        scl_f = P.sb("scl_f", [128, 8], F32)
        gm_b = P.sb("gm_b", [128, D], F32)
        gf_b = P.sb("gf_b", [128, D], F32)

        P.dma(SY, ident_f[:], ident_in[:, :], writes=[ident_f])
        P.dma(SY, flg[:], flags[:, :], writes=[flg])
        P.op(VE, lambda e: e.tensor_copy(out=ident_b[:], in_=ident_f[:]), [ident_f], [ident_b])
        P.op(VE, lambda e: e.memset(ones_f[:], 1.0), [], [ones_f])
        P.op(VE, lambda e: e.memset(ones_b[:], 1.0), [], [ones_b])
        P.op(VE, lambda e: e.memset(zero_c[:], 0.0), [], [zero_c])

        with ExitStack() as s0:
            ccol = P.sb("ccol", [128, 8], F32, s0)
            cact = P.sb("cact", [128, 8], F32, s0)
            gmc = P.sb("gmc", [128, 8], F32, s0)
            gfc = P.sb("gfc", [128, 8], F32, s0)
            ada_row = P.sb("ada_row", [1, 6 * D], F32, s0)
            bada = P.sb("bada", [1, 6 * D], F32, s0)
            wa = [P.sb("wa%d" % i, [128, 3072], F32, s0) for i in range(4)]
            P.dma(SY, ccol[:], c_col[:, :], writes=[ccol])
            P.dma(SY, gmc[:], gmix_col[:, :], writes=[gmc])
            P.dma(SY, gfc[:], gffn_col[:, :], writes=[gfc])
            P.dma(SY, bada[:], b_ada[:, :], writes=[bada])
            P.op(AC, lambda e: e.activation(out=cact[:], in_=ccol[:], func=AF.Silu), [ccol], [cact])
            it = 0
            for hh in range(2):
                banks = [next_ps() for _ in range(6)]
                for k in range(8):
                    w = wa[it % 4]
                    it += 1
                    P.dma(SY, w[:], w_ada[k * 128:(k + 1) * 128, hh * 3072:(hh + 1) * 3072], writes=[w])
                    for n in range(6):
                        P.op(PE, lambda e, b=banks[n], w=w, k=k, n=n: e.matmul(
                            b[0:1, :], lhsT=cact[:, k:k + 1], rhs=w[:, n * 512:(n + 1) * 512],
                            start=(k == 0), stop=(k == 7)),
                            [cact, w], [banks[n]], sig=True)
                for n in range(6):
                    c0 = hh * 3072 + n * 512
                    P.op(VE, lambda e, b=banks[n], c0=c0: e.tensor_tensor(
                        out=ada_row[0:1, c0:c0 + 512], in0=b[0:1, :], in1=bada[0:1, c0:c0 + 512], op=ALU.add),
                        [banks[n], bada], [ada_row])
            pcol = next_ps()
            segs = [0, 1, 3, 4]
            for si, sg in enumerate(segs):
                for j in range(8):
                    c0 = sg * D + j * 128
                    idx = si * 8 + j
                    P.op(PE, lambda e, c0=c0, idx=idx: e.matmul(
                        pcol[:, idx:idx + 1], lhsT=ada_row[0:1, c0:c0 + 128], rhs=ones_f[0:1, 0:1],
                        start=True, stop=True), [ada_row, ones_f], [pcol])
            P.op(VE, lambda e: e.tensor_copy(out=adacol[:], in_=pcol[:, 0:32]), [pcol], [adacol])
            P.op(VE, lambda e: e.scalar_tensor_tensor(out=scl_m[:], in0=adacol[:, 8:16], scalar=1.0, in1=gmc[:],
                                                      op0=ALU.add, op1=ALU.mult), [adacol, gmc], [scl_m])
            P.op(VE, lambda e: e.scalar_tensor_tensor(out=scl_f[:], in0=adacol[:, 24:32], scalar=1.0, in1=gfc[:],
                                                      op0=ALU.add, op1=ALU.mult), [adacol, gfc], [scl_f])
            for sg, dst in ((2, gm_b), (5, gf_b)):
                for n in range(2):
                    pb = next_ps()
                    c0 = sg * D + n * 512
                    P.op(PE, lambda e, pb=pb, c0=c0: e.matmul(
                        pb[:, :], lhsT=ones_f[0:1, 0:128], rhs=ada_row[0:1, c0:c0 + 512],
                        start=True, stop=True), [ada_row, ones_f], [pb])
                    P.op(VE, lambda e, pb=pb, dst=dst, n=n: e.tensor_copy(
                        out=dst[:, n * 512:(n + 1) * 512], in_=pb[:, :]), [pb], [dst])
            if dbg and "ada" in dbg:
                P.dma(SY, dbg_aps["ada"][0:1, :], ada_row[0:1, :], reads=[ada_row], sembuf=ada_row)
            if dbg and "gm_b" in dbg:
                P.dma(SY, dbg_aps["gm_b"][:, :], gm_b[:], reads=[gm_b], sembuf=gm_b)
            if dbg and "adacol" in dbg:
                P.dma(SY, dbg_aps["adacol"][:, :], adacol[:], reads=[adacol], sembuf=adacol)

        if stop_after == "0":
            P.finish()
            return nc

        def norm_T(xt, xn, ss, rs, scl, shcol_off, dst, dst_c0):
            P.op(VE, lambda e: e.memset(ss[:], 0.0), [], [ss])
            P.op(AC, lambda e: e.activation(out=xn[:], in_=xt[:], func=AF.Square, accum_out=ss[:]),
                 [xt, ss], [xn, ss])
            P.op(VE, lambda e: e.tensor_scalar(out=rs[:], in0=ss[:], scalar1=1.0 / D, scalar2=EPS,
                                               op0=ALU.mult, op1=ALU.add), [ss], [rs])
            P.op(AC, lambda e: e.sqrt(out=rs[:], in_=rs[:]), [rs], [rs]); P.op(VE, lambda e: e.reciprocal(out=rs[:], in_=rs[:]), [rs], [rs])
            P.op(AC, lambda e: e.activation(out=xn[:], in_=xt[:], func=AF.Identity, scale=rs[:, 0:1]),
                 [xt, rs], [xn])
            pt = next_ps()
            ptb = pt[:].bitcast(BF16)
            for j in range(8):
                P.op(PE, lambda e, j=j, ptb=ptb: e.transpose(
                    ptb[:, j * 128:(j + 1) * 128], xn[:, j * 128:(j + 1) * 128], ident_b[:]),
                    [xn, ident_b], [pt], sig=(j == 7))
            for j in range(8):
                P.op(VE, lambda e, j=j, ptb=ptb: e.tensor_scalar(
                    out=dst[:, j, dst_c0:dst_c0 + 128], in0=ptb[:, j * 128:(j + 1) * 128],
                    scalar1=scl[:, j:j + 1], scalar2=adacol[:, shcol_off + j:shcol_off + j + 1],
                    op0=ALU.mult, op1=ALU.add), [pt, scl, adacol], [dst])

        def rstd_bcast(dst, src_ps, inv_n):
            P.op(VE, lambda e: e.tensor_scalar(out=dst[:], in0=src_ps[:], scalar1=inv_n, scalar2=EPS,
                                               op0=ALU.mult, op1=ALU.add), [src_ps], [dst])
            P.op(AC, lambda e: e.sqrt(out=dst[:], in_=dst[:]), [dst], [dst]); P.op(VE, lambda e: e.reciprocal(out=dst[:], in_=dst[:]), [dst], [dst])

        attn_es = ExitStack()
        es.enter_context(attn_es)
        attnT = P.sb("attnT", [128, NH, TOK], BF16, attn_es)

        with ExitStack() as sa:
            KT = P.sb("KT", [128, NH, SEQ], BF16, sa)
            Vt = P.sb("Vt", [128, 32, NH, 65], BF16, sa)
            wkv = P.sb("wkv", [128, 8, 160], BF16, sa)
            wks = P.sb("wks", [128, 8, 96], BF16, sa)
            wq = P.sb("wq", [128, 8, 256], BF16, sa)
            wuq = P.sb("wuq", [128, 2, 768], BF16, sa)
            wuqs = P.sb("wuqs", [128, 2, 768], BF16, sa)
            wuk = P.sb("wuk", [128, 512], BF16, sa)
            wuv = P.sb("wuv", [128, 512], BF16, sa)
            qg = P.sb("qg", [128, 2], F32, sa)
            kvg = P.sb("kvg", [128, 1], F32, sa)
            rpc = P.sb("rpc", [128, 2], F32, sa)
            hTg = P.sb("hTg", [128, 8, 512], BF16, sa)
            xts = [P.sb("xta%d" % i, [128, D], F32, sa) for i in range(2)]
            xn2 = [P.sb("xna%d" % i, [128, D], BF16, sa) for i in range(2)]
            ss2 = [P.sb("ssa%d" % i, [128, 1], F32, sa) for i in range(2)]
            rs2 = [P.sb("rsa%d" % i, [128, 1], F32, sa) for i in range(2)]
            posi = P.sb("posi", [128, 512], I32, sa)
            ang = P.sb("ang", [128, 512], F32, sa)
            cosT = P.sb("cosT", [128, 512], F32, sa)
            sinT = P.sb("sinT", [128, 512], F32, sa)
            tmp = [P.sb("tmpa%d" % i, [128, 512], F32, sa) for i in range(4)]
            sqb = P.sb("sqb", [128, 2, 512], BF16, sa)
            kvn = P.sb("kvn", [128, 512], BF16, sa)
            qn = P.sb("qn", [128, 2, 512], BF16, sa)
            qT = P.sb("qT", [128, NH, 512], BF16, sa)
            PT = [P.sb("PT%d" % i, [128, 512], BF16, sa) for i in range(4)]
            rsum, rbc = tmp[0], tmp[1]

            w_in_k = w_in.rearrange("(k p) n -> p k n", p=128)
            P.dma(GP, wkv[:], w_in_k[:, :, 256:416], writes=[wkv])
            P.dma(GP, wks[:], w_kpe_sw.rearrange("(k p) n -> p k n", p=128), writes=[wks])
            P.dma(GP, wq[:], w_in_k[:, :, 0:256], writes=[wq])
            P.dma(GP, wuq[:], w_uq.rearrange("(k p) n -> p k n", p=128), writes=[wuq])
            P.dma(GP, wuqs[:], w_uq_sw.rearrange("(k p) n -> p k n", p=128), writes=[wuqs])
            P.dma(GP, wuk[:], w_uk[:, :], writes=[wuk])
            P.dma(GP, wuv[:], w_uv[:, :], writes=[wuv])
            P.dma(SY, qg[:], qg_col[:, :], writes=[qg])
            P.dma(SY, kvg[:], kvg_col[:, :], writes=[kvg])
            P.dma(SY, rpc[:], rope_c[:, :], writes=[rpc])
            P.op(GP, lambda e: e.memset(Vt[:], 1.0), [], [Vt])

            R = slice(64, 96)

            def range_reduce_sin(dst, src, shift):
                t0, t1 = tmp[0], tmp[1]
                if shift != 0.0:
                    P.op(VE, lambda e: e.tensor_scalar(out=t1[R, :], in0=src[R, :], scalar1=shift, scalar2=None,
                                                       op0=ALU.add), [src], [t1])
                    a = t1
                else:
                    a = src
                P.op(VE, lambda e: e.tensor_scalar(out=t0[R, :], in0=a[R, :], scalar1=1.0 / TWO_PI, scalar2=None,
                                                   op0=ALU.mult), [a], [t0])
                P.op(VE, lambda e: e.tensor_copy(out=posi[R, :], in_=t0[R, :]), [t0], [posi])
                P.op(VE, lambda e: e.tensor_copy(out=t0[R, :], in_=posi[R, :]), [posi], [t0])
                P.op(VE, lambda e: e.scalar_tensor_tensor(out=dst[R, :], in0=t0[R, :], scalar=-CW1, in1=a[R, :],
                                                          op0=ALU.mult, op1=ALU.add), [t0, a], [dst])
                P.op(VE, lambda e: e.scalar_tensor_tensor(out=dst[R, :], in0=t0[R, :], scalar=-CW2, in1=dst[R, :],
                                                          op0=ALU.mult, op1=ALU.add), [t0, dst], [dst])
                P.op(VE, lambda e: e.tensor_scalar(out=t0[R, :], in0=dst[R, :], scalar1=float(np.pi),
                                                   scalar2=-TWO_PI, op0=ALU.is_gt, op1=ALU.mult), [dst], [t0])
                P.op(VE, lambda e: e.tensor_tensor(out=dst[R, :], in0=dst[R, :], in1=t0[R, :], op=ALU.add),
                     [dst, t0], [dst])
                P.op(VE, lambda e: e.tensor_scalar(out=t0[R, :], in0=dst[R, :], scalar1=-float(np.pi),
                                                   scalar2=TWO_PI, op0=ALU.is_lt, op1=ALU.mult), [dst], [t0])
                P.op(VE, lambda e: e.tensor_tensor(out=dst[R, :], in0=dst[R, :], in1=t0[R, :], op=ALU.add),
                     [dst, t0], [dst])
                P.op(AC, lambda e: e.activation(out=dst[R, :], in_=dst[R, :], func=AF.Sin), [dst], [dst])

            xi = 0
            for kg in range(8):
                own = kg >= 4
                g = kg - 4
                src = x_own if own else x_ctx
                row0 = (kg % 4) * 512
                k0 = kg * 512
                for tt in range(4):
                    xt = xts[xi % 2]
                    xi += 1
                    r0 = row0 + tt * 128
                    P.dma(SY, xt[:], src[r0:r0 + 128, :], writes=[xt])
                    norm_T(xt, xn2[xi % 2], ss2[xi % 2], rs2[xi % 2], scl_m, 0, hTg, tt * 128)
                pA, pB, pC = next_ps(), next_ps(), next_ps()
                for k in range(8):
                    P.op(PE, lambda e, k=k: e.matmul(pA[:, :], lhsT=wkv[:, k, 0:128], rhs=hTg[:, k, :],
                                                     start=(k == 0), stop=(k == 7)), [wkv, hTg], [pA], sig=(k == 7))
                for k in range(8):
                    P.op(PE, lambda e, k=k: e.matmul(pB[0:96, :], lhsT=wkv[:, k, 64:160], rhs=hTg[:, k, :],
                                                     start=(k == 0), stop=(k == 7)), [wkv, hTg], [pB], sig=(k == 7))
                for k in range(8):
                    P.op(PE, lambda e, k=k: e.matmul(pC[0:96, :], lhsT=wks[:, k, :], rhs=hTg[:, k, :],
                                                     start=(k == 0), stop=(k == 7)), [wks, hTg], [pC], sig=(k == 7))
                P.dma(SY, posi[R, :], pos_all[0:1, k0:k0 + 512].broadcast_to([32, 512]), writes=[posi])
                P.op(VE, lambda e: e.tensor_copy(out=ang[R, :], in_=posi[R, :]), [posi], [ang])
                P.op(VE, lambda e: e.tensor_scalar(out=ang[R, :], in0=ang[R, :], scalar1=rpc[R, 0:1], scalar2=None,
                                                   op0=ALU.mult), [ang, rpc], [ang])
                range_reduce_sin(sinT, ang, 0.0)
                range_reduce_sin(cosT, ang, float(np.pi / 2))
                P.op(VE, lambda e: e.tensor_scalar(out=sinT[R, :], in0=sinT[R, :], scalar1=rpc[R, 1:2], scalar2=None,
                                                   op0=ALU.mult), [sinT, rpc], [sinT])
                t2, t3 = tmp[2], tmp[3]
                P.op(VE, lambda e: e.tensor_tensor(out=t2[R, :], in0=pB[R, :], in1=cosT[R, :], op=ALU.mult),
                     [pB, cosT], [t2])
                P.op(VE, lambda e: e.tensor_tensor(out=t3[R, :], in0=pC[R, :], in1=sinT[R, :], op=ALU.mult),
                     [pC, sinT], [t3])
                P.op(VE, lambda e: e.tensor_tensor(out=t2[R, :], in0=t2[R, :], in1=t3[R, :], op=ALU.add),
                     [t2, t3], [t2])
                for h in range(NH):
                    eng = GP if h % 2 else VE
                    P.op(eng, lambda e, h=h: e.tensor_copy(out=KT[R, h, k0:k0 + 512], in_=t2[R, :]), [t2], [KT])
                P.op(AC, lambda e: e.activation(out=sqb[:, 0, :], in_=pA[:, :], func=AF.Square), [pA], [sqb])
                pD = next_ps()
                P.op(PE, lambda e: e.matmul(pD[:, :], lhsT=ones_b[:], rhs=sqb[:, 0, :], start=True, stop=True),
                     [ones_b, sqb], [pD])
                rstd_bcast(tmp[0], pD, 1.0 / 128)
                P.op(VE, lambda e: e.scalar_tensor_tensor(out=kvn[:], in0=pA[:, :], scalar=kvg[:, 0:1], in1=tmp[0][:],
                                                          op0=ALU.mult, op1=ALU.mult), [pA, kvg, tmp[0]], [kvn])
                for h in range(NH):
                    pk = next_ps()
                    P.op(PE, lambda e, h=h, pk=pk: e.matmul(pk[0:64, :], lhsT=wuk[:, h * 64:(h + 1) * 64], rhs=kvn[:],
                                                            start=True, stop=True), [wuk, kvn], [pk])
                    if h % 2:
                        P.op(AC, lambda e, h=h, pk=pk: e.copy(out=KT[0:64, h, k0:k0 + 512], in_=pk[0:64, :]),
                             [pk], [KT])
                    else:
                        P.op(VE, lambda e, h=h, pk=pk: e.tensor_copy(out=KT[0:64, h, k0:k0 + 512], in_=pk[0:64, :]),
                             [pk], [KT])
                for tt in range(4):
                    pv = next_ps()
                    kt = kg * 4 + tt
                    P.op(PE, lambda e, tt=tt, pv=pv: e.matmul(pv[:, :], lhsT=kvn[:, tt * 128:(tt + 1) * 128], rhs=wuv[:],
                                                              start=True, stop=True), [kvn, wuv], [pv])
                    if tt % 2:
                        P.op(AC, lambda e, kt=kt, pv=pv: e.copy(
                            out=Vt[:, kt, :, 0:64], in_=pv[:, :].rearrange("p (h d) -> p h d", h=NH)), [pv], [Vt])
                    else:
                        P.op(VE, lambda e, kt=kt, pv=pv: e.tensor_copy(
                            out=Vt[:, kt, :, 0:64], in_=pv[:, :].rearrange("p (h d) -> p h d", h=NH)), [pv], [Vt])
                if not own:
                    continue
                pE = [next_ps(), next_ps()]
                for c in range(2):
                    for k in range(8):
                        P.op(PE, lambda e, c=c, k=k: e.matmul(pE[c][:, :], lhsT=wq[:, k, c * 128:(c + 1) * 128],
                                                              rhs=hTg[:, k, :], start=(k == 0), stop=(k == 7)),
                             [wq, hTg], [pE[c]], sig=(k == 7))
                    P.op(AC, lambda e, c=c: e.activation(out=sqb[:, c, :], in_=pE[c][:, :], func=AF.Square),
                         [pE[c]], [sqb])
                pD = next_ps()
                for c in range(2):
                    P.op(PE, lambda e, c=c: e.matmul(pD[:, :], lhsT=ones_b[:], rhs=sqb[:, c, :],
                                                     start=(c == 0), stop=(c == 1)), [ones_b, sqb], [pD], sig=(c == 1))
                rstd_bcast(tmp[0], pD, 1.0 / 256)
                for c in range(2):
                    P.op(VE, lambda e, c=c: e.scalar_tensor_tensor(
                        out=qn[:, c, :], in0=pE[c][:, :], scalar=qg[:, c:c + 1], in1=tmp[0][:],
                        op0=ALU.mult, op1=ALU.mult), [pE[c], qg, tmp[0]], [qn])
                for h in range(NH):
                    pF, pG = next_ps(), next_ps()
                    for c in range(2):
                        P.op(PE, lambda e, c=c, h=h, pF=pF: e.matmul(
                            pF[0:96, :], lhsT=wuq[:, c, h * 96:(h + 1) * 96], rhs=qn[:, c, :],
                            start=(c == 0), stop=(c == 1)), [wuq, qn], [pF], sig=(c == 1))
                    for c in range(2):
                        P.op(PE, lambda e, c=c, h=h, pG=pG: e.matmul(
                            pG[0:96, :], lhsT=wuqs[:, c, h * 96:(h + 1) * 96], rhs=qn[:, c, :],
                            start=(c == 0), stop=(c == 1)), [wuqs, qn], [pG], sig=(c == 1))
                    P.op(AC, lambda e, h=h, pF=pF: e.copy(out=qT[0:64, h, :], in_=pF[0:64, :]), [pF], [qT])
                    P.op(VE, lambda e, pF=pF: e.tensor_tensor(out=t2[R, :], in0=pF[R, :], in1=cosT[R, :], op=ALU.mult),
                         [pF, cosT], [t2])
                    P.op(VE, lambda e, pG=pG: e.tensor_tensor(out=t3[R, :], in0=pG[R, :], in1=sinT[R, :], op=ALU.mult),
                         [pG, sinT], [t3])
                    P.op(VE, lambda e, h=h: e.tensor_tensor(out=qT[R, h, :], in0=t2[R, :], in1=t3[R, :], op=ALU.add),
                         [t2, t3], [qT])
                pti = 0
                for h in range(NH):
                    pO = next_ps()
                    nkt = (kg + 1) * 4
                    inflight = []

                    def emit_pv(st, pO=pO, h=h, nkt=nkt):
                        kt, qoff, n, pt = st
                        P.op(PE, lambda e: e.matmul(
                            pO[0:65, qoff:512], lhsT=Vt[:, kt, h, :], rhs=pt[:, 0:n],
                            start=(kt == 0), stop=(kt == nkt - 1)), [Vt, pt], [pO], sig=(kt == nkt - 1))

                    for kt in range(nkt):
                        j = kt - kg * 4
                        qoff = max(j, 0) * 128
                        n = 512 - qoff
                        pS = next_ps()
                        if pS is pO:
                            pS = next_ps()
                        P.op(PE, lambda e, h=h, kt=kt, qoff=qoff, n=n, pS=pS: e.matmul(
                            pS[:, 0:n], lhsT=KT[0:96, h, kt * 128:(kt + 1) * 128], rhs=qT[0:96, h, qoff:512],
                            start=True, stop=True), [KT, qT], [pS])
                        pt = PT[pti % 4]
                        pti += 1
                        bias = flg[:, 0:1] if kt < 16 else zero_c[:, 0:1]
                        P.op(AC, lambda e, pt=pt, pS=pS, n=n, bias=bias: e.activation(
                            out=pt[:, 0:n], in_=pS[:, 0:n], func=AF.Exp, bias=bias, scale=SM_SCALE),
                            [pS, flg, zero_c], [pt])
                        if j >= 0:
                            P.op(GP, lambda e, pt=pt: e.memset(pt[64:128, 0:64], 0.0), [], [pt])
                        inflight.append((kt, qoff, n, pt))
                        if len(inflight) > 2:
                            emit_pv(inflight.pop(0))
                    while inflight:
                        emit_pv(inflight.pop(0))
                    P.op(VE, lambda e, pO=pO: e.reciprocal(out=rsum[64:65, :], in_=pO[64:65, :]), [pO], [rsum])
                    pN = next_ps()
                    if pN is pO:
                        pN = next_ps()
                    P.op(PE, lambda e, pN=pN: e.matmul(pN[0:64, :], lhsT=ones_f[64:65, 0:64], rhs=rsum[64:65, :],
                                                       start=True, stop=True), [ones_f, rsum], [pN])
                    P.op(AC, lambda e, pN=pN: e.copy(out=rbc[0:64, :], in_=pN[0:64, :]), [pN], [rbc])
                    P.op(VE, lambda e, h=h, pO=pO, g=g: e.tensor_tensor(
                        out=attnT[0:64, h, g * 512:(g + 1) * 512], in0=pO[0:64, :], in1=rbc[0:64, :], op=ALU.mult),
                        [pO, rbc], [attnT])
            if dbg and "attnT" in dbg:
                for h in range(NH):
                    P.op(VE, lambda e: e.tensor_copy(out=tmp[0][0:64, :], in_=attnT[0:64, h, 0:512]), [attnT], [tmp[0]])
                    P.dma(SY, dbg_aps["attnT"][h * 64:(h + 1) * 64, :], tmp[0][0:64, :], reads=[tmp[0]], sembuf=tmp[0])

        if stop_after == "A":
            P.finish()
            return nc

        with ExitStack() as sbk:
            win = P.sb("win", [128, 8, 3584], BF16, sbk)
            wua = P.sb("wua", [64, NH, D], BF16, sbk)
            wuc = P.sb("wuc", [128, 4, D], BF16, sbk)
            wo = P.sb("wo", [128, 8, D], BF16, sbk)
            cvc = P.sb("cvc", [128, 12], F32, sbk)
            xres = P.sb("xres", [128, 4, D], F32, sbk)
            xn2 = [P.sb("xnb%d" % i, [128, D], BF16, sbk) for i in range(2)]
            ss2 = [P.sb("ssb%d" % i, [128, 1], F32, sbk) for i in range(2)]
            rs2 = [P.sb("rsb%d" % i, [128, 1], F32, sbk) for i in range(2)]
            nrm_i = [0]
            hTg = P.sb("hTgb", [128, 8, 512], BF16, sbk)
            cu = P.sb("cu", [128, 4, 514], F32, sbk)
            bzT = P.sb("bzT", [128, 4, 512], BF16, sbk)
            mT = P.sb("mT", [128, 8, 512], BF16, sbk)
            tb = [P.sb("tmpb%d" % i, [128, 512], F32, sbk) for i in range(6)]
            x1t = [P.sb("x1t%d" % i, [128, D], F32, sbk) for i in range(2)]
            w_in_k = w_in.rearrange("(k p) n -> p k n", p=128)
            for k in range(8):
                P.dma(GP, win[:, k, :], w_in_k[:, k, 416:4000], writes=[win])
            P.dma(GP, wua[:], w_up_attn.rearrange("(h p) n -> p h n", p=64), writes=[wua])
            P.dma(GP, wuc[:], w_up_conv.rearrange("(k p) n -> p k n", p=128), writes=[wuc])
            P.dma(GP, wo[:], w_o.rearrange("(k p) n -> p k n", p=128), writes=[wo])
            P.dma(SY, cvc[:], conv_col[:, :], writes=[cvc])

            def ucb(colbase, ch, rhs_ap, nfree, reads):
                pz = next_ps()
                for k in range(8):
                    P.op(PE, lambda e, k=k, pz=pz: e.matmul(
                        pz[:, 0:nfree], lhsT=win[:, k, colbase + ch * 128:colbase + (ch + 1) * 128],
                        rhs=rhs_ap(k), start=(k == 0), stop=(k == 7)), [win] + reads, [pz], sig=(k == 7))
                return pz

            P.dma(SY, xres[:, 0, :], x_ctx[TOK - 128:TOK, :], writes=[xres])
            def norm_T_res(slot, dst_c0):
                xt = xres
                xn, ss, rs = xn2[nrm_i[0] % 2], ss2[nrm_i[0] % 2], rs2[nrm_i[0] % 2]
                nrm_i[0] += 1
                P.op(VE, lambda e: e.memset(ss[:], 0.0), [], [ss])
                P.op(AC, lambda e: e.activation(out=xn[:], in_=xt[:, slot, :], func=AF.Square, accum_out=ss[:]),
                     [xt, ss], [xn, ss])
                P.op(VE, lambda e: e.tensor_scalar(out=rs[:], in0=ss[:], scalar1=1.0 / D, scalar2=EPS,
                                                   op0=ALU.mult, op1=ALU.add), [ss], [rs])
                P.op(AC, lambda e: e.sqrt(out=rs[:], in_=rs[:]), [rs], [rs]); P.op(VE, lambda e: e.reciprocal(out=rs[:], in_=rs[:]), [rs], [rs])
                P.op(AC, lambda e: e.activation(out=xn[:], in_=xt[:, slot, :], func=AF.Identity, scale=rs[:, 0:1]),
                     [xt, rs], [xn])
                pt = next_ps()
                ptb = pt[:].bitcast(BF16)
                for j in range(8):
                    P.op(PE, lambda e, j=j, ptb=ptb: e.transpose(
                        ptb[:, j * 128:(j + 1) * 128], xn[:, j * 128:(j + 1) * 128], ident_b[:]),
                        [xn, ident_b], [pt], sig=(j == 7))
                for j in range(8):
                    P.op(VE, lambda e, j=j, ptb=ptb: e.tensor_scalar(
                        out=hTg[:, j, dst_c0:dst_c0 + 128], in0=ptb[:, j * 128:(j + 1) * 128],
                        scalar1=scl_m[:, j:j + 1], scalar2=adacol[:, j:j + 1],
                        op0=ALU.mult, op1=ALU.add), [pt, scl_m, adacol], [hTg])

            norm_T_res(0, 0)
            for ch in range(4):
                pu = ucb(0, ch, lambda k: hTg[:, k, 0:128], 128, [hTg])
                P.op(AC, lambda e, pu=pu: e.copy(out=tb[0][:, 0:128], in_=pu[:, 0:128]), [pu], [tb[0]])
                pc = ucb(512, ch, lambda k: hTg[:, k, 0:128], 128, [hTg])
                P.op(VE, lambda e, pc=pc: e.tensor_tensor(out=tb[1][:, 0:128], in0=pc[:, 0:128], in1=tb[0][:, 0:128],
                                                          op=ALU.mult), [pc, tb[0]], [tb[1]])
                P.op(VE, lambda e, ch=ch: e.tensor_scalar(out=cu[:, ch, 512:514], in0=tb[1][:, 126:128],
                                                          scalar1=flg[:, 1:2], scalar2=None, op0=ALU.mult),
                     [tb[1], flg], [cu])

            xo = 0
            for g in range(NG):
                t0 = g * 512
                for tt in range(4):
                    P.dma(SY, xres[:, tt, :], x_own[t0 + tt * 128:t0 + (tt + 1) * 128, :], writes=[xres])
                for tt in range(4):
                    norm_T_res(tt, tt * 128)
                for ch in range(4):
                    P.op(VE, lambda e, ch=ch: e.tensor_copy(out=cu[:, ch, 0:2], in_=cu[:, ch, 512:514]), [cu], [cu])
                    pu = ucb(0, ch, lambda k: hTg[:, k, :], 512, [hTg])
                    P.op(AC, lambda e, pu=pu: e.copy(out=tb[0][:], in_=pu[:, :]), [pu], [tb[0]])
                    pc = ucb(512, ch, lambda k: hTg[:, k, :], 512, [hTg])
                    P.op(VE, lambda e, pc=pc, ch=ch: e.tensor_tensor(out=cu[:, ch, 2:514], in0=pc[:, :], in1=tb[0][:],
                                                                     op=ALU.mult), [pc, tb[0]], [cu])
                    P.op(GP, lambda e, ch=ch: e.tensor_scalar(out=tb[1][:], in0=cu[:, ch, 2:514],
                                                              scalar1=cvc[:, ch * 3 + 2:ch * 3 + 3], scalar2=None,
                                                              op0=ALU.mult), [cu, cvc], [tb[1]])
                    P.op(VE, lambda e, ch=ch: e.scalar_tensor_tensor(out=tb[1][:], in0=cu[:, ch, 1:513],
                                                                     scalar=cvc[:, ch * 3 + 1:ch * 3 + 2], in1=tb[1][:],
                                                                     op0=ALU.mult, op1=ALU.add), [cu, cvc, tb[1]], [tb[1]])
                    P.op(VE, lambda e, ch=ch: e.scalar_tensor_tensor(out=tb[1][:], in0=cu[:, ch, 0:512],
                                                                     scalar=cvc[:, ch * 3:ch * 3 + 1], in1=tb[1][:],
                                                                     op0=ALU.mult, op1=ALU.add), [cu, cvc, tb[1]], [tb[1]])
                    pb = ucb(1024, ch, lambda k: hTg[:, k, :], 512, [hTg])
                    P.op(VE, lambda e, pb=pb, ch=ch: e.tensor_tensor(out=bzT[:, ch, :], in0=pb[:, :], in1=tb[1][:],
                                                                     op=ALU.mult), [pb, tb[1]], [bzT])
                for oc in range(8):
                    pga = ucb(1536, oc, lambda k: hTg[:, k, :], 512, [hTg])
                    P.op(AC, lambda e, pga=pga: e.activation(out=tb[2][:], in_=pga[:, :], func=AF.Sigmoid),
                         [pga], [tb[2]])
                    pgc = ucb(2560, oc, lambda k: hTg[:, k, :], 512, [hTg])
                    P.op(AC, lambda e, pgc=pgc: e.activation(out=tb[3][:], in_=pgc[:, :], func=AF.Sigmoid),
                         [pgc], [tb[3]])
                    pab = next_ps()
                    for h in range(NH):
                        P.op(PE, lambda e, h=h, oc=oc, pab=pab, t0=t0: e.matmul(
                            pab[:, :], lhsT=wua[0:64, h, oc * 128:(oc + 1) * 128], rhs=attnT[0:64, h, t0:t0 + 512],
                            start=(h == 0), stop=(h == NH - 1)), [wua, attnT], [pab], sig=(h == NH - 1))
                    pcb = next_ps()
                    for ch in range(4):
                        P.op(PE, lambda e, ch=ch, oc=oc, pcb=pcb: e.matmul(
                            pcb[:, :], lhsT=wuc[:, ch, oc * 128:(oc + 1) * 128], rhs=bzT[:, ch, :],
                            start=(ch == 0), stop=(ch == 3)), [wuc, bzT], [pcb], sig=(ch == 3))
                    P.op(VE, lambda e, pab=pab: e.tensor_tensor(out=tb[4][:], in0=pab[:, :], in1=tb[2][:], op=ALU.mult),
                         [pab, tb[2]], [tb[4]])
                    P.op(VE, lambda e, pcb=pcb: e.tensor_tensor(out=tb[5][:], in0=pcb[:, :], in1=tb[3][:], op=ALU.mult),
                         [pcb, tb[3]], [tb[5]])
                    P.op(GP, lambda e, oc=oc: e.tensor_tensor(out=mT[:, oc, :], in0=tb[4][:], in1=tb[5][:], op=ALU.add),
                         [tb[4], tb[5]], [mT])
                for tt in range(4):
                    xo_t = x1t[xo % 2]
                    xo += 1
                    for n in range(2):
                        pm = next_ps()
                        for oc in range(8):
                            P.op(PE, lambda e, oc=oc, tt=tt, n=n, pm=pm: e.matmul(
                                pm[:, :], lhsT=mT[:, oc, tt * 128:(tt + 1) * 128], rhs=wo[:, oc, n * 512:(n + 1) * 512],
                                start=(oc == 0), stop=(oc == 7)), [mT, wo], [pm], sig=(oc == 7))
                        P.op(VE, lambda e, pm=pm, n=n, xo_t=xo_t: e.tensor_tensor(
                            out=xo_t[:, n * 512:(n + 1) * 512], in0=pm[:, :], in1=gm_b[:, n * 512:(n + 1) * 512],
                            op=ALU.mult), [pm, gm_b], [xo_t])
                    P.op(GP, lambda e, tt=tt, xo_t=xo_t: e.tensor_tensor(out=xo_t[:], in0=xo_t[:], in1=xres[:, tt, :],
                                                                         op=ALU.add), [xo_t, xres], [xo_t])
                    r0 = t0 + tt * 128
                    P.dma(SY, x1_scr[r0:r0 + 128, :], xo_t[:], reads=[xo_t], writes=[x1d], sembuf=xo_t)
                    if dbg and "x1" in dbg:
                        P.dma(SY, dbg_aps["x1"][r0:r0 + 128, :], xo_t[:], reads=[xo_t], writes=[dbgx], sembuf=dbgx)
        attn_es.close()
        if stop_after == "B":
            P.finish()
            return nc

        with ExitStack() as sc:
            acc = P.sb("acc", [128, NT, D], F32, sc)
            h2T = P.sb("h2T", [128, 8, TOK], BF16, sc)
            gates = P.sb("gates", [128, NT, NE], F32, sc)
            gT = P.sb("gT", [32, 128], F32, sc)
            gatesE = P.sb("gatesE", [128, NE, NT], F32, sc)
            bge = P.sb("bge", [128, 16], F32, sc)
            gce = P.sb("gce", [128, NT], F32, sc)
            rw = P.sb("rw", [128, 8, NE], BF16, sc)
            rb = P.sb("rb", [1, NE], BF16, sc)
            xn = P.sb("xnc", [128, D], BF16, sc)
            ss = P.sb("ssc", [128, 1], F32, sc)
            rs = P.sb("rsc", [128, 1], F32, sc)
            sm = [P.sb("smc%d" % i, [128, 32], F32, sc) for i in range(4)]
            m8 = P.sb("m8", [128, 8], F32, sc)
            wg = [P.sb("wg%d" % i, [128, 8, 512], BF16, sc) for i in range(2)]
            wu = [P.sb("wu%d" % i, [128, 8, 512], BF16, sc) for i in range(2)]
            wd = [P.sb("wd%d" % i, [128, 4, D], BF16, sc) for i in range(2)]
            wst = [P.sb("wst%d" % i, [128, D], F32, sc) for i in range(4)]
            actT = [P.sb("actT%d" % i, [128, 4, 512], BF16, sc) for i in range(2)]
            tcs = [[P.sb("tc%d_%d" % (i, j), [128, 512], F32, sc) for j in range(5)] for i in range(2)]
            ot = wst[0:2]

            P.dma(GP, rw[:], router_w.rearrange("(k p) n -> p k n", p=128), writes=[rw])
            P.dma(GP, rb[:], router_b[:, :], writes=[rb])

            for t in range(NT):
                P.dma(SY, acc[:, t, :], x1_scr[t * 128:(t + 1) * 128, :], reads=[x1d], writes=[acc])
            for t in range(NT):
                r0 = t * 128
                P.op(VE, lambda e: e.memset(ss[:], 0.0), [], [ss])
                P.op(AC, lambda e, t=t: e.activation(out=xn[:], in_=acc[:, t, :], func=AF.Square, accum_out=ss[:]),
                     [acc, ss], [xn, ss])
                P.op(VE, lambda e: e.tensor_scalar(out=rs[:], in0=ss[:], scalar1=1.0 / D, scalar2=EPS,
                                                   op0=ALU.mult, op1=ALU.add), [ss], [rs])
                P.op(AC, lambda e: e.sqrt(out=rs[:], in_=rs[:]), [rs], [rs]); P.op(VE, lambda e: e.reciprocal(out=rs[:], in_=rs[:]), [rs], [rs])
                P.op(AC, lambda e, t=t: e.activation(out=xn[:], in_=acc[:, t, :], func=AF.Identity, scale=rs[:, 0:1]),
                     [acc, rs], [xn])
                pt = next_ps()
                ptb = pt[:].bitcast(BF16)
                for j in range(8):
                    P.op(PE, lambda e, j=j, ptb=ptb: e.transpose(
                        ptb[:, j * 128:(j + 1) * 128], xn[:, j * 128:(j + 1) * 128], ident_b[:]),
                        [xn, ident_b], [pt], sig=(j == 7))
                for j in range(8):
                    P.op(VE, lambda e, j=j, ptb=ptb, r0=r0: e.tensor_scalar(
                        out=h2T[:, j, r0:r0 + 128], in0=ptb[:, j * 128:(j + 1) * 128],
                        scalar1=scl_f[:, j:j + 1], scalar2=adacol[:, 16 + j:17 + j],
                        op0=ALU.mult, op1=ALU.add), [pt, scl_f, adacol], [h2T])
                pl = next_ps()
                for k in range(8):
                    P.op(PE, lambda e, k=k, r0=r0, pl=pl: e.matmul(
                        pl[:, 0:NE], lhsT=h2T[:, k, r0:r0 + 128], rhs=rw[:, k, :], start=(k == 0), stop=False),
                        [h2T, rw], [pl], sig=False)
                P.op(PE, lambda e, pl=pl: e.matmul(pl[:, 0:NE], lhsT=ones_b[0:1, 0:128], rhs=rb[0:1, :],
                                                   start=False, stop=True), [ones_b, rb], [pl])
                lg, ex, mk, _ = sm
                P.op(VE, lambda e, pl=pl: e.tensor_copy(out=lg[:], in_=pl[:, 0:NE]), [pl], [lg])
                P.op(VE, lambda e: e.max(out=m8[:], in_=lg[:]), [lg], [m8])
                P.op(VE, lambda e: e.tensor_scalar(out=mk[:], in0=lg[:], scalar1=m8[:, 3:4], scalar2=None,
                                                   op0=ALU.is_ge), [lg, m8], [mk])
                P.op(VE, lambda e: e.tensor_scalar(out=lg[:], in0=lg[:], scalar1=m8[:, 0:1], scalar2=None,
                                                   op0=ALU.subtract), [lg, m8], [lg])
                P.op(AC, lambda e: e.activation(out=ex[:], in_=lg[:], func=AF.Exp), [lg], [ex])
                P.op(VE, lambda e: e.tensor_tensor(out=ex[:], in0=ex[:], in1=mk[:], op=ALU.mult), [ex, mk], [ex])
                P.op(VE, lambda e: e.reduce_sum(out=ss[:], in_=ex[:], axis=mybir.AxisListType.X), [ex], [ss])
                P.op(VE, lambda e: e.reciprocal(out=rs[:], in_=ss[:]), [ss], [rs])
                P.op(VE, lambda e, t=t: e.tensor_scalar(out=gates[:, t, :], in0=ex[:], scalar1=rs[:, 0:1], scalar2=None,
                                                        op0=ALU.mult), [ex, rs], [gates])
                P.op(GP, lambda e, t=t: e.tensor_copy(out=gatesE[:, :, t], in_=gates[:, t, :]), [gates], [gatesE])
            if dbg and "gates" in dbg:
                P.dma(SY, dbg_aps["gates"][:, :], gates[:].rearrange("p t e -> p (t e)"), reads=[gates], sembuf=gates)

            if acc_in:
                for t in range(NT):
                    P.dma(SY, acc[:, t, :], acc_prev[t * 128:(t + 1) * 128, :], writes=[acc])
            P.dma(SY, g_scr[:, :], gatesE[:].rearrange("p e t -> p (e t)"), reads=[gatesE], writes=[gsd], sembuf=gatesE)
            w_gu_k = w_gu.rearrange("e (k p) n -> e p k n", p=128)
            w_dn_k = w_down.rearrange("e (k p) n -> e p k n", p=128)
            cnts = {"it": 0, "st": 0, "gi": 0}

            pend = [None]

            def down_stage(g, b, aT):
                for tt in range(4):
                    t = g * 4 + tt
                    for n in range(2):
                        pY = next_ps()
                        for fc in range(4):
                            P.op(PE, lambda e, fc=fc, tt=tt, n=n, b=b, pY=pY, aT=aT: e.matmul(
                                pY[:, :], lhsT=aT[:, fc, tt * 128:(tt + 1) * 128],
                                rhs=wd[b][:, fc, n * 512:(n + 1) * 512],
                                start=(fc == 0), stop=(fc == 3)), [aT, wd[b]], [pY], sig=(fc == 3))
                        P.op(VE, lambda e, pY=pY, t=t, n=n: e.scalar_tensor_tensor(
                            out=acc[:, t, n * 512:(n + 1) * 512], in0=pY[:, :],
                            scalar=gce[:, t:t + 1], in1=acc[:, t, n * 512:(n + 1) * 512],
                            op0=ALU.mult, op1=ALU.add), [pY, gce, acc], [acc])

            def expert_iter(ex_s):
                def esel(i):
                    return ex_s if i is None else i

                P.dma(SY, bge[:], lambda i: bgu_col[:, bass.ds(esel(i) * 16, 16)], writes=[bge])
                P.dma(SY, gce[:], lambda i: g_scr[:, bass.ds(esel(i) * NT, NT)], reads=[gsd], writes=[gce])
                for hf in range(2):
                    b = cnts["it"] % 2
                    cnts["it"] += 1
                    P.dma(GP, wg[b][:], lambda i, hf=hf: w_gu_k[bass.ds(esel(i), 1), :, :, hf * 512:(hf + 1) * 512]
                          .rearrange("o p k n -> p (o k) n"), writes=[wg[b]])
                    P.dma(GP, wu[b][:], lambda i, hf=hf: w_gu_k[bass.ds(esel(i), 1), :, :, D + hf * 512:D + (hf + 1) * 512]
                          .rearrange("o p k n -> p (o k) n"), writes=[wu[b]])
                    for fc in range(4):
                        st = wst[cnts["st"] % 4]
                        cnts["st"] += 1
                        P.dma(SY, st[:], lambda i, hf=hf, fc=fc: w_dn_k[bass.ds(esel(i), 1), :, hf * 4 + fc, :]
                              .rearrange("o p n -> p (o n)"), writes=[st])
                        P.op(GP, lambda e, st=st, b=b, fc=fc: e.tensor_tensor(out=wd[b][:, fc, :], in0=st[:], in1=gf_b[:],
                                                                              op=ALU.mult), [st, gf_b], [wd[b]])
                    for g in range(NG):
                        t0 = g * 512
                        aT = actT[cnts["gi"] % 2]
                        tc = tcs[cnts["gi"] % 2]
                        cnts["gi"] += 1
                        for fc in range(4):
                            cg = hf * 4 + fc
                            cuu = 8 + hf * 4 + fc
                            pG, pU = next_ps(), next_ps()
                            for k in range(8):
                                P.op(PE, lambda e, k=k, fc=fc, b=b, pG=pG, t0=t0: e.matmul(
                                    pG[:, :], lhsT=wg[b][:, k, fc * 128:(fc + 1) * 128], rhs=h2T[:, k, t0:t0 + 512],
                                    start=(k == 0), stop=(k == 7)), [wg[b], h2T], [pG], sig=(k == 7))
                            for k in range(8):
                                P.op(PE, lambda e, k=k, fc=fc, b=b, pU=pU, t0=t0: e.matmul(
                                    pU[:, :], lhsT=wu[b][:, k, fc * 128:(fc + 1) * 128], rhs=h2T[:, k, t0:t0 + 512],
                                    start=(k == 0), stop=(k == 7)), [wu[b], h2T], [pU], sig=(k == 7))
                            gcl, sg_, ub, uc, gs = tc
                            P.op(VE, lambda e, pG=pG, cg=cg, gcl=gcl: e.tensor_scalar(
                                out=gcl[:], in0=pG[:, :], scalar1=bge[:, cg:cg + 1], scalar2=7.0,
                                op0=ALU.add, op1=ALU.min), [pG, bge], [gcl])
                            P.op(AC, lambda e, gcl=gcl, sg_=sg_: e.activation(out=sg_[:], in_=gcl[:], func=AF.Sigmoid,
                                                                              scale=1.702), [gcl], [sg_])
                            P.op(AC, lambda e, pU=pU, cuu=cuu, ub=ub: e.activation(
                                out=ub[:], in_=pU[:, :], func=AF.Identity, bias=bge[:, cuu:cuu + 1]), [pU, bge], [ub])
                            P.op(GP, lambda e, ub=ub, uc=uc: e.tensor_scalar(out=uc[:], in0=ub[:], scalar1=7.0, scalar2=-7.0,
                                                                             op0=ALU.min, op1=ALU.max), [ub], [uc])
                            P.op(GP, lambda e, gcl=gcl, sg_=sg_, gs=gs: e.tensor_tensor(out=gs[:], in0=gcl[:], in1=sg_[:],
                                                                                        op=ALU.mult), [gcl, sg_], [gs])
                            P.op(VE, lambda e, uc=uc, gs=gs, aT=aT, fc=fc: e.scalar_tensor_tensor(
                                out=aT[:, fc, :], in0=uc[:], scalar=1.0, in1=gs[:], op0=ALU.add, op1=ALU.mult),
                                [uc, gs], [aT])
                        if pend[0] is not None:
                            down_stage(*pend[0])
                        pend[0] = (g, b, aT)
                if pend[0] is not None:
                    down_stage(*pend[0])
                    pend[0] = None

            e_end = e_off + n_experts
            if n_experts <= 4:
                for ex_i in range(e_off, e_end):
                    expert_iter(ex_i)
            else:
                expert_iter(e_off)
                expert_iter(e_off + 1)
                m_a = P.mark()
                expert_iter(e_off + 2)
                m_b = P.mark()
                expert_iter(e_off + 3)
                P.make_loop(m_a, m_b, e_off + 3, e_end)

            if final:
                last = None
                gfin, bd = wst[2], wst[3]
                P.dma(SY, gfin[:], gfin_b[:, :], writes=[gfin])
                P.dma(SY, bd[0:32, :], b_down[:, :], writes=[bd])
                P.op(VE, lambda e: e.tensor_tensor(out=bd[0:32, :], in0=bd[0:32, :], in1=gf_b[0:32, :], op=ALU.mult),
                     [bd, gf_b], [bd])
                for t in range(NT):
                    r0 = t * 128
                    pg = next_ps()
                    P.op(PE, lambda e, t=t, pg=pg: e.transpose(pg[0:32, 0:128], gates[:, t, :], ident_f[:]),
                         [gates, ident_f], [pg])
                    P.op(VE, lambda e, pg=pg: e.tensor_copy(out=gT[:], in_=pg[0:32, 0:128]), [pg], [gT])
                    for n in range(2):
                        pbd = next_ps()
                        P.op(PE, lambda e, n=n, pbd=pbd: e.matmul(
                            pbd[:, :], lhsT=gT[0:32, :], rhs=bd[0:32, n * 512:(n + 1) * 512],
                            start=True, stop=True), [gT, bd], [pbd])
                        P.op(VE, lambda e, t=t, n=n, pbd=pbd: e.tensor_tensor(
                            out=acc[:, t, n * 512:(n + 1) * 512], in0=pbd[:, :], in1=acc[:, t, n * 512:(n + 1) * 512],
                            op=ALU.add), [pbd, acc], [acc])
                    P.op(VE, lambda e: e.memset(ss[:], 0.0), [], [ss])
                    P.op(AC, lambda e, t=t: e.activation(out=xn[:], in_=acc[:, t, :], func=AF.Square, accum_out=ss[:]),
                         [acc, ss], [xn, ss])
                    P.op(VE, lambda e: e.tensor_scalar(out=rs[:], in0=ss[:], scalar1=1.0 / D, scalar2=EPS,
                                                       op0=ALU.mult, op1=ALU.add), [ss], [rs])
                    P.op(AC, lambda e: e.sqrt(out=rs[:], in_=rs[:]), [rs], [rs]); P.op(VE, lambda e: e.reciprocal(out=rs[:], in_=rs[:]), [rs], [rs])
                    o = ot[t % 2]
                    P.op(VE, lambda e, t=t, o=o: e.scalar_tensor_tensor(out=o[:], in0=acc[:, t, :], scalar=rs[:, 0:1],
                                                                        in1=gfin[:], op0=ALU.mult, op1=ALU.mult),
                         [acc, rs, gfin], [o])
                    last = P.dma(SY, y[r0:r0 + 128, :], o[:], reads=[o], writes=[yd], sembuf=o)
            else:
                for t in range(NT):
                    r0 = t * 128
                    o = ot[t % 2]
                    P.op(GP, lambda e, t=t, o=o: e.tensor_copy(out=o[:], in_=acc[:, t, :]), [acc], [o])
                    P.dma(SY, y[r0:r0 + 128, :], o[:], reads=[o], writes=[yd], sembuf=o)
            for o in wst:
                P.wait_tok(SY, Tok(o.dsem, o.dcnt))
        P.finish()
    return nc


def _host_layout(inputs):
    f = np.float32
    x = np.asarray(inputs["x"], f)
    c = np.asarray(inputs["c"], f)
    pos = np.asarray(inputs["positions"], np.int32)
    w_in = np.ascontiguousarray(np.asarray(inputs["w_in"], f)[0])
    w_uq = np.asarray(inputs["w_uq"], f)[0]
    w_ukv = np.asarray(inputs["w_ukv"], f)[0]
    col = lambda v, n: np.ascontiguousarray(np.asarray(v, f).reshape(n, 128).T)
    w_kpe_sw = np.ascontiguousarray(np.concatenate([w_in[:, 320:384], w_in[:, 400:416], w_in[:, 384:400]], axis=1))
    wq3 = w_uq.reshape(256, NH, 96)
    w_uq_sw = np.ascontiguousarray(
        np.concatenate([wq3[:, :, 0:64], wq3[:, :, 80:96], wq3[:, :, 64:80]], axis=2).reshape(256, 768))
    wkv3 = w_ukv.reshape(128, NH, 128)
    w_uk = np.ascontiguousarray(wkv3[:, :, 0:64].reshape(128, 512))
    w_uv = np.ascontiguousarray(wkv3[:, :, 64:128].reshape(128, 512))
    conv_w = np.asarray(inputs["conv_w"], f)[0]
    conv_col = np.ascontiguousarray(conv_w.reshape(3, 4, 128).transpose(2, 1, 0).reshape(128, 12))
    b_gu = np.asarray(inputs["b_gu"], f)[0]
    bgu_col = np.ascontiguousarray(b_gu.reshape(NE, 16, 128).transpose(2, 0, 1).reshape(128, NE * 16))
    inv_freq = (1.0 / (10000.0 ** (np.arange(0, 32, 2, dtype=np.float32) / np.float32(32)))).astype(f)
    rope_c = np.zeros((128, 2), f)
    for p in range(64, 96):
        rope_c[p, 0] = inv_freq[(p - 64) % 16]
        rope_c[p, 1] = -1.0 if p < 80 else 1.0
    shared = {
        "w_ada": np.ascontiguousarray(np.asarray(inputs["w_ada"], f)[0]),
        "b_ada": np.ascontiguousarray(np.asarray(inputs["b_ada"], f)[0].reshape(1, -1)),
        "gmix_col": col(np.asarray(inputs["norm_mix_g"])[0], 8),
        "gffn_col": col(np.asarray(inputs["norm_ffn_g"])[0], 8),
        "w_in": w_in,
        "w_kpe_sw": w_kpe_sw,
        "qg_col": col(np.asarray(inputs["q_norm_g"])[0], 2),
        "kvg_col": col(np.asarray(inputs["kv_norm_g"])[0], 1),
        "w_uq": np.ascontiguousarray(w_uq),
        "w_uq_sw": w_uq_sw,
        "w_uk": w_uk,
        "w_uv": w_uv,
        "w_up_attn": np.ascontiguousarray(np.asarray(inputs["w_up_attn"], f)[0]),
        "conv_col": conv_col,
        "w_up_conv": np.ascontiguousarray(np.asarray(inputs["w_up_conv"], f)[0]),
        "w_o": np.ascontiguousarray(np.asarray(inputs["w_o"], f)[0]),
        "router_w": np.ascontiguousarray(np.asarray(inputs["router_w"], f)[0]),
        "router_b": np.ascontiguousarray(np.asarray(inputs["router_b"], f)[0].reshape(1, -1)),
        "w_gu": np.ascontiguousarray(np.asarray(inputs["w_gu"], f)[0]),
        "bgu_col": bgu_col,
        "w_down": np.ascontiguousarray(np.asarray(inputs["w_down"], f)[0]),
        "b_down": np.ascontiguousarray(np.asarray(inputs["b_down"], f)[0]),
        "gfin_b": np.ascontiguousarray(np.broadcast_to(np.asarray(inputs["norm_final_g"], f)[None, :], (128, D))),
        "rope_c": rope_c,
        "ident": np.eye(128, dtype=f),
    }
    in_maps = []
    for i in range(8):
        b, half = i // 2, i % 2
        own = slice(half * TOK, (half + 1) * TOK)
        flags = np.zeros((128, 2), f)
        flags[:, 0] = 0.0 if half == 1 else NEG
        flags[:, 1] = 1.0 if half == 1 else 0.0
        m = dict(shared)
        m["x_own"] = np.ascontiguousarray(x[b, own])
        m["x_ctx"] = np.ascontiguousarray(x[b, 0:TOK])
        m["pos_all"] = np.ascontiguousarray(np.concatenate([pos[b, 0:TOK], pos[b, own]]).reshape(1, SEQ))
        m["flags"] = flags
        m["c_col"] = col(c[b], 8)
        in_maps.append(m)
    return in_maps


def kernel(**inputs):
    in_maps = _host_layout(inputs)
    nc = build_program()
    res = run_bass_kernel_spmd(nc, in_maps, core_ids=list(range(8)))
    out = np.zeros((4, SEQ, D), np.float32)
    for i in range(8):
        b, half = i // 2, i % 2
        out[b, half * TOK:(half + 1) * TOK] = res.results[i]["y"]
    return out
```

```python
import numpy as np
from contextlib import ExitStack
import concourse.bass as bass
import concourse.mybir as mybir
from concourse.bass_utils import run_bass_kernel_spmd

F32 = mybir.dt.float32
BF16 = mybir.dt.bfloat16
I32 = mybir.dt.int32
AF = mybir.ActivationFunctionType
ALU = mybir.AluOpType

D = 1024
SEQ = 4096
TOK = 2048
NT = TOK // 128
NG = TOK // 512
NH = 8
NE = 32
EPS = 1e-6
TWO_PI = float(2 * np.pi)
CW1 = 6.28125
CW2 = float(2 * np.pi - 6.28125)
SM_SCALE = float(96 ** -0.5)
NEG = -30000.0

ENGS = ("sync", "scalar", "gpsimd", "vector", "tensor")


class Tok:
    __slots__ = ("sem", "val")

    def __init__(self, sem, val):
        self.sem = sem
        self.val = val


class Buf:
    def __init__(self, name, h):
        self.name = name
        self.h = h
        self.last_w = None
        self.reads = {}
        self.dsem = None
        self.dcnt = 0

    def __getitem__(self, idx):
        return self.h[idx]


class _Rec:
    def __init__(self):
        self.call = None

    def __getattr__(self, name):
        def f(*a, **k):
            assert self.call is None
            self.call = (name, a, k)
            return self
        return f


class Prog:
    def __init__(self, nc, es):
        self.nc = nc
        self.es = es
        self.streams = {e: [] for e in ENGS}
        self.cnt = {e: 0 for e in ENGS}
        self.pending = {e: False for e in ENGS}
        self.esem = {e: es.enter_context(nc.semaphore("c_" + e)) for e in ENGS}
        self.waited = {e: {} for e in ENGS}
        self.nsem = len(ENGS)
        self.fence = {}
        self.dbufs = {}
        self.allbufs = []

    def sb(self, name, shape, dt, es=None):
        h = (es or self.es).enter_context(self.nc.sbuf_tensor(name, list(shape), dt))
        b = Buf(name, h)
        self.allbufs.append(b)
        b.reads = dict(self.fence)
        if es is not None:
            es.callback(self.release, [b])
        return b

    def release(self, bufs):
        for b in bufs:
            toks = list(b.reads.values()) + ([b.last_w] if b.last_w is not None else [])
            for t in toks:
                k = id(t.sem)
                if k not in self.fence or self.fence[k].val < t.val:
                    self.fence[k] = t

    def ps(self, name, shape, dt, es=None):
        h = (es or self.es).enter_context(self.nc.psum_tensor(name, list(shape), dt))
        b = Buf(name, h)
        self.allbufs.append(b)
        return b

    def dram(self, name, h):
        b = Buf(name, h)
        self.allbufs.append(b)
        return b

    def _dsem(self, b):
        if b.dsem is None:
            b.dsem = self.es.enter_context(self.nc.semaphore("d_" + b.name))
            self.nsem += 1
        return b.dsem

    def _deps(self, eng, reads, writes):
        deps = []
        for b in reads:
            if b.last_w is not None:
                deps.append(b.last_w)
        for b in writes:
            if b.last_w is not None:
                deps.append(b.last_w)
            deps.extend(b.reads.values())
        out = {}
        for t in deps:
            if t.sem is self.esem[eng] and eng == "tensor":
                continue
            k = id(t.sem)
            if self.waited[eng].get(k, 0) >= t.val:
                continue
            if k not in out or out[k].val < t.val:
                out[k] = t
        for k, t in out.items():
            self.waited[eng][k] = t.val
        return list(out.values())

    def _record(self, tok, reads, writes):
        for b in reads:
            b.reads[id(tok.sem)] = tok
        for b in writes:
            b.last_w = tok
            b.reads = {}

    def _push(self, eng, deps, act):
        self.streams[eng].append({"deps": [[t.sem, t.val, 0] for t in deps], "act": act})

    def op(self, eng, fn, reads=(), writes=(), sig=True):
        deps = self._deps(eng, reads, writes)
        sem = self.esem[eng]
        if sig:
            self.cnt[eng] += 1
            self.pending[eng] = False
            tok = Tok(sem, self.cnt[eng])
        else:
            self.pending[eng] = True
            tok = Tok(sem, self.cnt[eng] + 1)
        rec = _Rec()
        fn(rec)
        name, a, k = rec.call

        def act(e, i, name=name, a=a, k=k, sig=sig, sem=sem):
            ins = getattr(e, name)(*a, **k)
            if sig:
                ins.then_inc(sem, 1)

        self._push(eng, deps, act)
        self._record(tok, reads, writes)

    def dma(self, eng, out, in_, reads=(), writes=(), sembuf=None):
        deps = self._deps(eng, reads, writes)
        sb = sembuf or (writes[0] if writes else reads[0])
        sem = self._dsem(sb)
        sb.dcnt += 16
        tok = Tok(sem, sb.dcnt)
        self.dbufs[id(sb)] = sb

        def act(e, i, out=out, in_=in_, sem=sem):
            src = in_(i) if callable(in_) else in_
            e.dma_start(out=out, in_=src).then_inc(sem, 16)

        self._push(eng, deps, act)
        self._record(tok, reads, writes)
        return tok

    def wait_tok(self, eng, tok):
        self._push(eng, [tok], lambda e, i: None)

    def mark(self):
        return {"len": {e: len(self.streams[e]) for e in ENGS},
                "cnt": dict(self.cnt),
                "dcnt": {k: b.dcnt for k, b in self.dbufs.items()}}

    def make_loop(self, m_a, m_b, it_b, it_end):
        extra = it_end - it_b - 1
        for e in ENGS:
            a0, b0, b1 = m_a["len"][e], m_b["len"][e], len(self.streams[e])
            A, B = self.streams[e][a0:b0], self.streams[e][b0:b1]
            assert len(A) == len(B), (e, len(A), len(B))
            for x, y_ in zip(A, B):
                assert len(x["deps"]) == len(y_["deps"]), (e, x["deps"], y_["deps"])
                for dx, dy in zip(x["deps"], y_["deps"]):
                    assert dx[0] is dy[0], e
                    dy[2] = dy[1] - dx[1]
                    assert dy[2] >= 0
            self.streams[e][b0:b1] = [{"loop": (it_b, it_end), "body": B}]
        shift = {}
        for e in ENGS:
            d = self.cnt[e] - m_b["cnt"][e]
            assert d == m_b["cnt"][e] - m_a["cnt"][e], e
            shift[id(self.esem[e])] = (m_b["cnt"][e], d * extra)
            self.cnt[e] += d * extra
        for k, b in self.dbufs.items():
            d = b.dcnt - m_b["dcnt"].get(k, 0)
            assert d == m_b["dcnt"].get(k, 0) - m_a["dcnt"].get(k, 0), b.name
            if d:
                shift[id(b.dsem)] = (m_b["dcnt"].get(k, 0), d * extra)
                b.dcnt += d * extra
        seen = set()

        def bump(t):
            if id(t) in seen:
                return
            seen.add(id(t))
            sh = shift.get(id(t.sem))
            if sh and t.val > sh[0]:
                t.val += sh[1]

        for b in self.allbufs:
            if b.last_w is not None:
                bump(b.last_w)
            for t in b.reads.values():
                bump(t)
        for t in self.fence.values():
            bump(t)
        for e in ENGS:
            for k, v in list(self.waited[e].items()):
                sh = shift.get(k)
                if sh and v > sh[0]:
                    self.waited[e][k] = v + sh[1]

    def finish(self):
        nc = self.nc
        for e in ENGS:
            assert not self.pending[e], e

        def run(eng_name, e):
            def emit(item, bases, tmp, it0):
                for sem, val, dv in item["deps"]:
                    if bases is not None and dv:
                        e.reg_add(tmp, bases[dv], val - it0 * dv)
                        e.wait_ge(sem, tmp)
                    else:
                        e.wait_ge(sem, val)

            for item in self.streams[eng_name]:
                if "loop" in item:
                    it0, it1 = item["loop"]
                    dvs = sorted({d[2] for sub in item["body"] for d in sub["deps"] if d[2]})
                    tmp = e.alloc_register("wtmp")
                    bases = {dv: e.alloc_register("wb%d" % dv) for dv in dvs}
                    with e.Fori(it0, it1) as i:
                        for dv, r in bases.items():
                            e.reg_mul(r, i, dv)
                        for sub in item["body"]:
                            emit(sub, bases, tmp, it0)
                            sub["act"](e, i)
                else:
                    emit(item, None, None, 0)
                    item["act"](e, None)

        with nc.Block() as block:
            @block.sync
            def _(e):
                run("sync", e)

            @block.scalar
            def _(e):
                run("scalar", e)

            @block.gpsimd
            def _(e):
                run("gpsimd", e)

            @block.vector
            def _(e):
                run("vector", e)

            @block.tensor
            def _(e):
                run("tensor", e)


def build_program(dbg=None, n_experts=NE, stop_after=None, e_off=0, acc_in=False, final=True):
    nc = bass.Bass("TRN2", target_bir_lowering=False)

    def din(name, shape, dt=F32):
        return nc.dram_tensor(name, list(shape), dt, kind="ExternalInput").ap()

    x_own = din("x_own", [TOK, D])
    x_ctx = din("x_ctx", [TOK, D])
    pos_all = din("pos_all", [1, SEQ], I32)
    flags = din("flags", [128, 2])
    c_col = din("c_col", [128, 8])
    w_ada = din("w_ada", [D, 6 * D])
    b_ada = din("b_ada", [1, 6 * D])
    gmix_col = din("gmix_col", [128, 8])
    gffn_col = din("gffn_col", [128, 8])
    w_in = din("w_in", [D, 4000])
    w_kpe_sw = din("w_kpe_sw", [D, 96])
    qg_col = din("qg_col", [128, 2])
    kvg_col = din("kvg_col", [128, 1])
    w_uq = din("w_uq", [256, 768])
    w_uq_sw = din("w_uq_sw", [256, 768])
    w_uk = din("w_uk", [128, 512])
    w_uv = din("w_uv", [128, 512])
    w_up_attn = din("w_up_attn", [512, D])
    conv_col = din("conv_col", [128, 12])
    w_up_conv = din("w_up_conv", [512, D])
    w_o = din("w_o", [D, D])
    router_w = din("router_w", [D, NE])
    router_b = din("router_b", [1, NE])
    w_gu = din("w_gu", [NE, D, 2 * D])
    bgu_col = din("bgu_col", [128, NE * 16])
    w_down = din("w_down", [NE, D, D])
    b_down = din("b_down", [NE, D])
    gfin_b = din("gfin_b", [128, D])
    rope_c = din("rope_c", [128, 2])
    ident_in = din("ident", [128, 128])
    y = nc.dram_tensor("y", [TOK, D], F32, kind="ExternalOutput").ap()
    acc_prev = din("acc_prev", [TOK, D]) if acc_in else None
    x1_scr = nc.dram_tensor("x1_scr", [TOK, D], F32, kind="Internal").ap()
    g_scr = nc.dram_tensor("g_scr", [128, NE * NT], F32, kind="Internal").ap()
    dbg_aps = {}
    if dbg:
        for k, shp in dbg.items():
            dbg_aps[k] = nc.dram_tensor("dbg_" + k, list(shp), F32, kind="ExternalOutput").ap()

    es = ExitStack()
    with es:
        P = Prog(nc, es)
        SY, AC, GP, VE, PE = "sync", "scalar", "gpsimd", "vector", "tensor"
        x1d = P.dram("x1d", None)
        gsd = P.dram("gsd", None)
        dbgx = P.dram("dbgx", None)
        yd = P.dram("yd", None)

        psb = [P.ps("ps%d" % i, [128, 512], F32) for i in range(8)]
        ps_rr = [0]

        def next_ps():
            b = psb[ps_rr[0] % 8]
            ps_rr[0] += 1
            return b

        ident_f = P.sb("ident_f", [128, 128], F32)
        ident_b = P.sb("ident_b", [128, 128], BF16)
        ones_f = P.sb("ones_f", [128, 128], F32)
        ones_b = P.sb("ones_b", [128, 128], BF16)
        zero_c = P.sb("zero_c", [128, 1], F32)
        flg = P.sb("flg", [128, 2], F32)
        adacol = P.sb("adacol", [128, 32], F32)
        scl_m = P.sb("scl_m", [128, 8], F32)
        scl_f = P.sb("scl_f", [128, 8], F32)
        gm_b = P.sb("gm_b", [128, D], F32)
        gf_b = P.sb("gf_b", [128, D], F32)

        P.dma(SY, ident_f[:], ident_in[:, :], writes=[ident_f])
        P.dma(SY, flg[:], flags[:, :], writes=[flg])
        P.op(VE, lambda e: e.tensor_copy(out=ident_b[:], in_=ident_f[:]), [ident_f], [ident_b])
        P.op(VE, lambda e: e.memset(ones_f[:], 1.0), [], [ones_f])
        P.op(VE, lambda e: e.memset(ones_b[:], 1.0), [], [ones_b])
        P.op(VE, lambda e: e.memset(zero_c[:], 0.0), [], [zero_c])

        with ExitStack() as s0:
            ccol = P.sb("ccol", [128, 8], F32, s0)
            cact = P.sb("cact", [128, 8], F32, s0)
            gmc = P.sb("gmc", [128, 8], F32, s0)
            gfc = P.sb("gfc", [128, 8], F32, s0)
            ada_row = P.sb("ada_row", [1, 6 * D], F32, s0)
            bada = P.sb("bada", [1, 6 * D], F32, s0)
            wa = [P.sb("wa%d" % i, [128, 3072], F32, s0) for i in range(4)]
            P.dma(SY, ccol[:], c_col[:, :], writes=[ccol])
            P.dma(SY, gmc[:], gmix_col[:, :], writes=[gmc])
            P.dma(SY, gfc[:], gffn_col[:, :], writes=[gfc])
            P.dma(SY, bada[:], b_ada[:, :], writes=[bada])
            P.op(AC, lambda e: e.activation(out=cact[:], in_=ccol[:], func=AF.Silu), [ccol], [cact])
            it = 0
            for hh in range(2):
                banks = [next_ps() for _ in range(6)]
                for k in range(8):
                    w = wa[it % 4]
                    it += 1
                    P.dma(SY, w[:], w_ada[k * 128:(k + 1) * 128, hh * 3072:(hh + 1) * 3072], writes=[w])
                    for n in range(6):
                        P.op(PE, lambda e, b=banks[n], w=w, k=k, n=n: e.matmul(
                            b[0:1, :], lhsT=cact[:, k:k + 1], rhs=w[:, n * 512:(n + 1) * 512],
                            start=(k == 0), stop=(k == 7)),
                            [cact, w], [banks[n]], sig=True)
                for n in range(6):
                    c0 = hh * 3072 + n * 512
                    P.op(VE, lambda e, b=banks[n], c0=c0: e.tensor_tensor(
                        out=ada_row[0:1, c0:c0 + 512], in0=b[0:1, :], in1=bada[0:1, c0:c0 + 512], op=ALU.add),
                        [banks[n], bada], [ada_row])
            pcol = next_ps()
            segs = [0, 1, 3, 4]
            for si, sg in enumerate(segs):
                for j in range(8):
                    c0 = sg * D + j * 128
                    idx = si * 8 + j
                    P.op(PE, lambda e, c0=c0, idx=idx: e.matmul(
                        pcol[:, idx:idx + 1], lhsT=ada_row[0:1, c0:c0 + 128], rhs=ones_f[0:1, 0:1],
                        start=True, stop=True), [ada_row, ones_f], [pcol])
            P.op(VE, lambda e: e.tensor_copy(out=adacol[:], in_=pcol[:, 0:32]), [pcol], [adacol])
            P.op(VE, lambda e: e.scalar_tensor_tensor(out=scl_m[:], in0=adacol[:, 8:16], scalar=1.0, in1=gmc[:],
                                                      op0=ALU.add, op1=ALU.mult), [adacol, gmc], [scl_m])
            P.op(VE, lambda e: e.scalar_tensor_tensor(out=scl_f[:], in0=adacol[:, 24:32], scalar=1.0, in1=gfc[:],
                                                      op0=ALU.add, op1=ALU.mult), [adacol, gfc], [scl_f])
            for sg, dst in ((2, gm_b), (5, gf_b)):
                for n in range(2):
                    pb = next_ps()
                    c0 = sg * D + n * 512
                    P.op(PE, lambda e, pb=pb, c0=c0: e.matmul(
                        pb[:, :], lhsT=ones_f[0:1, 0:128], rhs=ada_row[0:1, c0:c0 + 512],
                        start=True, stop=True), [ada_row, ones_f], [pb])
                    P.op(VE, lambda e, pb=pb, dst=dst, n=n: e.tensor_copy(
                        out=dst[:, n * 512:(n + 1) * 512], in_=pb[:, :]), [pb], [dst])
            if dbg and "ada" in dbg:
                P.dma(SY, dbg_aps["ada"][0:1, :], ada_row[0:1, :], reads=[ada_row], sembuf=ada_row)
            if dbg and "gm_b" in dbg:
                P.dma(SY, dbg_aps["gm_b"][:, :], gm_b[:], reads=[gm_b], sembuf=gm_b)
            if dbg and "adacol" in dbg:
                P.dma(SY, dbg_aps["adacol"][:, :], adacol[:], reads=[adacol], sembuf=adacol)

        if stop_after == "0":
            P.finish()
            return nc

        def norm_T(xt, xn, ss, rs, scl, shcol_off, dst, dst_c0):
            P.op(VE, lambda e: e.memset(ss[:], 0.0), [], [ss])
            P.op(AC, lambda e: e.activation(out=xn[:], in_=xt[:], func=AF.Square, accum_out=ss[:]),
                 [xt, ss], [xn, ss])
            P.op(VE, lambda e: e.tensor_scalar(out=rs[:], in0=ss[:], scalar1=1.0 / D, scalar2=EPS,
                                               op0=ALU.mult, op1=ALU.add), [ss], [rs])
            P.op(AC, lambda e: e.sqrt(out=rs[:], in_=rs[:]), [rs], [rs]); P.op(VE, lambda e: e.reciprocal(out=rs[:], in_=rs[:]), [rs], [rs])
            P.op(AC, lambda e: e.activation(out=xn[:], in_=xt[:], func=AF.Identity, scale=rs[:, 0:1]),
                 [xt, rs], [xn])
            pt = next_ps()
            ptb = pt[:].bitcast(BF16)
            for j in range(8):
                P.op(PE, lambda e, j=j, ptb=ptb: e.transpose(
                    ptb[:, j * 128:(j + 1) * 128], xn[:, j * 128:(j + 1) * 128], ident_b[:]),
                    [xn, ident_b], [pt], sig=(j == 7))
            for j in range(8):
                P.op(VE, lambda e, j=j, ptb=ptb: e.tensor_scalar(
                    out=dst[:, j, dst_c0:dst_c0 + 128], in0=ptb[:, j * 128:(j + 1) * 128],
                    scalar1=scl[:, j:j + 1], scalar2=adacol[:, shcol_off + j:shcol_off + j + 1],
                    op0=ALU.mult, op1=ALU.add), [pt, scl, adacol], [dst])

        def rstd_bcast(dst, src_ps, inv_n):
            P.op(VE, lambda e: e.tensor_scalar(out=dst[:], in0=src_ps[:], scalar1=inv_n, scalar2=EPS,
                                               op0=ALU.mult, op1=ALU.add), [src_ps], [dst])
            P.op(AC, lambda e: e.sqrt(out=dst[:], in_=dst[:]), [dst], [dst]); P.op(VE, lambda e: e.reciprocal(out=dst[:], in_=dst[:]), [dst], [dst])

        attn_es = ExitStack()
        es.enter_context(attn_es)
        attnT = P.sb("attnT", [128, NH, TOK], BF16, attn_es)

        with ExitStack() as sa:
            KT = P.sb("KT", [128, NH, SEQ], BF16, sa)
            Vt = P.sb("Vt", [128, 32, NH, 65], BF16, sa)
            wkv = P.sb("wkv", [128, 8, 160], BF16, sa)
            wks = P.sb("wks", [128, 8, 96], BF16, sa)
            wq = P.sb("wq", [128, 8, 256], BF16, sa)
            wuq = P.sb("wuq", [128, 2, 768], BF16, sa)
            wuqs = P.sb("wuqs", [128, 2, 768], BF16, sa)
            wuk = P.sb("wuk", [128, 512], BF16, sa)
            wuv = P.sb("wuv", [128, 512], BF16, sa)
            qg = P.sb("qg", [128, 2], F32, sa)
            kvg = P.sb("kvg", [128, 1], F32, sa)
            rpc = P.sb("rpc", [128, 2], F32, sa)
            hTg = P.sb("hTg", [128, 8, 512], BF16, sa)
            xts = [P.sb("xta%d" % i, [128, D], F32, sa) for i in range(2)]
            xn2 = [P.sb("xna%d" % i, [128, D], BF16, sa) for i in range(2)]
            ss2 = [P.sb("ssa%d" % i, [128, 1], F32, sa) for i in range(2)]
            rs2 = [P.sb("rsa%d" % i, [128, 1], F32, sa) for i in range(2)]
            posi = P.sb("posi", [128, 512], I32, sa)
            ang = P.sb("ang", [128, 512], F32, sa)
            cosT = P.sb("cosT", [128, 512], F32, sa)
            sinT = P.sb("sinT", [128, 512], F32, sa)
            tmp = [P.sb("tmpa%d" % i, [128, 512], F32, sa) for i in range(4)]
            sqb = P.sb("sqb", [128, 2, 512], BF16, sa)
            kvn = P.sb("kvn", [128, 512], BF16, sa)
            qn = P.sb("qn", [128, 2, 512], BF16, sa)
            qT = P.sb("qT", [128, NH, 512], BF16, sa)
            PT = [P.sb("PT%d" % i, [128, 512], BF16, sa) for i in range(4)]
            rsum, rbc = tmp[0], tmp[1]

            w_in_k = w_in.rearrange("(k p) n -> p k n", p=128)
            P.dma(GP, wkv[:], w_in_k[:, :, 256:416], writes=[wkv])
            P.dma(GP, wks[:], w_kpe_sw.rearrange("(k p) n -> p k n", p=128), writes=[wks])
            P.dma(GP, wq[:], w_in_k[:, :, 0:256], writes=[wq])
            P.dma(GP, wuq[:], w_uq.rearrange("(k p) n -> p k n", p=128), writes=[wuq])
            P.dma(GP, wuqs[:], w_uq_sw.rearrange("(k p) n -> p k n", p=128), writes=[wuqs])
            P.dma(GP, wuk[:], w_uk[:, :], writes=[wuk])
            P.dma(GP, wuv[:], w_uv[:, :], writes=[wuv])
            P.dma(SY, qg[:], qg_col[:, :], writes=[qg])
            P.dma(SY, kvg[:], kvg_col[:, :], writes=[kvg])
            P.dma(SY, rpc[:], rope_c[:, :], writes=[rpc])
            P.op(GP, lambda e: e.memset(Vt[:], 1.0), [], [Vt])

            R = slice(64, 96)

            def range_reduce_sin(dst, src, shift):
                t0, t1 = tmp[0], tmp[1]
                if shift != 0.0:
                    P.op(VE, lambda e: e.tensor_scalar(out=t1[R, :], in0=src[R, :], scalar1=shift, scalar2=None,
                                                       op0=ALU.add), [src], [t1])
                    a = t1
                else:
                    a = src
                P.op(VE, lambda e: e.tensor_scalar(out=t0[R, :], in0=a[R, :], scalar1=1.0 / TWO_PI, scalar2=None,
                                                   op0=ALU.mult), [a], [t0])
                P.op(VE, lambda e: e.tensor_copy(out=posi[R, :], in_=t0[R, :]), [t0], [posi])
                P.op(VE, lambda e: e.tensor_copy(out=t0[R, :], in_=posi[R, :]), [posi], [t0])
                P.op(VE, lambda e: e.scalar_tensor_tensor(out=dst[R, :], in0=t0[R, :], scalar=-CW1, in1=a[R, :],
                                                          op0=ALU.mult, op1=ALU.add), [t0, a], [dst])
                P.op(VE, lambda e: e.scalar_tensor_tensor(out=dst[R, :], in0=t0[R, :], scalar=-CW2, in1=dst[R, :],
                                                          op0=ALU.mult, op1=ALU.add), [t0, dst], [dst])
                P.op(VE, lambda e: e.tensor_scalar(out=t0[R, :], in0=dst[R, :], scalar1=float(np.pi),
                                                   scalar2=-TWO_PI, op0=ALU.is_gt, op1=ALU.mult), [dst], [t0])
                P.op(VE, lambda e: e.tensor_tensor(out=dst[R, :], in0=dst[R, :], in1=t0[R, :], op=ALU.add),
                     [dst, t0], [dst])
                P.op(VE, lambda e: e.tensor_scalar(out=t0[R, :], in0=dst[R, :], scalar1=-float(np.pi),
                                                   scalar2=TWO_PI, op0=ALU.is_lt, op1=ALU.mult), [dst], [t0])
                P.op(VE, lambda e: e.tensor_tensor(out=dst[R, :], in0=dst[R, :], in1=t0[R, :], op=ALU.add),
                     [dst, t0], [dst])
                P.op(AC, lambda e: e.activation(out=dst[R, :], in_=dst[R, :], func=AF.Sin), [dst], [dst])

            xi = 0
            for kg in range(8):
                own = kg >= 4
                g = kg - 4
                src = x_own if own else x_ctx
                row0 = (kg % 4) * 512
                k0 = kg * 512
                for tt in range(4):
                    xt = xts[xi % 2]
                    xi += 1
                    r0 = row0 + tt * 128
                    P.dma(SY, xt[:], src[r0:r0 + 128, :], writes=[xt])
                    norm_T(xt, xn2[xi % 2], ss2[xi % 2], rs2[xi % 2], scl_m, 0, hTg, tt * 128)
                pA, pB, pC = next_ps(), next_ps(), next_ps()
                for k in range(8):
                    P.op(PE, lambda e, k=k: e.matmul(pA[:, :], lhsT=wkv[:, k, 0:128], rhs=hTg[:, k, :],
                                                     start=(k == 0), stop=(k == 7)), [wkv, hTg], [pA], sig=(k == 7))
                for k in range(8):
                    P.op(PE, lambda e, k=k: e.matmul(pB[0:96, :], lhsT=wkv[:, k, 64:160], rhs=hTg[:, k, :],
                                                     start=(k == 0), stop=(k == 7)), [wkv, hTg], [pB], sig=(k == 7))
                for k in range(8):
                    P.op(PE, lambda e, k=k: e.matmul(pC[0:96, :], lhsT=wks[:, k, :], rhs=hTg[:, k, :],
                                                     start=(k == 0), stop=(k == 7)), [wks, hTg], [pC], sig=(k == 7))
                P.dma(SY, posi[R, :], pos_all[0:1, k0:k0 + 512].broadcast_to([32, 512]), writes=[posi])
                P.op(VE, lambda e: e.tensor_copy(out=ang[R, :], in_=posi[R, :]), [posi], [ang])
                P.op(VE, lambda e: e.tensor_scalar(out=ang[R, :], in0=ang[R, :], scalar1=rpc[R, 0:1], scalar2=None,
                                                   op0=ALU.mult), [ang, rpc], [ang])
                range_reduce_sin(sinT, ang, 0.0)
                range_reduce_sin(cosT, ang, float(np.pi / 2))
                P.op(VE, lambda e: e.tensor_scalar(out=sinT[R, :], in0=sinT[R, :], scalar1=rpc[R, 1:2], scalar2=None,
                                                   op0=ALU.mult), [sinT, rpc], [sinT])
                t2, t3 = tmp[2], tmp[3]
                P.op(VE, lambda e: e.tensor_tensor(out=t2[R, :], in0=pB[R, :], in1=cosT[R, :], op=ALU.mult),
                     [pB, cosT], [t2])
                P.op(VE, lambda e: e.tensor_tensor(out=t3[R, :], in0=pC[R, :], in1=sinT[R, :], op=ALU.mult),
                     [pC, sinT], [t3])
                P.op(VE, lambda e: e.tensor_tensor(out=t2[R, :], in0=t2[R, :], in1=t3[R, :], op=ALU.add),
                     [t2, t3], [t2])
                for h in range(NH):
                    eng = GP if h % 2 else VE
                    P.op(eng, lambda e, h=h: e.tensor_copy(out=KT[R, h, k0:k0 + 512], in_=t2[R, :]), [t2], [KT])
                P.op(AC, lambda e: e.activation(out=sqb[:, 0, :], in_=pA[:, :], func=AF.Square), [pA], [sqb])
                pD = next_ps()
                P.op(PE, lambda e: e.matmul(pD[:, :], lhsT=ones_b[:], rhs=sqb[:, 0, :], start=True, stop=True),
                     [ones_b, sqb], [pD])
                rstd_bcast(tmp[0], pD, 1.0 / 128)
                P.op(VE, lambda e: e.scalar_tensor_tensor(out=kvn[:], in0=pA[:, :], scalar=kvg[:, 0:1], in1=tmp[0][:],
                                                          op0=ALU.mult, op1=ALU.mult), [pA, kvg, tmp[0]], [kvn])
                for h in range(NH):
                    pk = next_ps()
                    P.op(PE, lambda e, h=h, pk=pk: e.matmul(pk[0:64, :], lhsT=wuk[:, h * 64:(h + 1) * 64], rhs=kvn[:],
                                                            start=True, stop=True), [wuk, kvn], [pk])
                    if h % 2:
                        P.op(AC, lambda e, h=h, pk=pk: e.copy(out=KT[0:64, h, k0:k0 + 512], in_=pk[0:64, :]),
                             [pk], [KT])
                    else:
                        P.op(VE, lambda e, h=h, pk=pk: e.tensor_copy(out=KT[0:64, h, k0:k0 + 512], in_=pk[0:64, :]),
                             [pk], [KT])
                for tt in range(4):
                    pv = next_ps()
                    kt = kg * 4 + tt
                    P.op(PE, lambda e, tt=tt, pv=pv: e.matmul(pv[:, :], lhsT=kvn[:, tt * 128:(tt + 1) * 128], rhs=wuv[:],
                                                              start=True, stop=True), [kvn, wuv], [pv])
                    if tt % 2:
                        P.op(AC, lambda e, kt=kt, pv=pv: e.copy(
                            out=Vt[:, kt, :, 0:64], in_=pv[:, :].rearrange("p (h d) -> p h d", h=NH)), [pv], [Vt])
                    else:
                        P.op(VE, lambda e, kt=kt, pv=pv: e.tensor_copy(
                            out=Vt[:, kt, :, 0:64], in_=pv[:, :].rearrange("p (h d) -> p h d", h=NH)), [pv], [Vt])
                if not own:
                    continue
                pE = [next_ps(), next_ps()]
                for c in range(2):
                    for k in range(8):
                        P.op(PE, lambda e, c=c, k=k: e.matmul(pE[c][:, :], lhsT=wq[:, k, c * 128:(c + 1) * 128],
                                                              rhs=hTg[:, k, :], start=(k == 0), stop=(k == 7)),
                             [wq, hTg], [pE[c]], sig=(k == 7))
                    P.op(AC, lambda e, c=c: e.activation(out=sqb[:, c, :], in_=pE[c][:, :], func=AF.Square),
                         [pE[c]], [sqb])
                pD = next_ps()
                for c in range(2):
                    P.op(PE, lambda e, c=c: e.matmul(pD[:, :], lhsT=ones_b[:], rhs=sqb[:, c, :],
                                                     start=(c == 0), stop=(c == 1)), [ones_b, sqb], [pD], sig=(c == 1))
                rstd_bcast(tmp[0], pD, 1.0 / 256)
                for c in range(2):
                    P.op(VE, lambda e, c=c: e.scalar_tensor_tensor(
                        out=qn[:, c, :], in0=pE[c][:, :], scalar=qg[:, c:c + 1], in1=tmp[0][:],
                        op0=ALU.mult, op1=ALU.mult), [pE[c], qg, tmp[0]], [qn])
                for h in range(NH):
                    pF, pG = next_ps(), next_ps()
                    for c in range(2):
                        P.op(PE, lambda e, c=c, h=h, pF=pF: e.matmul(
                            pF[0:96, :], lhsT=wuq[:, c, h * 96:(h + 1) * 96], rhs=qn[:, c, :],
                            start=(c == 0), stop=(c == 1)), [wuq, qn], [pF], sig=(c == 1))
                    for c in range(2):
                        P.op(PE, lambda e, c=c, h=h, pG=pG: e.matmul(
                            pG[0:96, :], lhsT=wuqs[:, c, h * 96:(h + 1) * 96], rhs=qn[:, c, :],
                            start=(c == 0), stop=(c == 1)), [wuqs, qn], [pG], sig=(c == 1))
                    P.op(AC, lambda e, h=h, pF=pF: e.copy(out=qT[0:64, h, :], in_=pF[0:64, :]), [pF], [qT])
                    P.op(VE, lambda e, pF=pF: e.tensor_tensor(out=t2[R, :], in0=pF[R, :], in1=cosT[R, :], op=ALU.mult),
                         [pF, cosT], [t2])
                    P.op(VE, lambda e, pG=pG: e.tensor_tensor(out=t3[R, :], in0=pG[R, :], in1=sinT[R, :], op=ALU.mult),
                         [pG, sinT], [t3])
                    P.op(VE, lambda e, h=h: e.tensor_tensor(out=qT[R, h, :], in0=t2[R, :], in1=t3[R, :], op=ALU.add),
                         [t2, t3], [qT])
                pti = 0
                for h in range(NH):
                    pO = next_ps()
                    nkt = (kg + 1) * 4
                    inflight = []

                    def emit_pv(st, pO=pO, h=h, nkt=nkt):
                        kt, qoff, n, pt = st
                        P.op(PE, lambda e: e.matmul(
                            pO[0:65, qoff:512], lhsT=Vt[:, kt, h, :], rhs=pt[:, 0:n],
                            start=(kt == 0), stop=(kt == nkt - 1)), [Vt, pt], [pO], sig=(kt == nkt - 1))

                    for kt in range(nkt):
                        j = kt - kg * 4
                        qoff = max(j, 0) * 128
                        n = 512 - qoff
                        pS = next_ps()
                        if pS is pO:
                            pS = next_ps()
                        P.op(PE, lambda e, h=h, kt=kt, qoff=qoff, n=n, pS=pS: e.matmul(
                            pS[:, 0:n], lhsT=KT[0:96, h, kt * 128:(kt + 1) * 128], rhs=qT[0:96, h, qoff:512],
                            start=True, stop=True), [KT, qT], [pS])
                        pt = PT[pti % 4]
                        pti += 1
                        bias = flg[:, 0:1] if kt < 16 else zero_c[:, 0:1]
                        P.op(AC, lambda e, pt=pt, pS=pS, n=n, bias=bias: e.activation(
                            out=pt[:, 0:n], in_=pS[:, 0:n], func=AF.Exp, bias=bias, scale=SM_SCALE),
                            [pS, flg, zero_c], [pt])
                        if j >= 0:
                            P.op(GP, lambda e, pt=pt: e.memset(pt[64:128, 0:64], 0.0), [], [pt])
                        inflight.append((kt, qoff, n, pt))
                        if len(inflight) > 2:
                            emit_pv(inflight.pop(0))
                    while inflight:
                        emit_pv(inflight.pop(0))
                    P.op(VE, lambda e, pO=pO: e.reciprocal(out=rsum[64:65, :], in_=pO[64:65, :]), [pO], [rsum])
                    pN = next_ps()
                    if pN is pO:
                        pN = next_ps()
                    P.op(PE, lambda e, pN=pN: e.matmul(pN[0:64, :], lhsT=ones_f[64:65, 0:64], rhs=rsum[64:65, :],
                                                       start=True, stop=True), [ones_f, rsum], [pN])
                    P.op(AC, lambda e, pN=pN: e.copy(out=rbc[0:64, :], in_=pN[0:64, :]), [pN], [rbc])
                    P.op(VE, lambda e, h=h, pO=pO, g=g: e.tensor_tensor(
                        out=attnT[0:64, h, g * 512:(g + 1) * 512], in0=pO[0:64, :], in1=rbc[0:64, :], op=ALU.mult),
                        [pO, rbc], [attnT])
            if dbg and "attnT" in dbg:
                for h in range(NH):
                    P.op(VE, lambda e: e.tensor_copy(out=tmp[0][0:64, :], in_=attnT[0:64, h, 0:512]), [attnT], [tmp[0]])
                    P.dma(SY, dbg_aps["attnT"][h * 64:(h + 1) * 64, :], tmp[0][0:64, :], reads=[tmp[0]], sembuf=tmp[0])

        if stop_after == "A":
            P.finish()
            return nc

        with ExitStack() as sbk:
            win = P.sb("win", [128, 8, 3584], BF16, sbk)
            wua = P.sb("wua", [64, NH, D], BF16, sbk)
            wuc = P.sb("wuc", [128, 4, D], BF16, sbk)
            wo = P.sb("wo", [128, 8, D], BF16, sbk)
            cvc = P.sb("cvc", [128, 12], F32, sbk)
            xres = P.sb("xres", [128, 4, D], F32, sbk)
            xn2 = [P.sb("xnb%d" % i, [128, D], BF16, sbk) for i in range(2)]
            ss2 = [P.sb("ssb%d" % i, [128, 1], F32, sbk) for i in range(2)]
            rs2 = [P.sb("rsb%d" % i, [128, 1], F32, sbk) for i in range(2)]
            nrm_i = [0]
            hTg = P.sb("hTgb", [128, 8, 512], BF16, sbk)
            cu = P.sb("cu", [128, 4, 514], F32, sbk)
            bzT = P.sb("bzT", [128, 4, 512], BF16, sbk)
            mT = P.sb("mT", [128, 8, 512], BF16, sbk)
            tb = [P.sb("tmpb%d" % i, [128, 512], F32, sbk) for i in range(6)]
            x1t = [P.sb("x1t%d" % i, [128, D], F32, sbk) for i in range(2)]
            w_in_k = w_in.rearrange("(k p) n -> p k n", p=128)
            for k in range(8):
                P.dma(GP, win[:, k, :], w_in_k[:, k, 416:4000], writes=[win])
            P.dma(GP, wua[:], w_up_attn.rearrange("(h p) n -> p h n", p=64), writes=[wua])
            P.dma(GP, wuc[:], w_up_conv.rearrange("(k p) n -> p k n", p=128), writes=[wuc])
            P.dma(GP, wo[:], w_o.rearrange("(k p) n -> p k n", p=128), writes=[wo])
            P.dma(SY, cvc[:], conv_col[:, :], writes=[cvc])

            def ucb(colbase, ch, rhs_ap, nfree, reads):
                pz = next_ps()
                for k in range(8):
                    P.op(PE, lambda e, k=k, pz=pz: e.matmul(
                        pz[:, 0:nfree], lhsT=win[:, k, colbase + ch * 128:colbase + (ch + 1) * 128],
                        rhs=rhs_ap(k), start=(k == 0), stop=(k == 7)), [win] + reads, [pz], sig=(k == 7))
                return pz

            P.dma(SY, xres[:, 0, :], x_ctx[TOK - 128:TOK, :], writes=[xres])
            def norm_T_res(slot, dst_c0):
                xt = xres
                xn, ss, rs = xn2[nrm_i[0] % 2], ss2[nrm_i[0] % 2], rs2[nrm_i[0] % 2]
                nrm_i[0] += 1
                P.op(VE, lambda e: e.memset(ss[:], 0.0), [], [ss])
                P.op(AC, lambda e: e.activation(out=xn[:], in_=xt[:, slot, :], func=AF.Square, accum_out=ss[:]),
                     [xt, ss], [xn, ss])
                P.op(VE, lambda e: e.tensor_scalar(out=rs[:], in0=ss[:], scalar1=1.0 / D, scalar2=EPS,
                                                   op0=ALU.mult, op1=ALU.add), [ss], [rs])
                P.op(AC, lambda e: e.sqrt(out=rs[:], in_=rs[:]), [rs], [rs]); P.op(VE, lambda e: e.reciprocal(out=rs[:], in_=rs[:]), [rs], [rs])
                P.op(AC, lambda e: e.activation(out=xn[:], in_=xt[:, slot, :], func=AF.Identity, scale=rs[:, 0:1]),
                     [xt, rs], [xn])
                pt = next_ps()
                ptb = pt[:].bitcast(BF16)
                for j in range(8):
                    P.op(PE, lambda e, j=j, ptb=ptb: e.transpose(
                        ptb[:, j * 128:(j + 1) * 128], xn[:, j * 128:(j + 1) * 128], ident_b[:]),
                        [xn, ident_b], [pt], sig=(j == 7))
                for j in range(8):
                    P.op(VE, lambda e, j=j, ptb=ptb: e.tensor_scalar(
                        out=hTg[:, j, dst_c0:dst_c0 + 128], in0=ptb[:, j * 128:(j + 1) * 128],
                        scalar1=scl_m[:, j:j + 1], scalar2=adacol[:, j:j + 1],
                        op0=ALU.mult, op1=ALU.add), [pt, scl_m, adacol], [hTg])

            norm_T_res(0, 0)
            for ch in range(4):
                pu = ucb(0, ch, lambda k: hTg[:, k, 0:128], 128, [hTg])
                P.op(AC, lambda e, pu=pu: e.copy(out=tb[0][:, 0:128], in_=pu[:, 0:128]), [pu], [tb[0]])
                pc = ucb(512, ch, lambda k: hTg[:, k, 0:128], 128, [hTg])
                P.op(VE, lambda e, pc=pc: e.tensor_tensor(out=tb[1][:, 0:128], in0=pc[:, 0:128], in1=tb[0][:, 0:128],
                                                          op=ALU.mult), [pc, tb[0]], [tb[1]])
                P.op(VE, lambda e, ch=ch: e.tensor_scalar(out=cu[:, ch, 512:514], in0=tb[1][:, 126:128],
                                                          scalar1=flg[:, 1:2], scalar2=None, op0=ALU.mult),
                     [tb[1], flg], [cu])

            xo = 0
            for g in range(NG):
                t0 = g * 512
                for tt in range(4):
                    P.dma(SY, xres[:, tt, :], x_own[t0 + tt * 128:t0 + (tt + 1) * 128, :], writes=[xres])
                for tt in range(4):
                    norm_T_res(tt, tt * 128)
                for ch in range(4):
                    P.op(VE, lambda e, ch=ch: e.tensor_copy(out=cu[:, ch, 0:2], in_=cu[:, ch, 512:514]), [cu], [cu])
                    pu = ucb(0, ch, lambda k: hTg[:, k, :], 512, [hTg])
                    P.op(AC, lambda e, pu=pu: e.copy(out=tb[0][:], in_=pu[:, :]), [pu], [tb[0]])
                    pc = ucb(512, ch, lambda k: hTg[:, k, :], 512, [hTg])
                    P.op(VE, lambda e, pc=pc, ch=ch: e.tensor_tensor(out=cu[:, ch, 2:514], in0=pc[:, :], in1=tb[0][:],
                                                                     op=ALU.mult), [pc, tb[0]], [cu])
                    P.op(GP, lambda e, ch=ch: e.tensor_scalar(out=tb[1][:], in0=cu[:, ch, 2:514],
                                                              scalar1=cvc[:, ch * 3 + 2:ch * 3 + 3], scalar2=None,
                                                              op0=ALU.mult), [cu, cvc], [tb[1]])
                    P.op(VE, lambda e, ch=ch: e.scalar_tensor_tensor(out=tb[1][:], in0=cu[:, ch, 1:513],
                                                                     scalar=cvc[:, ch * 3 + 1:ch * 3 + 2], in1=tb[1][:],
                                                                     op0=ALU.mult, op1=ALU.add), [cu, cvc, tb[1]], [tb[1]])
                    P.op(VE, lambda e, ch=ch: e.scalar_tensor_tensor(out=tb[1][:], in0=cu[:, ch, 0:512],
                                                                     scalar=cvc[:, ch * 3:ch * 3 + 1], in1=tb[1][:],
                                                                     op0=ALU.mult, op1=ALU.add), [cu, cvc, tb[1]], [tb[1]])
                    pb = ucb(1024, ch, lambda k: hTg[:, k, :], 512, [hTg])
                    P.op(VE, lambda e, pb=pb, ch=ch: e.tensor_tensor(out=bzT[:, ch, :], in0=pb[:, :], in1=tb[1][:],
                                                                     op=ALU.mult), [pb, tb[1]], [bzT])
                for oc in range(8):
                    pga = ucb(1536, oc, lambda k: hTg[:, k, :], 512, [hTg])
                    P.op(AC, lambda e, pga=pga: e.activation(out=tb[2][:], in_=pga[:, :], func=AF.Sigmoid),
                         [pga], [tb[2]])
                    pgc = ucb(2560, oc, lambda k: hTg[:, k, :], 512, [hTg])
                    P.op(AC, lambda e, pgc=pgc: e.activation(out=tb[3][:], in_=pgc[:, :], func=AF.Sigmoid),
                         [pgc], [tb[3]])
                    pab = next_ps()
                    for h in range(NH):
                        P.op(PE, lambda e, h=h, oc=oc, pab=pab, t0=t0: e.matmul(
                            pab[:, :], lhsT=wua[0:64, h, oc * 128:(oc + 1) * 128], rhs=attnT[0:64, h, t0:t0 + 512],
                            start=(h == 0), stop=(h == NH - 1)), [wua, attnT], [pab], sig=(h == NH - 1))
                    pcb = next_ps()
                    for ch in range(4):
                        P.op(PE, lambda e, ch=ch, oc=oc, pcb=pcb: e.matmul(
                            pcb[:, :], lhsT=wuc[:, ch, oc * 128:(oc + 1) * 128], rhs=bzT[:, ch, :],
                            start=(ch == 0), stop=(ch == 3)), [wuc, bzT], [pcb], sig=(ch == 3))
                    P.op(VE, lambda e, pab=pab: e.tensor_tensor(out=tb[4][:], in0=pab[:, :], in1=tb[2][:], op=ALU.mult),
                         [pab, tb[2]], [tb[4]])
                    P.op(VE, lambda e, pcb=pcb: e.tensor_tensor(out=tb[5][:], in0=pcb[:, :], in1=tb[3][:], op=ALU.mult),
                         [pcb, tb[3]], [tb[5]])
                    P.op(GP, lambda e, oc=oc: e.tensor_tensor(out=mT[:, oc, :], in0=tb[4][:], in1=tb[5][:], op=ALU.add),
                         [tb[4], tb[5]], [mT])
                for tt in range(4):
                    xo_t = x1t[xo % 2]
                    xo += 1
                    for n in range(2):
                        pm = next_ps()
                        for oc in range(8):
                            P.op(PE, lambda e, oc=oc, tt=tt, n=n, pm=pm: e.matmul(
                                pm[:, :], lhsT=mT[:, oc, tt * 128:(tt + 1) * 128], rhs=wo[:, oc, n * 512:(n + 1) * 512],
                                start=(oc == 0), stop=(oc == 7)), [mT, wo], [pm], sig=(oc == 7))
                        P.op(VE, lambda e, pm=pm, n=n, xo_t=xo_t: e.tensor_tensor(
                            out=xo_t[:, n * 512:(n + 1) * 512], in0=pm[:, :], in1=gm_b[:, n * 512:(n + 1) * 512],
                            op=ALU.mult), [pm, gm_b], [xo_t])
                    P.op(GP, lambda e, tt=tt, xo_t=xo_t: e.tensor_tensor(out=xo_t[:], in0=xo_t[:], in1=xres[:, tt, :],
                                                                         op=ALU.add), [xo_t, xres], [xo_t])
                    r0 = t0 + tt * 128
                    P.dma(SY, x1_scr[r0:r0 + 128, :], xo_t[:], reads=[xo_t], writes=[x1d], sembuf=xo_t)
                    if dbg and "x1" in dbg:
                        P.dma(SY, dbg_aps["x1"][r0:r0 + 128, :], xo_t[:], reads=[xo_t], writes=[dbgx], sembuf=dbgx)
        attn_es.close()
        if stop_after == "B":
            P.finish()
            return nc

        with ExitStack() as sc:
            acc = P.sb("acc", [128, NT, D], F32, sc)
            h2T = P.sb("h2T", [128, 8, TOK], BF16, sc)
            gates = P.sb("gates", [128, NT, NE], F32, sc)
            gT = P.sb("gT", [32, 128], F32, sc)
            gatesE = P.sb("gatesE", [128, NE, NT], F32, sc)
            bge = P.sb("bge", [128, 16], F32, sc)
            bgu1 = P.sb("bgu1", [128, 8], F32, sc)
            gce = P.sb("gce", [128, NT], F32, sc)
            rw = P.sb("rw", [128, 8, NE], BF16, sc)
            rb = P.sb("rb", [1, NE], BF16, sc)
            xn = P.sb("xnc", [128, D], BF16, sc)
            ss = P.sb("ssc", [128, 1], F32, sc)
            rs = P.sb("rsc", [128, 1], F32, sc)
            sm = [P.sb("smc%d" % i, [128, 32], F32, sc) for i in range(4)]
            m8 = P.sb("m8", [128, 8], F32, sc)
            wg = [P.sb("wg%d" % i, [128, 8, 512], BF16, sc) for i in range(2)]
            wu = [P.sb("wu%d" % i, [128, 8, 512], BF16, sc) for i in range(2)]
            wd = [P.sb("wd%d" % i, [128, 4, D], BF16, sc) for i in range(2)]
            wst = [P.sb("wst%d" % i, [128, D], F32, sc) for i in range(4)]
            actT = [P.sb("actT%d" % i, [128, 4, 512], BF16, sc) for i in range(2)]
            tcs = [[P.sb("tc%d_%d" % (i, j), [128, 512], F32, sc) for j in range(5)] for i in range(2)]
            ot = wst[0:2]

            P.dma(GP, rw[:], router_w.rearrange("(k p) n -> p k n", p=128), writes=[rw])
            P.dma(GP, rb[:], router_b[:, :], writes=[rb])

            for t in range(NT):
                P.dma(SY, acc[:, t, :], x1_scr[t * 128:(t + 1) * 128, :], reads=[x1d], writes=[acc])
            for t in range(NT):
                r0 = t * 128
                P.op(VE, lambda e: e.memset(ss[:], 0.0), [], [ss])
                P.op(AC, lambda e, t=t: e.activation(out=xn[:], in_=acc[:, t, :], func=AF.Square, accum_out=ss[:]),
                     [acc, ss], [xn, ss])
                P.op(VE, lambda e: e.tensor_scalar(out=rs[:], in0=ss[:], scalar1=1.0 / D, scalar2=EPS,
                                                   op0=ALU.mult, op1=ALU.add), [ss], [rs])
                P.op(AC, lambda e: e.sqrt(out=rs[:], in_=rs[:]), [rs], [rs]); P.op(VE, lambda e: e.reciprocal(out=rs[:], in_=rs[:]), [rs], [rs])
                P.op(AC, lambda e, t=t: e.activation(out=xn[:], in_=acc[:, t, :], func=AF.Identity, scale=rs[:, 0:1]),
                     [acc, rs], [xn])
                pt = next_ps()
                ptb = pt[:].bitcast(BF16)
                for j in range(8):
                    P.op(PE, lambda e, j=j, ptb=ptb: e.transpose(
                        ptb[:, j * 128:(j + 1) * 128], xn[:, j * 128:(j + 1) * 128], ident_b[:]),
                        [xn, ident_b], [pt], sig=(j == 7))
                for j in range(8):
                    P.op(VE, lambda e, j=j, ptb=ptb, r0=r0: e.tensor_scalar(
                        out=h2T[:, j, r0:r0 + 128], in0=ptb[:, j * 128:(j + 1) * 128],
                        scalar1=scl_f[:, j:j + 1], scalar2=adacol[:, 16 + j:17 + j],
                        op0=ALU.mult, op1=ALU.add), [pt, scl_f, adacol], [h2T])
                pl = next_ps()
                for k in range(8):
                    P.op(PE, lambda e, k=k, r0=r0, pl=pl: e.matmul(
                        pl[:, 0:NE], lhsT=h2T[:, k, r0:r0 + 128], rhs=rw[:, k, :], start=(k == 0), stop=False),
                        [h2T, rw], [pl], sig=False)
                P.op(PE, lambda e, pl=pl: e.matmul(pl[:, 0:NE], lhsT=ones_b[0:1, 0:128], rhs=rb[0:1, :],
                                                   start=False, stop=True), [ones_b, rb], [pl])
                lg, ex, mk, _ = sm
                P.op(VE, lambda e, pl=pl: e.tensor_copy(out=lg[:], in_=pl[:, 0:NE]), [pl], [lg])
                P.op(VE, lambda e: e.max(out=m8[:], in_=lg[:]), [lg], [m8])
                P.op(VE, lambda e: e.tensor_scalar(out=mk[:], in0=lg[:], scalar1=m8[:, 3:4], scalar2=None,
                                                   op0=ALU.is_ge), [lg, m8], [mk])
                P.op(VE, lambda e: e.tensor_scalar(out=lg[:], in0=lg[:], scalar1=m8[:, 0:1], scalar2=None,
                                                   op0=ALU.subtract), [lg, m8], [lg])
                P.op(AC, lambda e: e.activation(out=ex[:], in_=lg[:], func=AF.Exp), [lg], [ex])
                P.op(VE, lambda e: e.tensor_tensor(out=ex[:], in0=ex[:], in1=mk[:], op=ALU.mult), [ex, mk], [ex])
                P.op(VE, lambda e: e.reduce_sum(out=ss[:], in_=ex[:], axis=mybir.AxisListType.X), [ex], [ss])
                P.op(VE, lambda e: e.reciprocal(out=rs[:], in_=ss[:]), [ss], [rs])
                P.op(VE, lambda e, t=t: e.tensor_scalar(out=gates[:, t, :], in0=ex[:], scalar1=rs[:, 0:1], scalar2=None,
                                                        op0=ALU.mult), [ex, rs], [gates])
                P.op(GP, lambda e, t=t: e.tensor_copy(out=gatesE[:, :, t], in_=gates[:, t, :]), [gates], [gatesE])
            if dbg and "gates" in dbg:
                P.dma(SY, dbg_aps["gates"][:, :], gates[:].rearrange("p t e -> p (t e)"), reads=[gates], sembuf=gates)

            if acc_in:
                for t in range(NT):
                    P.dma(SY, acc[:, t, :], acc_prev[t * 128:(t + 1) * 128, :], writes=[acc])
            P.dma(SY, g_scr[:, :], gatesE[:].rearrange("p e t -> p (e t)"), reads=[gatesE], writes=[gsd], sembuf=gatesE)
            w_gu_k = w_gu.rearrange("e (k p) n -> e p k n", p=128)
            w_dn_k = w_down.rearrange("e (k p) n -> e p k n", p=128)
            cnts = {"it": 0, "st": 0, "gi": 0}

            pend = [None]

            def down_stage(g, b, aT):
                for tt in range(4):
                    t = g * 4 + tt
                    for n in range(2):
                        pY = next_ps()
                        for fc in range(4):
                            P.op(PE, lambda e, fc=fc, tt=tt, n=n, b=b, pY=pY, aT=aT: e.matmul(
                                pY[:, :], lhsT=aT[:, fc, tt * 128:(tt + 1) * 128],
                                rhs=wd[b][:, fc, n * 512:(n + 1) * 512],
                                start=(fc == 0), stop=(fc == 3)), [aT, wd[b]], [pY], sig=(fc == 3))
                        P.op(VE, lambda e, pY=pY, t=t, n=n: e.scalar_tensor_tensor(
                            out=acc[:, t, n * 512:(n + 1) * 512], in0=pY[:, :],
                            scalar=gce[:, t:t + 1], in1=acc[:, t, n * 512:(n + 1) * 512],
                            op0=ALU.mult, op1=ALU.add), [pY, gce, acc], [acc])

            def expert_iter(ex_s):
                def esel(i):
                    return ex_s if i is None else i

                P.dma(SY, bge[:], lambda i: bgu_col[:, bass.ds(esel(i) * 16, 16)], writes=[bge])
                P.dma(SY, gce[:], lambda i: g_scr[:, bass.ds(esel(i) * NT, NT)], reads=[gsd], writes=[gce])
                P.op(VE, lambda e: e.tensor_scalar(out=bgu1[:], in0=bge[:, 8:16], scalar1=1.0, scalar2=None,
                                                   op0=ALU.add), [bge], [bgu1])
                for hf in range(2):
                    b = cnts["it"] % 2
                    cnts["it"] += 1
                    P.dma(GP, wg[b][:], lambda i, hf=hf: w_gu_k[bass.ds(esel(i), 1), :, :, hf * 512:(hf + 1) * 512]
                          .rearrange("o p k n -> p (o k) n"), writes=[wg[b]])
                    P.dma(GP, wu[b][:], lambda i, hf=hf: w_gu_k[bass.ds(esel(i), 1), :, :, D + hf * 512:D + (hf + 1) * 512]
                          .rearrange("o p k n -> p (o k) n"), writes=[wu[b]])
                    for fc in range(4):
                        st = wst[cnts["st"] % 4]
                        cnts["st"] += 1
                        P.dma(SY, st[:], lambda i, hf=hf, fc=fc: w_dn_k[bass.ds(esel(i), 1), :, hf * 4 + fc, :]
                              .rearrange("o p n -> p (o n)"), writes=[st])
                        P.op(GP, lambda e, st=st, b=b, fc=fc: e.tensor_tensor(out=wd[b][:, fc, :], in0=st[:], in1=gf_b[:],
                                                                              op=ALU.mult), [st, gf_b], [wd[b]])
                    for g in range(NG):
                        t0 = g * 512
                        aT = actT[cnts["gi"] % 2]
                        tc = tcs[cnts["gi"] % 2]
                        cnts["gi"] += 1
                        for fc in range(4):
                            cg = hf * 4 + fc
                            cuu = 8 + hf * 4 + fc
                            pG, pU = next_ps(), next_ps()
                            for k in range(8):
                                P.op(PE, lambda e, k=k, fc=fc, b=b, pG=pG, t0=t0: e.matmul(
                                    pG[:, :], lhsT=wg[b][:, k, fc * 128:(fc + 1) * 128], rhs=h2T[:, k, t0:t0 + 512],
                                    start=(k == 0), stop=(k == 7)), [wg[b], h2T], [pG], sig=(k == 7))
                            for k in range(8):
                                P.op(PE, lambda e, k=k, fc=fc, b=b, pU=pU, t0=t0: e.matmul(
                                    pU[:, :], lhsT=wu[b][:, k, fc * 128:(fc + 1) * 128], rhs=h2T[:, k, t0:t0 + 512],
                                    start=(k == 0), stop=(k == 7)), [wu[b], h2T], [pU], sig=(k == 7))
                            gcl, sg_, ub, uc, gs = tc
                            P.op(VE, lambda e, pG=pG, cg=cg, gcl=gcl: e.tensor_scalar(
                                out=gcl[:], in0=pG[:, :], scalar1=bge[:, cg:cg + 1], scalar2=7.0,
                                op0=ALU.add, op1=ALU.min), [pG, bge], [gcl])
                            P.op(AC, lambda e, gcl=gcl, sg_=sg_: e.activation(out=sg_[:], in_=gcl[:], func=AF.Sigmoid,
                                                                              scale=1.702), [gcl], [sg_])
                            P.op(VE, lambda e, pU=pU, cg=cg, ub=ub: e.tensor_scalar(
                                out=ub[:], in0=pU[:, :], scalar1=bgu1[:, cg:cg + 1], scalar2=8.0,
                                op0=ALU.add, op1=ALU.min), [pU, bgu1], [ub])
                            P.op(VE, lambda e, gcl=gcl, sg_=sg_, gs=gs: e.tensor_tensor(out=gs[:], in0=gcl[:], in1=sg_[:],
                                                                                        op=ALU.mult), [gcl, sg_], [gs])
                            P.op(VE, lambda e, ub=ub, gs=gs, aT=aT, fc=fc: e.scalar_tensor_tensor(
                                out=aT[:, fc, :], in0=ub[:], scalar=-6.0, in1=gs[:], op0=ALU.max, op1=ALU.mult),
                                [ub, gs], [aT])
                        if pend[0] is not None:
                            down_stage(*pend[0])
                        pend[0] = (g, b, aT)
                if pend[0] is not None:
                    down_stage(*pend[0])
                    pend[0] = None

            e_end = e_off + n_experts
            if n_experts <= 4:
                for ex_i in range(e_off, e_end):
                    expert_iter(ex_i)
            else:
                expert_iter(e_off)
                expert_iter(e_off + 1)
                m_a = P.mark()
                expert_iter(e_off + 2)
                m_b = P.mark()
                expert_iter(e_off + 3)
                P.make_loop(m_a, m_b, e_off + 3, e_end)

            if final:
                last = None
                gfin, bd = wst[2], wst[3]
                P.dma(SY, gfin[:], gfin_b[:, :], writes=[gfin])
                P.dma(SY, bd[0:32, :], b_down[:, :], writes=[bd])
                P.op(VE, lambda e: e.tensor_tensor(out=bd[0:32, :], in0=bd[0:32, :], in1=gf_b[0:32, :], op=ALU.mult),
                     [bd, gf_b], [bd])
                for t in range(NT):
                    r0 = t * 128
                    pg = next_ps()
                    P.op(PE, lambda e, t=t, pg=pg: e.transpose(pg[0:32, 0:128], gates[:, t, :], ident_f[:]),
                         [gates, ident_f], [pg])
                    P.op(VE, lambda e, pg=pg: e.tensor_copy(out=gT[:], in_=pg[0:32, 0:128]), [pg], [gT])
                    for n in range(2):
                        pbd = next_ps()
                        P.op(PE, lambda e, n=n, pbd=pbd: e.matmul(
                            pbd[:, :], lhsT=gT[0:32, :], rhs=bd[0:32, n * 512:(n + 1) * 512],
                            start=True, stop=True), [gT, bd], [pbd])
                        P.op(VE, lambda e, t=t, n=n, pbd=pbd: e.tensor_tensor(
                            out=acc[:, t, n * 512:(n + 1) * 512], in0=pbd[:, :], in1=acc[:, t, n * 512:(n + 1) * 512],
                            op=ALU.add), [pbd, acc], [acc])
                    P.op(VE, lambda e: e.memset(ss[:], 0.0), [], [ss])
                    P.op(AC, lambda e, t=t: e.activation(out=xn[:], in_=acc[:, t, :], func=AF.Square, accum_out=ss[:]),
                         [acc, ss], [xn, ss])
                    P.op(VE, lambda e: e.tensor_scalar(out=rs[:], in0=ss[:], scalar1=1.0 / D, scalar2=EPS,
                                                       op0=ALU.mult, op1=ALU.add), [ss], [rs])
                    P.op(AC, lambda e: e.sqrt(out=rs[:], in_=rs[:]), [rs], [rs]); P.op(VE, lambda e: e.reciprocal(out=rs[:], in_=rs[:]), [rs], [rs])
                    o = ot[t % 2]
                    P.op(VE, lambda e, t=t, o=o: e.scalar_tensor_tensor(out=o[:], in0=acc[:, t, :], scalar=rs[:, 0:1],
                                                                        in1=gfin[:], op0=ALU.mult, op1=ALU.mult),
                         [acc, rs, gfin], [o])
                    last = P.dma(SY, y[r0:r0 + 128, :], o[:], reads=[o], writes=[yd], sembuf=o)
            else:
                for t in range(NT):
                    r0 = t * 128
                    o = ot[t % 2]
                    P.op(GP, lambda e, t=t, o=o: e.tensor_copy(out=o[:], in_=acc[:, t, :]), [acc], [o])
                    P.dma(SY, y[r0:r0 + 128, :], o[:], reads=[o], writes=[yd], sembuf=o)
            for o in wst:
                P.wait_tok(SY, Tok(o.dsem, o.dcnt))
        P.finish()
    return nc


def _host_layout(inputs):
    f = np.float32
    x = np.asarray(inputs["x"], f)
    c = np.asarray(inputs["c"], f)
    pos = np.asarray(inputs["positions"], np.int32)
    w_in = np.ascontiguousarray(np.asarray(inputs["w_in"], f)[0])
    w_uq = np.asarray(inputs["w_uq"], f)[0]
    w_ukv = np.asarray(inputs["w_ukv"], f)[0]
    col = lambda v, n: np.ascontiguousarray(np.asarray(v, f).reshape(n, 128).T)
    w_kpe_sw = np.ascontiguousarray(np.concatenate([w_in[:, 320:384], w_in[:, 400:416], w_in[:, 384:400]], axis=1))
    wq3 = w_uq.reshape(256, NH, 96)
    w_uq_sw = np.ascontiguousarray(
        np.concatenate([wq3[:, :, 0:64], wq3[:, :, 80:96], wq3[:, :, 64:80]], axis=2).reshape(256, 768))
    wkv3 = w_ukv.reshape(128, NH, 128)
    w_uk = np.ascontiguousarray(wkv3[:, :, 0:64].reshape(128, 512))
    w_uv = np.ascontiguousarray(wkv3[:, :, 64:128].reshape(128, 512))
    conv_w = np.asarray(inputs["conv_w"], f)[0]
    conv_col = np.ascontiguousarray(conv_w.reshape(3, 4, 128).transpose(2, 1, 0).reshape(128, 12))
    b_gu = np.asarray(inputs["b_gu"], f)[0]
    bgu_col = np.ascontiguousarray(b_gu.reshape(NE, 16, 128).transpose(2, 0, 1).reshape(128, NE * 16))
    inv_freq = (1.0 / (10000.0 ** (np.arange(0, 32, 2, dtype=np.float32) / np.float32(32)))).astype(f)
    rope_c = np.zeros((128, 2), f)
    for p in range(64, 96):
        rope_c[p, 0] = inv_freq[(p - 64) % 16]
        rope_c[p, 1] = -1.0 if p < 80 else 1.0
    shared = {
        "w_ada": np.ascontiguousarray(np.asarray(inputs["w_ada"], f)[0]),
        "b_ada": np.ascontiguousarray(np.asarray(inputs["b_ada"], f)[0].reshape(1, -1)),
        "gmix_col": col(np.asarray(inputs["norm_mix_g"])[0], 8),
        "gffn_col": col(np.asarray(inputs["norm_ffn_g"])[0], 8),
        "w_in": w_in,
        "w_kpe_sw": w_kpe_sw,
        "qg_col": col(np.asarray(inputs["q_norm_g"])[0], 2),
        "kvg_col": col(np.asarray(inputs["kv_norm_g"])[0], 1),
        "w_uq": np.ascontiguousarray(w_uq),
        "w_uq_sw": w_uq_sw,
        "w_uk": w_uk,
        "w_uv": w_uv,
        "w_up_attn": np.ascontiguousarray(np.asarray(inputs["w_up_attn"], f)[0]),
        "conv_col": conv_col,
        "w_up_conv": np.ascontiguousarray(np.asarray(inputs["w_up_conv"], f)[0]),
        "w_o": np.ascontiguousarray(np.asarray(inputs["w_o"], f)[0]),
        "router_w": np.ascontiguousarray(np.asarray(inputs["router_w"], f)[0]),
        "router_b": np.ascontiguousarray(np.asarray(inputs["router_b"], f)[0].reshape(1, -1)),
        "w_gu": np.ascontiguousarray(np.asarray(inputs["w_gu"], f)[0]),
        "bgu_col": bgu_col,
        "w_down": np.ascontiguousarray(np.asarray(inputs["w_down"], f)[0]),
        "b_down": np.ascontiguousarray(np.asarray(inputs["b_down"], f)[0]),
        "gfin_b": np.ascontiguousarray(np.broadcast_to(np.asarray(inputs["norm_final_g"], f)[None, :], (128, D))),
        "rope_c": rope_c,
        "ident": np.eye(128, dtype=f),
    }
    in_maps = []
    for i in range(8):
        b, half = i // 2, i % 2
        own = slice(half * TOK, (half + 1) * TOK)
        flags = np.zeros((128, 2), f)
        flags[:, 0] = 0.0 if half == 1 else NEG
        flags[:, 1] = 1.0 if half == 1 else 0.0
        m = dict(shared)
        m["x_own"] = np.ascontiguousarray(x[b, own])
        m["x_ctx"] = np.ascontiguousarray(x[b, 0:TOK])
        m["pos_all"] = np.ascontiguousarray(np.concatenate([pos[b, 0:TOK], pos[b, own]]).reshape(1, SEQ))
        m["flags"] = flags
        m["c_col"] = col(c[b], 8)
        in_maps.append(m)
    return in_maps


def kernel(**inputs):
    in_maps = _host_layout(inputs)
    nc = build_program()
    res = run_bass_kernel_spmd(nc, in_maps, core_ids=list(range(8)))
    out = np.zeros((4, SEQ, D), np.float32)
    for i in range(8):
        b, half = i // 2, i % 2
        out[b, half * TOK:(half + 1) * TOK] = res.results[i]["y"]
    return out
```
